# Optimizing a Trainium2 kernel written in Bass

```python
import math
import jax, jax.numpy as jnp
from jax import lax
import numpy as np

D_MODEL = 1024
BATCH = 16
SEQ = 2048
DEPTH = 1

CHUNK = 64
LEFT_CHUNKS = 8
BAND = LEFT_CHUNKS + 1
ATTN_HEADS = 8
HEAD_DIM = 64
D_ATTN = ATTN_HEADS * HEAD_DIM
MAX_REL = 256
REL_FUTURE = CHUNK - 1
N_REL = REL_FUTURE + MAX_REL + 1
D_SSM = D_MODEL // 2
SSM_GROUP = 16
SSM_GROUPS = D_SSM // SSM_GROUP
SSM_STATE = 64
STEP_MIN = 0.001
STEP_MAX = 0.1
N_BRANCH = 2
D_IN = 3 * D_ATTN + D_SSM + N_BRANCH * D_MODEL
N_GROUPS = 4
EXPERTS_PER_GROUP = 4
N_EXPERTS = N_GROUPS * EXPERTS_PER_GROUP
TOP_K = 2
D_EXPERT = D_MODEL // 4
EPS = 1e-6
NEG_INF = -1e30

kernel_name = 'hybrid_chunk_attn_s5_hmoe'


def rms_norm(x, gain):
    xf = x.astype(jnp.float32)
    y = xf * lax.rsqrt(jnp.mean(xf * xf, axis=-1, keepdims=True) + EPS)
    return (y * gain.astype(jnp.float32)).astype(x.dtype)


def chunked_relpos_attention(q, k, v, q_gain, k_gain, rel_bias):
    b, l = q.shape[0], q.shape[1]
    nc = l // CHUNK
    q = rms_norm(q, q_gain)
    k = rms_norm(k, k_gain)
    pad = ((0, 0), (LEFT_CHUNKS * CHUNK, 0), (0, 0), (0, 0))
    kp = jnp.pad(k, pad).reshape(b, nc + LEFT_CHUNKS, CHUNK, ATTN_HEADS, HEAD_DIM)
    vp = jnp.pad(v, pad).reshape(b, nc + LEFT_CHUNKS, CHUNK, ATTN_HEADS, HEAD_DIM)
    band = jnp.arange(nc)[:, None] + jnp.arange(BAND)[None, :]
    kb = kp[:, band].reshape(b, nc, BAND * CHUNK, ATTN_HEADS, HEAD_DIM)
    vb = vp[:, band].reshape(b, nc, BAND * CHUNK, ATTN_HEADS, HEAD_DIM)
    qc = q.reshape(b, nc, CHUNK, ATTN_HEADS, HEAD_DIM)
    s = jnp.einsum('bnqhd,bnkhd->bhnqk', qc, kb).astype(jnp.float32) * (HEAD_DIM ** -0.5)
    q_off = jnp.arange(CHUNK)[:, None] + LEFT_CHUNKS * CHUNK
    k_off = jnp.arange(BAND * CHUNK)[None, :]
    rel_idx = jnp.clip(q_off - k_off, -REL_FUTURE, MAX_REL) + REL_FUTURE
    bias = rel_bias.astype(jnp.float32)[:, rel_idx]
    key_pos = (jnp.arange(nc)[:, None] - LEFT_CHUNKS) * CHUNK + k_off
    valid = key_pos >= 0
    s = jnp.where(valid[None, None, :, None, :], s + bias[None, :, None], NEG_INF)
    p = jax.nn.softmax(s, axis=-1).astype(v.dtype)
    o = jnp.einsum('bhnqk,bnkhd->bnqhd', p, vb)
    return o.reshape(b, l, D_ATTN)


def _complex_linear_combine(e1, e2):
    a1r, a1i, b1r, b1i = e1
    a2r, a2i, b2r, b2i = e2
    ar = a2r * a1r - a2i * a1i
    ai = a2r * a1i + a2i * a1r
    br = a2r * b1r - a2i * b1i + b2r
    bi = a2r * b1i + a2i * b1r + b2i
    return (ar, ai, br, bi)


def s5_ssm_glu(u, lambda_re, lambda_im, log_step, b_re, b_im, c_re, c_im, d_skip, w_glu, b_glu):
    bsz, l = u.shape[0], u.shape[1]
    uf = u.astype(jnp.float32).reshape(bsz, l, SSM_GROUPS, SSM_GROUP)
    lre = lambda_re.astype(jnp.float32)
    lim = lambda_im.astype(jnp.float32)
    step = jnp.exp(log_step.astype(jnp.float32))[:, None]
    mag = jnp.exp(lre * step)
    ang = lim * step
    a_re = mag * jnp.cos(ang)
    a_im = mag * jnp.sin(ang)
    num_re = a_re - 1.0
    num_im = a_im
    den = lre * lre + lim * lim
    f_re = (num_re * lre + num_im * lim) / den
    f_im = (num_im * lre - num_re * lim) / den
    br = b_re.astype(jnp.float32)
    bi = b_im.astype(jnp.float32)
    bb_re = f_re[..., None] * br - f_im[..., None] * bi
    bb_im = f_re[..., None] * bi + f_im[..., None] * br
    bu_re = jnp.einsum('blgc,gpc->blgp', uf, bb_re)
    bu_im = jnp.einsum('blgc,gpc->blgp', uf, bb_im)
    a_re_t = jnp.broadcast_to(a_re[None, None], (1, l, SSM_GROUPS, SSM_STATE))
    a_im_t = jnp.broadcast_to(a_im[None, None], (1, l, SSM_GROUPS, SSM_STATE))
    _, _, s_re, s_im = lax.associative_scan(
        _complex_linear_combine, (a_re_t, a_im_t, bu_re, bu_im), axis=1)
    y = (jnp.einsum('blgp,gcp->blgc', s_re, c_re.astype(jnp.float32))
         - jnp.einsum('blgp,gcp->blgc', s_im, c_im.astype(jnp.float32))
         + d_skip.astype(jnp.float32).reshape(SSM_GROUPS, SSM_GROUP) * uf)
    y = y.reshape(bsz, l, D_SSM).astype(u.dtype)
    z = jax.nn.gelu(y)
    return z * jax.nn.sigmoid(z @ w_glu + b_glu)


def hierarchical_moe(h, w_group_router, group_bias, w_expert_router, expert_bias, w_e_gate, w_e_up, w_e_down):
    bsz, l, d = h.shape
    t = bsz * l
    ht = h.reshape(t, d)
    group_logits = (ht @ w_group_router).astype(jnp.float32) + group_bias.astype(jnp.float32)
    group_prob = jax.nn.softmax(group_logits, axis=-1)
    g_prob, g_idx = lax.top_k(group_prob, 1)
    expert_logits = (ht @ w_expert_router).astype(jnp.float32) + expert_bias.astype(jnp.float32)
    expert_logits = expert_logits.reshape(t, N_GROUPS, EXPERTS_PER_GROUP)
    in_group = jnp.take_along_axis(expert_logits, g_idx[:, :, None], axis=1)[:, 0]
    e_prob = jax.nn.softmax(in_group, axis=-1)
    e_val, e_loc = lax.top_k(e_prob, TOP_K)
    e_val = e_val / jnp.sum(e_val, axis=-1, keepdims=True)
    weights = g_prob * e_val
    e_glob = g_idx * EXPERTS_PER_GROUP + e_loc
    gates = jnp.sum(jax.nn.one_hot(e_glob, N_EXPERTS, dtype=jnp.float32) * weights[..., None], axis=1)
    gates = gates.astype(h.dtype)
    a = jnp.einsum('td,edf->tef', ht, w_e_gate)
    u = jnp.einsum('td,edf->tef', ht, w_e_up)
    hid = jax.nn.silu(a) * u * gates[:, :, None]
    out = hid.reshape(t, N_EXPERTS * D_EXPERT) @ w_e_down.reshape(N_EXPERTS * D_EXPERT, d)
    return out.reshape(bsz, l, d)


def setup_inputs(seed: int = 0) -> dict:
    key = jax.random.key(seed)
    ks = jax.random.split(key, 32)
    f32 = jnp.float32

    def nrm(k, shape, scale):
        return jax.random.normal(k, shape, f32) * scale

    n_idx = jnp.arange(SSM_STATE, dtype=f32)
    return {
        'x': nrm(ks[0], (BATCH, SEQ, D_MODEL), 1.0),
        'mix_norm_gain': 1.0 + nrm(ks[1], (DEPTH, D_MODEL), 0.02),
        'w_in': nrm(ks[2], (DEPTH, D_MODEL, D_IN), D_MODEL ** -0.5),
        'b_gate': nrm(ks[3], (DEPTH, N_BRANCH * D_MODEL), 0.02),
        'q_gain': 1.0 + nrm(ks[4], (DEPTH, HEAD_DIM), 0.02),
        'k_gain': 1.0 + nrm(ks[5], (DEPTH, HEAD_DIM), 0.02),
        'rel_bias': nrm(ks[6], (DEPTH, ATTN_HEADS, N_REL), 0.1),
        'ssm_lambda_re': -0.5 + nrm(ks[7], (DEPTH, SSM_GROUPS, SSM_STATE), 0.01),
        'ssm_lambda_im': math.pi * n_idx[None, None, :] + nrm(ks[8], (DEPTH, SSM_GROUPS, SSM_STATE), 0.01),
        'ssm_log_step': jax.random.uniform(ks[9], (DEPTH, SSM_GROUPS), f32, math.log(STEP_MIN), math.log(STEP_MAX)),
        'ssm_b_re': nrm(ks[10], (DEPTH, SSM_GROUPS, SSM_STATE, SSM_GROUP), (2 * SSM_GROUP) ** -0.5),
        'ssm_b_im': nrm(ks[11], (DEPTH, SSM_GROUPS, SSM_STATE, SSM_GROUP), (2 * SSM_GROUP) ** -0.5),
        'ssm_c_re': nrm(ks[12], (DEPTH, SSM_GROUPS, SSM_GROUP, SSM_STATE), (2 * SSM_STATE) ** -0.5),
        'ssm_c_im': nrm(ks[13], (DEPTH, SSM_GROUPS, SSM_GROUP, SSM_STATE), (2 * SSM_STATE) ** -0.5),
        'ssm_d': nrm(ks[14], (DEPTH, D_SSM), 1.0),
        'w_glu': nrm(ks[15], (DEPTH, D_SSM, D_SSM), D_SSM ** -0.5),
        'b_glu': nrm(ks[16], (DEPTH, D_SSM), 0.02),
        'w_branch': nrm(ks[17], (DEPTH, D_ATTN + D_SSM, D_MODEL), D_ATTN ** -0.5),
        'w_out': nrm(ks[18], (DEPTH, D_MODEL, D_MODEL), D_MODEL ** -0.5),
        'ffn_norm_gain': 1.0 + nrm(ks[19], (DEPTH, D_MODEL), 0.02),
        'w_group_router': nrm(ks[20], (DEPTH, D_MODEL, N_GROUPS), D_MODEL ** -0.5),
        'group_bias': nrm(ks[21], (DEPTH, N_GROUPS), 0.01),
        'w_expert_router': nrm(ks[22], (DEPTH, D_MODEL, N_EXPERTS), D_MODEL ** -0.5),
        'expert_bias': nrm(ks[23], (DEPTH, N_EXPERTS), 0.01),
        'w_e_gate': nrm(ks[24], (DEPTH, N_EXPERTS, D_MODEL, D_EXPERT), D_MODEL ** -0.5),
        'w_e_up': nrm(ks[25], (DEPTH, N_EXPERTS, D_MODEL, D_EXPERT), D_MODEL ** -0.5),
        'w_e_down': nrm(ks[26], (DEPTH, N_EXPERTS, D_EXPERT, D_MODEL), D_EXPERT ** -0.5),
    }


def reference(x, mix_norm_gain, w_in, b_gate, q_gain, k_gain, rel_bias,
              ssm_lambda_re, ssm_lambda_im, ssm_log_step, ssm_b_re, ssm_b_im,
              ssm_c_re, ssm_c_im, ssm_d, w_glu, b_glu, w_branch, w_out,
              ffn_norm_gain, w_group_router, group_bias, w_expert_router, expert_bias,
              w_e_gate, w_e_up, w_e_down):
    bsz, l, _ = x.shape
    for i in range(DEPTH):
        h = rms_norm(x, mix_norm_gain[i])
        proj = h @ w_in[i]
        q = proj[..., :D_ATTN].reshape(bsz, l, ATTN_HEADS, HEAD_DIM)
        k = proj[..., D_ATTN:2 * D_ATTN].reshape(bsz, l, ATTN_HEADS, HEAD_DIM)
        v = proj[..., 2 * D_ATTN:3 * D_ATTN].reshape(bsz, l, ATTN_HEADS, HEAD_DIM)
        u = proj[..., 3 * D_ATTN:3 * D_ATTN + D_SSM]
        gate = jax.nn.sigmoid(proj[..., 3 * D_ATTN + D_SSM:] + b_gate[i])
        gate = gate.reshape(bsz, l, N_BRANCH, D_MODEL)
        y_attn = chunked_relpos_attention(q, k, v, q_gain[i], k_gain[i], rel_bias[i])
        y_ssm = s5_ssm_glu(u, ssm_lambda_re[i], ssm_lambda_im[i], ssm_log_step[i],
                           ssm_b_re[i], ssm_b_im[i], ssm_c_re[i], ssm_c_im[i],
                           ssm_d[i], w_glu[i], b_glu[i])
        wb = w_branch[i]
        merged = (gate[:, :, 0] * (y_attn @ wb[:D_ATTN])
                  + gate[:, :, 1] * (y_ssm @ wb[D_ATTN:]))
        x = x + merged @ w_out[i]
        h = rms_norm(x, ffn_norm_gain[i])
        x = x + hierarchical_moe(h, w_group_router[i], group_bias[i], w_expert_router[i],
                                 expert_bias[i], w_e_gate[i], w_e_up[i], w_e_down[i])
    return x
```

```python
import math
from contextlib import ExitStack

import numpy as np
import concourse.bass as bass
import concourse.mybir as mybir
from concourse.bass_utils import run_bass_kernel_spmd

F32 = mybir.dt.float32
BF16 = mybir.dt.bfloat16
I32 = mybir.dt.int32
AF = mybir.ActivationFunctionType
ALU = mybir.AluOpType
AX = mybir.AxisListType

D = 1024
SEQ = 2048
NCORE = 8
TOK = 4096
NH = 8
DH = 64
DA = 512
DS = 512
DIN = 4096
NE = 16
FE = 256
EPS = 1e-6
NEG = -30000.0


class Buf:
    __slots__ = ("name", "w", "r", "dsem", "dcnt")

    def __init__(self, name):
        self.name = name
        self.w = None
        self.r = {}
        self.dsem = None
        self.dcnt = 0


class Sched:
    ENG = ("pe", "act", "dve", "pool", "sp")

    def __init__(self, nc, es):
        self.nc = nc
        self.es = es
        self.sem = {e: es.enter_context(nc.semaphore("sem_" + e)) for e in self.ENG}
        self.cnt = {e: 0 for e in self.ENG}
        self.known = {e: {} for e in self.ENG}
        self.nins = {e: 0 for e in self.ENG}
        self.engines = {"pe": nc.tensor, "act": nc.scalar, "dve": nc.vector, "pool": nc.gpsimd, "sp": nc.sync}
        self.semname = {}
        self.nsem = 0

    def _key(self, sem):
        return id(sem)

    def _need(self, e, ev, waits):
        if ev is None:
            return
        k = self.known[e]
        key = self._key(ev[0])
        if k.get(key, 0) >= ev[1]:
            return
        k[key] = ev[1]
        waits.append(ev)

    def _waits(self, e, reads, writes):
        waits = []
        for b in reads:
            self._need(e, b.w, waits)
        for b in writes:
            self._need(e, b.w, waits)
            for ev in b.r.values():
                self._need(e, ev, waits)
        return waits

    def _commit(self, ev, reads, writes):
        key = self._key(ev[0])
        for b in reads:
            old = b.r.get(key)
            if old is None or old[1] < ev[1]:
                b.r[key] = ev
        for b in writes:
            b.w = ev
            b.r = {}

    def _emit(self, e, waits, fn, ev, inc):
        engine = self.engines[e]
        for (s_, v) in waits:
            engine.wait_ge(s_, v)
        self.nins[e] += 1
        if fn is None:
            return
        ins = fn(engine)
        if ev is not None:
            ins.then_inc(ev[0], inc)

    def op(self, e, fn, reads=(), writes=()):
        waits = self._waits(e, reads, writes)
        self.cnt[e] += 1
        ev = (self.sem[e], self.cnt[e])
        self._emit(e, waits, fn, ev, 1)
        self._commit(ev, reads, writes)
        return ev

    def group(self, e, fns, reads=(), writes=()):
        waits = self._waits(e, reads, writes)
        self.cnt[e] += 1
        ev = (self.sem[e], self.cnt[e])
        n = len(fns)
        for i, fn in enumerate(fns):
            self._emit(e, waits if i == 0 else [], fn, ev if i == n - 1 else None, 1)
        self._commit(ev, reads, writes)
        return ev

    def dma(self, e, fn, owner, reads=(), writes=()):
        if owner.dsem is None:
            owner.dsem = self.es.enter_context(self.nc.semaphore("dsem_%d" % self.nsem))
            self.nsem += 1
        waits = self._waits(e, reads, writes)
        owner.dcnt += 16
        ev = (owner.dsem, owner.dcnt)
        self._emit(e, waits, fn, ev, 16)
        self._commit(ev, reads, writes)
        return ev

    def wait_event(self, e, ev):
        waits = []
        self._need(e, ev, waits)
        if waits:
            self._emit(e, waits, None, None, 0)

    def barrier(self):
        last = {e: (self.sem[e], self.cnt[e]) for e in self.ENG if self.cnt[e] > 0}
        for e in self.ENG:
            waits = []
            for f, ev in last.items():
                if f != e:
                    self._need(e, ev, waits)
            if waits:
                self._emit(e, waits, None, None, 0)

    def emit(self):
        pass


TWO_PI = 2.0 * math.pi


def build_program(TT=256, debug=(), stage=99, ntiles=None):
    nc = bass.Bass("TRN2", target_bir_lowering=False)
    es = ExitStack()
    S = Sched(nc, es)
    NT = TOK // TT
    TPS = SEQ // TT
    NB = TT // 128
    NK = TT // 8
    NSLOT = 512 // TT + 1
    LV = int(math.log2(NK))

    def dram_in(name, shape, dt=F32):
        return nc.dram_tensor(name, list(shape), dt, kind="ExternalInput").ap()

    xT_d = dram_in("xT", [D, TOK])
    w_in_d = dram_in("w_in", [D, DIN])
    w_glu_d = dram_in("w_glu", [DS, DS])
    w_br_d = dram_in("w_branch", [D, D])
    w_out_d = dram_in("w_out", [D, D])
    weg_d = dram_in("w_e_gate", [NE, D, FE])
    weu_d = dram_in("w_e_up", [NE, D, FE])
    wed_d = dram_in("w_e_down", [NE, FE, D])
    w_r_d = dram_in("w_r", [D, 20])
    vec_d = dram_in("vecs", [128, 64])
    biasT_d = dram_in("biasT", [128, 5 * 8 * 128])
    ssm_d = dram_in("ssm_small", [128, 48])
    ssm_bc_d = dram_in("ssm_bc", [128, 4, 256])
    drep_d = dram_in("drep", [128, 32])
    cst_d = dram_in("consts", [128, 128 * 3])
    outT_d = nc.dram_tensor("outT", [D, TOK], F32, kind="ExternalOutput").ap()
    dbg_d = {}
    for name, shape in debug:
        dbg_d[name] = nc.dram_tensor("dbg_" + name, list(shape), F32, kind="ExternalOutput").ap()

    def sbt(stack, name, shape, dt=F32):
        return stack.enter_context(nc.sbuf_tensor("sb_" + name, list(shape), dt))

    def sb(name, shape, dt=F32):
        return sbt(es, name, shape, dt)

    PS = [es.enter_context(nc.psum_tensor("ps%d" % i, [128, 512], F32)) for i in range(8)]
    b_PS = [Buf("ps%d" % i) for i in range(8)]
    ps_ctr = [0]

    def next_ps():
        i = ps_ctr[0] % 8
        ps_ctr[0] += 1
        return PS[i], b_PS[i]

    w_in_s = nc.dram_tensor("w_in_bf", [D, DIN], BF16).ap()
    wgu_s = nc.dram_tensor("wgu_bf", [NE, 2, 128, 2, 8, 128], BF16).ap()
    wd_s = nc.dram_tensor("wd_bf", [4, 8, 128, 8, 128], BF16).ap()
    b_w_in_s = Buf("w_in_s")
    wbr_s = nc.dram_tensor("wbr_bf", [D, D], BF16).ap()
    wout_s = nc.dram_tensor("wout_bf", [D, D], BF16).ap()
    b_wbr_s = Buf("wbr_s")
    b_wout_s = Buf("wout_s")
    b_wgu_s = Buf("wgu_s")
    b_wd_s = Buf("wd_s")

    cst = sb("cst", [128, 128 * 3])
    identf = cst[:, 0:128]
    maskLT = cst[:, 128:256]
    bd64f = cst[:, 256:384]
    vec = sb("vec", [128, 64])
    b_par = Buf("params")
    S.dma("sp", lambda q: q.dma_start(out=cst[:], in_=cst_d), b_par, writes=[b_par])
    S.dma("sp", lambda q: q.dma_start(out=vec[:], in_=vec_d), b_par, writes=[b_par])
    g1_sb = vec[:, 0:8]
    g2_sb = vec[:, 8:16]
    bgate_sb = vec[:, 16:32]
    bglu_sb = vec[:, 32:36]
    rbias_sb = vec[:, 40:60]
    hbias = sb("hbias", [128, 20])
    S.op("dve", lambda v: v.tensor_scalar(out=hbias[:], in0=vec[:, 16:36], scalar1=0.5, scalar2=None, op0=ALU.mult),
         reads=[b_par], writes=[b_par])
    cq = sb("cq", [128, 1])
    S.op("dve", lambda v: v.tensor_scalar(out=cq[:], in0=vec[:, 36:37], scalar1=vec[:, 37:38],
                                          scalar2=0.125, op0=ALU.mult, op1=ALU.mult),
         reads=[b_par], writes=[b_par])
    ones_bf = sb("ones_bf", [128, 128], BF16)
    identb = sb("identb", [128, 128], BF16)
    bd64 = sb("bd64", [128, 128], BF16)
    ones16f = sb("ones16f", [16, 128])
    S.op("pool", lambda g: g.memset(ones_bf[:], 1.0), writes=[b_par])
    S.op("pool", lambda g: g.memset(ones16f[:], 1.0), writes=[b_par])
    S.op("dve", lambda v: v.tensor_copy(out=identb[:], in_=identf), reads=[b_par], writes=[b_par])
    S.op("dve", lambda v: v.tensor_copy(out=bd64[:], in_=bd64f), reads=[b_par], writes=[b_par])

    wglu = sb("wglu", [128, 4, DS], BF16)
    wr = sb("wr", [128, 8, 20], BF16)
    b_wres = Buf("wres")
    for (tl, src) in ((wglu, w_glu_d), (wr, w_r_d)):
        S.dma("pool", lambda q, tl=tl, src=src: q.dma_start(
            out=tl[:], in_=src.rearrange("(c p) f -> p c f", p=128)), b_wres, writes=[b_wres])

    biasT = sb("biasT", [128, 5 * 8 * 128], BF16)
    b_bias = Buf("biasT")
    S.dma("pool", lambda q: q.dma_start(out=biasT[:], in_=biasT_d), b_bias, writes=[b_bias])
    biasT4 = biasT[:].rearrange("p (kb h q) -> p kb h q", kb=5, h=8)

    T_all = sb("T_all", [128, 32, 128], BF16)
    MBre = sb("MBre", [128, 32, 64], BF16)
    MBim = sb("MBim", [128, 32, 64], BF16)
    MCre = sb("MCre", [128, 32, 128], BF16)
    MCim = sb("MCim", [128, 32, 128], BF16)
    Ere = sb("Ere", [128, 16, NK])
    Eim = sb("Eim", [128, 16, NK])
    Rtab = sb("Rtab", [128, 16, NK])
    A8 = sb("A8", [128, 2, 16])
    b_tab = Buf("ssm_tables")

    with ExitStack() as ps_:
        def tb(name, shape, dt=F32):
            return sbt(ps_, name, shape, dt)
        small = tb("ssm_small", [128, 48])
        bc = tb("ssm_bc", [128, 4, 16, 16])
        drep = tb("drep", [128, 32])
        b_p = Buf("prep")
        S.dma("sp", lambda q: q.dma_start(out=small[:], in_=ssm_d), b_p, writes=[b_p])
        S.dma("sp", lambda q: q.dma_start(out=bc[:], in_=ssm_bc_d.rearrange("p a (g c) -> p a g c", g=16)),
              b_p, writes=[b_p])
        S.dma("sp", lambda q: q.dma_start(out=drep[:], in_=drep_d), b_p, writes=[b_p])
        lre = small[:, 0:16]
        lim = small[:, 16:32]
        lst = small[:, 32:48]
        W = tb("wk", [128, 24, 16])
        (STEP, XR, MAG, MAGI, ANG, TQ, TF, RS, RC, SN, CS, ARE, AIM, IRE, IIM, NRE, DEN, RDEN,
         FRE, FIM, T1, T2, T3, T4) = [W[:, i, :] for i in range(24)]
        WI = tb("wki", [128, 16], I32)

        def P(eng, fn):
            S.op(eng, fn, reads=[b_p, b_par], writes=[b_p])

        def tt(out, a, b, op):
            P("dve", lambda v: v.tensor_tensor(out=out, in0=a, in1=b, op=op))

        def ts(out, a, s1, op0, s2=None, op1=None):
            if op1 is None:
                P("dve", lambda v: v.tensor_scalar(out=out, in0=a, scalar1=s1, scalar2=None, op0=op0))
            else:
                P("dve", lambda v: v.tensor_scalar(out=out, in0=a, scalar1=s1, scalar2=s2, op0=op0, op1=op1))

        def actf(out, a, func, scale=1.0, bias=0.0):
            P("act", lambda x: x.activation(out=out, in_=a, func=func, scale=scale, bias=bias))

        def cmul(ore, oim, xre, xim, yre, yim, t1, t2):
            tt(t1, xre, yre, ALU.mult)
            tt(t2, xim, yim, ALU.mult)
            tt(ore, t1, t2, ALU.subtract)
            tt(t1, xre, yim, ALU.mult)
            tt(t2, xim, yre, ALU.mult)
            tt(oim, t1, t2, ALU.add)

        actf(STEP, lst, AF.Exp)
        tt(XR, lre, STEP, ALU.mult)
        actf(MAG, XR, AF.Exp)
        actf(MAGI, XR, AF.Exp, scale=-1.0)
        tt(ANG, lim, STEP, ALU.mult)
        ts(TQ, ANG, 1.0 / TWO_PI, ALU.mult)
        P("dve", lambda v: v.tensor_copy(out=WI[:], in_=TQ))
        P("dve", lambda v: v.tensor_copy(out=TF, in_=WI[:]))
        P("dve", lambda v: v.scalar_tensor_tensor(out=RS, in0=TF, scalar=-TWO_PI, in1=ANG,
                                                  op0=ALU.mult, op1=ALU.add))

        def wrap(x):
            ts(T1, x, math.pi, ALU.is_gt, TWO_PI, ALU.mult)
            tt(x, x, T1, ALU.subtract)
            ts(T1, x, -math.pi, ALU.is_lt, TWO_PI, ALU.mult)
            tt(x, x, T1, ALU.add)

        wrap(RS)
        ts(RC, RS, math.pi / 2, ALU.add)
        wrap(RC)
        actf(SN, RS, AF.Sin)
        actf(CS, RC, AF.Sin)
        tt(ARE, MAG, CS, ALU.mult)
        tt(AIM, MAG, SN, ALU.mult)
        tt(IRE, MAGI, CS, ALU.mult)
        tt(T1, MAGI, SN, ALU.mult)
        ts(IIM, T1, -1.0, ALU.mult)
        ts(NRE, ARE, -1.0, ALU.add)
        tt(T1, lre, lre, ALU.mult)
        tt(T2, lim, lim, ALU.mult)
        tt(DEN, T1, T2, ALU.add)
        P("dve", lambda v: v.reciprocal(out=RDEN, in_=DEN))
        tt(T1, NRE, lre, ALU.mult)
        tt(T2, AIM, lim, ALU.mult)
        tt(T1, T1, T2, ALU.add)
        tt(FRE, T1, RDEN, ALU.mult)
        tt(T1, AIM, lre, ALU.mult)
        tt(T2, NRE, lim, ALU.mult)
        tt(T1, T1, T2, ALU.subtract)
        tt(FIM, T1, RDEN, ALU.mult)
        BB = tb("bb", [128, 2, 16, 16])
        TB = tb("tbb", [128, 2, 16, 16])
        bre_, bim_, cre_, cim_ = bc[:, 0], bc[:, 1], bc[:, 2], bc[:, 3]

        def bcg(x):
            return x.rearrange("p (g o) -> p g o", o=1).to_broadcast([128, 16, 16])

        tt(TB[:, 0], bre_, bcg(FRE), ALU.mult)
        tt(TB[:, 1], bim_, bcg(FIM), ALU.mult)
        tt(BB[:, 0], TB[:, 0], TB[:, 1], ALU.subtract)
        tt(TB[:, 0], bim_, bcg(FRE), ALU.mult)
        tt(TB[:, 1], bre_, bcg(FIM), ALU.mult)
        tt(BB[:, 1], TB[:, 0], TB[:, 1], ALU.add)
        Pre = tb("Pre", [128, 9, 16])
        Pim = tb("Pim", [128, 9, 16])
        Qre = tb("Qre", [128, 9, 16])
        Qim = tb("Qim", [128, 9, 16])
        for (pr, pi, xr_, xi_) in ((Pre, Pim, ARE, AIM), (Qre, Qim, IRE, IIM)):
            P("dve", lambda v, pr=pr: v.memset(pr[:, 0, :], 1.0))
            P("dve", lambda v, pi=pi: v.memset(pi[:, 0, :], 0.0))
            for d in range(1, 9):
                cmul(pr[:, d, :], pi[:, d, :], pr[:, d - 1, :], pi[:, d - 1, :], xr_, xi_, T1, T2)
        P("dve", lambda v: v.tensor_copy(out=A8[:, 0, :], in_=Pre[:, 8, :]))
        P("dve", lambda v: v.tensor_copy(out=A8[:, 1, :], in_=Pim[:, 8, :]))
        MCf = tb("MCf", [128, 2, 16, 128])
        XTf = tb("XTf", [128, 2, 16, 128])
        MBf = tb("MBf", [128, 2, 16, 128])
        TMP = tb("TMPf", [128, 2, 16, 16])

        def v4(x, i):
            return x[:, i].rearrange("p g (t c) -> p g t c", t=8)

        for tq in range(8):
            prb = bcg(Pre[:, tq + 1, :])
            pib = bcg(Pim[:, tq + 1, :])
            tt(TMP[:, 0], cre_, prb, ALU.mult)
            tt(TMP[:, 1], cim_, pib, ALU.mult)
            tt(v4(MCf, 0)[:, :, tq, :], TMP[:, 0], TMP[:, 1], ALU.subtract)
            tt(TMP[:, 0], cre_, pib, ALU.mult)
            tt(TMP[:, 1], cim_, prb, ALU.mult)
            tt(TMP[:, 0], TMP[:, 0], TMP[:, 1], ALU.add)
            ts(v4(MCf, 1)[:, :, tq, :], TMP[:, 0], -1.0, ALU.mult)
            for (dst, pr, pi, d) in ((MBf, Pre, Pim, 7 - tq), (XTf, Qre, Qim, tq + 1)):
                prb2 = bcg(pr[:, d, :])
                pib2 = bcg(pi[:, d, :])
                tt(TMP[:, 0], BB[:, 0], prb2, ALU.mult)
                tt(TMP[:, 1], BB[:, 1], pib2, ALU.mult)
                tt(v4(dst, 0)[:, :, tq, :], TMP[:, 0], TMP[:, 1], ALU.subtract)
                tt(TMP[:, 0], BB[:, 1], prb2, ALU.mult)
                tt(TMP[:, 1], BB[:, 0], pib2, ALU.mult)
                tt(v4(dst, 1)[:, :, tq, :], TMP[:, 0], TMP[:, 1], ALU.add)
        for g in range(32):
            for ri, MCt in enumerate((MCre, MCim)):
                S.op("dve", lambda v, g=g, ri=ri, MCt=MCt: v.tensor_scalar(
                    out=MCt[:, g, :], in0=MCf[:, ri, g // 2, :], scalar1=bd64f[:, 64 * (g % 2):64 * (g % 2) + 1],
                    scalar2=64.0, op0=ALU.mult, op1=ALU.mult), reads=[b_p, b_par], writes=[b_tab])
        TMPM = tb("TMPM", [128, 128])
        for g in range(32):
            gp, g2 = g // 2, g % 2
            lo, hi = 64 * g2, 64 * g2 + 64
            pst, psb = next_ps()
            S.group("pe", [
                lambda p, pst=pst, gp=gp, lo=lo, hi=hi: p.matmul(pst[:, 0:128], XTf[lo:hi, 0, gp, :], MCf[lo:hi, 0, gp, :],
                                                                 start=True, stop=False),
                lambda p, pst=pst, gp=gp, lo=lo, hi=hi: p.matmul(pst[:, 0:128], XTf[lo:hi, 1, gp, :], MCf[lo:hi, 1, gp, :],
                                                                 start=False, stop=True),
                lambda p, pst=pst, gp=gp, lo=lo, hi=hi: p.matmul(pst[:, 128:192], MBf[lo:hi, 0, gp, :], identf[lo:hi, lo:hi],
                                                                 start=True, stop=True),
                lambda p, pst=pst, gp=gp, lo=lo, hi=hi: p.matmul(pst[:, 192:256], MBf[lo:hi, 1, gp, :], identf[lo:hi, lo:hi],
                                                                 start=True, stop=True),
            ], reads=[b_p, b_par], writes=[psb])
            S.op("dve", lambda v, pst=pst: v.tensor_tensor(out=TMPM[:], in0=pst[:, 0:128], in1=maskLT, op=ALU.mult),
                 reads=[psb, b_par, b_p], writes=[b_p])
            S.op("dve", lambda v, g=g: v.scalar_tensor_tensor(out=T_all[:, g, :], in0=identf, scalar=drep[:, g:g + 1],
                                                             in1=TMPM[:], op0=ALU.mult, op1=ALU.add),
                 reads=[b_p, b_par], writes=[b_tab])
            S.op("act", lambda a, pst=pst, g=g: a.activation(out=MBre[:, g, :], in_=pst[:, 128:192], func=AF.Copy),
                 reads=[psb], writes=[b_tab])
            S.op("act", lambda a, pst=pst, g=g: a.activation(out=MBim[:, g, :], in_=pst[:, 192:256], func=AF.Copy),
                 reads=[psb], writes=[b_tab])
        M8I = T3
        actf(M8I, XR, AF.Exp, scale=-8.0)
        actf(T4, XR, AF.Exp, scale=8.0)
        P("dve", lambda v: v.tensor_copy(out=Rtab[:], in_=T4.rearrange("p (g o) -> p g o", o=1).to_broadcast([128, 16, NK])))
        P("dve", lambda v: v.memset(Rtab[:, :, 0:1], 0.0))
        tt(Ere[:, :, 0], Pre[:, 8, :], M8I, ALU.mult)
        tt(Eim[:, :, 0], Pim[:, 8, :], M8I, ALU.mult)
        ETr = tb("ETr", [128, 16, NK])
        ETi = tb("ETi", [128, 16, NK])
        m = 1
        while m < NK:
            ub_r = Ere[:, :, m - 1:m].to_broadcast([128, 16, m])
            ub_i = Eim[:, :, m - 1:m].to_broadcast([128, 16, m])
            tt(ETr[:, :, 0:m], Ere[:, :, 0:m], ub_r, ALU.mult)
            tt(ETi[:, :, 0:m], Eim[:, :, 0:m], ub_i, ALU.mult)
            tt(Ere[:, :, m:2 * m], ETr[:, :, 0:m], ETi[:, :, 0:m], ALU.subtract)
            tt(ETr[:, :, 0:m], Ere[:, :, 0:m], ub_i, ALU.mult)
            tt(ETi[:, :, 0:m], Eim[:, :, 0:m], ub_r, ALU.mult)
            tt(Eim[:, :, m:2 * m], ETr[:, :, 0:m], ETi[:, :, 0:m], ALU.add)
            m *= 2
        S.op("dve", lambda v: v.memset(TMPM[0:1, 0:1], 0.0), reads=[b_p], writes=[b_tab, b_p])
        S.barrier()

    xs = [sb("x_sb%d" % i, [128, 8, TT]) for i in range(2)]
    b_xs = [[Buf("x%d_%d" % (i, c)) for c in range(8)] for i in range(2)]
    sq_sb = [sb("sq%d" % i, [128, TT], BF16) for i in range(2)]
    b_sq = [Buf("sq%d" % i) for i in range(2)]
    hT = sb("hT", [128, 8, TT], BF16)
    b_hT = Buf("hT")
    h2T = sb("h2T", [128, 8, TT], BF16)
    b_h2T = Buf("h2T")
    ln_sb = [sb("ln%d" % i, [128, TT]) for i in range(2)]
    b_ln = [Buf("ln%d" % i) for i in range(2)]
    rstd = [sb("rstd%d" % i, [128, TT]) for i in range(2)]
    b_rstd = [Buf("rstd%d" % i) for i in range(2)]
    rs_ctr = [0]
    NSLAB = 3
    wslab = [sb("wslab%d" % i, [128, 8, 256], BF16) for i in range(NSLAB)]
    b_wslab = [Buf("wslab%d" % i) for i in range(NSLAB)]
    slab_ctr = [0]
    ms_all = sb("ms_all", [128, 8, TT], BF16)
    b_ms = Buf("ms_all")
    qT = sb("qT", [128, 4, TT], BF16)
    b_qT = Buf("qT")
    kT = [sb("kT%d" % i, [128, 4, TT], BF16) for i in range(NSLOT)]
    b_kT = [Buf("kT%d" % i) for i in range(NSLOT)]
    Vaug = [sb("Vaug%d" % i, [128, NB, NH, 65], BF16) for i in range(NSLOT)]
    b_V = [Buf("V%d" % i) for i in range(NSLOT)]
    for i in range(NSLOT):
        S.op("pool", lambda g, i=i: g.memset(Vaug[i][:], 1.0), writes=[b_V[i]])
    Utok = sb("Utok", [NK, 8, 512], BF16)
    b_Utok = Buf("Utok")
    Ush = sb("Ush", [128, 32, NK], BF16)
    b_Ush = Buf("Ush")
    gateT = sb("gateT", [128, 16, TT], BF16)
    b_gate = Buf("gateT")
    att_tmp = [sb("att_tmp%d" % i, [128, 512]) for i in range(2)]
    b_att_tmp = [Buf("att_tmp%d" % i) for i in range(2)]
    PT = [sb("PT%d" % i, [128, 512], BF16) for i in range(3)]
    b_PT = [Buf("PT%d" % i) for i in range(3)]
    att_ctr = [0, 0]
    rden = sb("rden", [128, 2, 4])
    b_rden = [Buf("rden0"), Buf("rden1")]
    yatt = sb("yatt", [128, NB, 512], BF16)
    b_yatt = [Buf("yatt%d" % i) for i in range(NB)]
    yaT = sb("yaT", [128, 4, TT], BF16)
    b_yaT = Buf("yaT")
    Sf = sb("Sf", [128, 2, 16, NK])
    b_Sf = Buf("Sf")
    Xf = sb("Xf", [128, 2, 16, NK])
    b_Xf = Buf("Xf")
    Zf, b_Zf = Sf, b_Sf
    Tf = sb("Tf", [128, 2, 16, NK])
    b_Tf = Buf("Tf")
    Hf = sb("Hf", [128, 2, 16, NK + 1])
    b_Hf = Buf("Hf")
    Hb = sb("Hb", [128, 2, 16, NK], BF16)
    b_Hb = Buf("Hb")
    cz = sb("cz", [128, 4, 16])
    zT = sb("zT", [128, 4, TT], BF16)
    b_zT = Buf("zT")
    sig = [sb("sig%d" % i, [128, TT], BF16) for i in range(2)]
    b_sig = [Buf("sig%d" % i) for i in range(2)]
    ysT = sb("ysT", [128, 4, TT], BF16)
    b_ysT = Buf("ysT")
    mT = sb("mT", [128, 8, TT], BF16)
    b_mT = Buf("mT")
    m12 = [sb("m12_%d" % i, [128, TT], BF16) for i in range(2)]
    b_m12 = [Buf("m12_%d" % i) for i in range(2)]
    Lr = sb("Lr", [128, NB, 20])
    rt = sb("rt", [128, 12, NB, 4])
    rt16 = sb("rt16", [128, NB, 16])
    gates = sb("gates", [128, NB, 16])
    b_rt = Buf("router")
    gatesT = sb("gatesT", [16, TT])
    b_gatesT = Buf("gatesT")
    gm = [sb("gm%d" % i, [16, TT], BF16) for i in range(3)]
    b_gm = [Buf("gm%d" % i) for i in range(3)]
    if stage < 7:
        dbgtmp = sb("dbgtmp", [128, 8 * TT])
        b_dbgtmp = Buf("dbgtmp")
    hid = sb("hid", [128, 8, TT] if stage >= 7 else [128, 2, 2], BF16)
    b_hid = [Buf("hid%d" % i) for i in range(8)]
    NGU = 4
    wgu = [sb("wgu%d" % i, [128, 2, 8, 128], BF16) for i in range(NGU)]
    b_wgu = [Buf("wgu%d" % i) for i in range(NGU)]
    NWD = 4
    wd = [sb("wd%d" % i, [128, 8, 128], BF16) for i in range(NWD)]
    b_wd = [Buf("wd%d" % i) for i in range(NWD)]
    sil = [sb("sil%d" % i, [128, TT], BF16) for i in range(2)]
    b_sil = [Buf("sil%d" % i) for i in range(2)]
    gsb = [sb("gs%d" % i, [128, TT], BF16) for i in range(2)]
    gbs = [sb("gbs%d" % i, [128, TT], BF16) for i in range(2)]
    b_gbs = [Buf("gbs%d" % i) for i in range(2)]
    s2b = [sb("s2_%d" % i, [128, TT], BF16) for i in range(2)]
    b_s2 = [Buf("s2_%d" % i) for i in range(2)]
    b_gs = [Buf("gs%d" % i) for i in range(2)]
    moe_ctr = [0, 0]

    b_s_in = [Buf("s_in%d" % j) for j in range(16)]
    b_s_br = [Buf("s_br%d" % j) for j in range(4)]
    b_s_out = [Buf("s_out%d" % j) for j in range(4)]
    b_s_gu = [[Buf("s_gu%d_%d" % (e, fh)) for fh in range(2)] for e in range(NE)]
    b_s_wd = [[Buf("s_wd%d_%d" % (hf, fc)) for fc in range(8)] for hf in range(4)]
    wed_v = wed_d.rearrange("e (fh p) (fc j) -> fc p (e fh) j", fh=2, j=128)

    class PsPool:
        def __init__(self, idxs):
            self.idxs, self.ctr, self.held = list(idxs), 0, set()

        def get(self, hold=False):
            while True:
                i = self.idxs[self.ctr % len(self.idxs)]
                self.ctr += 1
                if i not in self.held:
                    break
            if hold:
                self.held.add(i)
            return PS[i], b_PS[i], i

        def release(self, i):
            self.held.discard(i)

    MP = PsPool(range(0, 4))
    EP = PsPool(range(4, 8))

    def load_slab(t, which, j):
        src_f, src_s, bs = ((w_in_d, w_in_s, b_s_in), (w_br_d, wbr_s, b_s_br), (w_out_d, wout_s, b_s_out))[which]
        i = slab_ctr[0] % NSLAB
        slab_ctr[0] += 1
        t_, b_ = wslab[i], b_wslab[i]
        col0 = 256 * j
        if t == 0:
            S.dma("pool", lambda q: q.dma_start(
                out=t_[:], in_=src_f[:, col0:col0 + 256].rearrange("(c p) f -> p c f", p=128)), b_, writes=[b_])
            S.dma("sp", lambda q: q.dma_start(
                out=src_s[:, col0:col0 + 256].rearrange("(c p) f -> p c f", p=128), in_=t_[:]),
                b_, reads=[b_], writes=[bs[j]])
        else:
            S.dma("sp", lambda q: q.dma_start(
                out=t_[:], in_=src_s[:, col0:col0 + 256].rearrange("(c p) f -> p c f", p=128)),
                b_, reads=[bs[j]], writes=[b_])
        return t_, b_

    def load_wgu(t, e, fh):
        iw = moe_ctr[0] % NGU
        moe_ctr[0] += 1
        t_, b_ = wgu[iw], b_wgu[iw]
        if t == 0:
            S.dma("pool", lambda q: q.dma_start(
                out=t_[:, 0], in_=weg_d[e][:, fh * 128:(fh + 1) * 128].rearrange("(c p) f -> p c f", p=128)), b_, writes=[b_])
            S.dma("pool", lambda q: q.dma_start(
                out=t_[:, 1], in_=weu_d[e][:, fh * 128:(fh + 1) * 128].rearrange("(c p) f -> p c f", p=128)), b_, writes=[b_])
            S.dma("sp", lambda q: q.dma_start(out=wgu_s[e, fh], in_=t_[:]), b_, reads=[b_], writes=[b_s_gu[e][fh]])
        else:
            S.dma("pool", lambda q: q.dma_start(out=t_[:], in_=wgu_s[e, fh]), b_, reads=[b_s_gu[e][fh]], writes=[b_])
        return t_, b_

    wd_live = {}
    wgu_live = {}

    def wgu_prefetch(t, idx):
        if idx < 32:
            wgu_live[(t, idx)] = load_wgu(t, idx // 2, idx % 2)

    def wd_prefetch(t, idx):
        if idx < 32:
            wd_live[(t, idx)] = load_wd(t, idx // 8, idx % 8)

    def load_wd(t, hf, fc):
        iw = moe_ctr[1] % NWD
        moe_ctr[1] += 1
        t_, b_ = wd[iw], b_wd[iw]
        if t == 0:
            S.dma("pool", lambda q: q.dma_start(out=t_[:], in_=wed_v[fc][:, 8 * hf:8 * hf + 8, :]), b_, writes=[b_])
            S.dma("sp", lambda q: q.dma_start(out=wd_s[hf, fc], in_=t_[:]), b_, reads=[b_], writes=[b_s_wd[hf][fc]])
        else:
            S.dma("pool", lambda q: q.dma_start(out=t_[:], in_=wd_s[hf, fc]), b_, reads=[b_s_wd[hf][fc]], writes=[b_])
        return t_, b_

    def dump(name, src, bufs, dst_ap):
        if name not in dbg_d:
            return
        shape = [int(s_) for s_ in src.shape]
        n = 1
        for s_ in shape[1:]:
            n *= s_
        tv = dbgtmp[0:shape[0], 0:n]
        if len(shape) == 3:
            tv = tv.rearrange("p (a b) -> p a b", a=shape[1])
        bt = b_dbgtmp
        S.op("dve", lambda v: v.tensor_copy(out=tv, in_=src), reads=bufs, writes=[bt])
        S.dma("sp", lambda q: q.dma_start(out=dst_ap, in_=tv), bt, reads=[bt])
        if bt not in dbg_final:
            dbg_final.append(bt)

    dbg_final = []

    def dslice(name, nrows, tok0):
        return dbg_d[name][0:nrows, tok0:tok0 + TT].rearrange("(c p) t -> p c t", p=128) if name in dbg_d else None

    def rs_from_ps(pst, psb, scale):
        i = rs_ctr[0] % 2
        rs_ctr[0] += 1
        S.op("act", lambda a: a.activation(out=ln_sb[i][:], in_=pst[:, :TT], func=AF.Ln, scale=scale, bias=EPS),
             reads=[psb], writes=[b_ln[i]])
        S.op("act", lambda a: a.activation(out=rstd[i][:], in_=ln_sb[i][:], func=AF.Exp, scale=-0.5),
             reads=[b_ln[i]], writes=[b_rstd[i]])
        return rstd[i], b_rstd[i]

    def rmsnorm(pool, x_sb, b_x, gain_sb, out_t, out_b):
        pst, psb, _ = pool.get()
        S.op("act", lambda a: a.activation(out=out_t[:], in_=x_sb[:], func=AF.Square), reads=list(b_x), writes=[out_b])
        S.group("pe", [lambda p, c=c: p.matmul(pst[:, :TT], ones_bf[:], out_t[:, c, :], start=(c == 0), stop=(c == 7))
                       for c in range(8)], reads=[out_b, b_par], writes=[psb])
        r_t, r_b = rs_from_ps(pst, psb, 1.0 / D)
        for c in range(8):
            S.op("dve", lambda v: v.scalar_tensor_tensor(
                out=out_t[:, c, :], in0=x_sb[:, c, :], scalar=gain_sb[:, c:c + 1], in1=r_t[:],
                op0=ALU.mult, op1=ALU.mult), reads=[b_x[c], r_b, b_par], writes=[out_b])
        yield 1.3
        yield 6.0

    def proj_fm(slab, bslab, col, rhs_t, rhs_b, nchunk=8, hold=False):
        pst, psb, pi_ = MP.get(hold=hold)
        S.group("pe", [lambda p, c=c: p.matmul(pst[:, :TT], slab[:, c, col:col + 128], rhs_t[:, c, :],
                                               start=(c == 0), stop=(c == nchunk - 1)) for c in range(nchunk)],
                reads=[bslab, rhs_b], writes=[psb])
        return pst, psb, pi_

    def mixer(t):
        tok0 = t * TT
        seq_t = t % TPS
        slot = t % NSLOT
        x_sb, b_x = xs[t % 2], b_xs[t % 2]
        for c in range(8):
            S.dma("sp", lambda q: q.dma_start(out=x_sb[:, c, :], in_=xT_d[c * 128:(c + 1) * 128, tok0:tok0 + TT]),
                  b_x[c], writes=[b_x[c]])
        yield 0.0
        if stage < 1:
            return
        yield from rmsnorm(MP, x_sb, b_x, g1_sb, hT, b_hT)
        dump("h", hT[:], [b_hT], dslice("h", 1024, tok0))

        pend = []

        def qk_finish(i, sq, bq):
            pm, pmb, _ = MP.get()
            S.group("pe", [lambda p: p.matmul(pm[:, :TT], bd64[:], sq[:], start=True, stop=True)],
                    reads=[bq, b_par], writes=[pmb])
            S.op("dve", lambda v: v.tensor_copy(out=ms_all[:, i, :], in_=pm[:, :TT]), reads=[pmb], writes=[b_ms])

        def qk_norm():
            S.op("act", lambda a: a.activation(out=ms_all[:], in_=ms_all[:], func=AF.Ln, bias=EPS), reads=[b_ms], writes=[b_ms])
            S.op("act", lambda a: a.activation(out=ms_all[:], in_=ms_all[:], func=AF.Exp, scale=-0.5), reads=[b_ms], writes=[b_ms])
            S.op("dve", lambda v: v.scalar_tensor_tensor(out=qT[:], in0=qT[:], scalar=cq[:, 0:1], in1=ms_all[:, 0:4, :],
                                                         op0=ALU.mult, op1=ALU.mult), reads=[b_qT, b_ms, b_par], writes=[b_qT])
            S.op("dve", lambda v: v.tensor_tensor(out=kT[slot][:], in0=kT[slot][:], in1=ms_all[:, 4:8, :], op=ALU.mult),
                 reads=[b_kT[slot], b_ms], writes=[b_kT[slot]])
        for which in range(2):
            for half in range(2):
                slab, bslab = load_slab(t, 0, 2 * which + half)
                for m in range(2 * half, 2 * half + 2):
                    pq, pqb, pqi = proj_fm(slab, bslab, (m % 2) * 128, hT, b_hT)
                    sq, bq = sq_sb[m % 2], b_sq[m % 2]
                    S.op("act", lambda a: a.activation(out=sq[:], in_=pq[:, :TT], func=AF.Square),
                         reads=[pqb], writes=[bq])
                    if which == 0:
                        S.op("act", lambda a: a.activation(out=qT[:, m, :], in_=pq[:, :TT], func=AF.Copy),
                             reads=[pqb], writes=[b_qT])
                    else:
                        S.op("act", lambda a: a.activation(out=kT[slot][:, m, :], in_=pq[:, :TT], func=AF.Copy),
                             reads=[pqb], writes=[b_kT[slot]])
                    yield 1.2
                    if pend:
                        qk_finish(*pend.pop())
                    pend.append((4 * which + m, sq, bq))
        if stage < 2:
            qk_finish(*pend.pop())
            qk_norm()
            return
        for half in range(2):
            slab, bslab = load_slab(t, 0, 4 + half)
            for blk in range(NB):
                pv, pvb, _ = MP.get()
                S.group("pe", [lambda p, c=c: p.matmul(
                    pv[:, 0:256], hT[:, c, blk * 128:(blk + 1) * 128], slab[:, c, :],
                    start=(c == 0), stop=(c == 7)) for c in range(8)],
                    reads=[bslab, b_hT], writes=[pvb])
                S.op("act", lambda a: a.activation(
                    out=Vaug[slot][:, blk, 4 * half:4 * half + 4, 0:64],
                    in_=pv[:, 0:256].rearrange("p (h d) -> p h d", h=4), func=AF.Copy),
                    reads=[pvb], writes=[b_V[slot]])
                yield 1.2
                if pend:
                    qk_finish(*pend.pop())
                    qk_norm()
        dump("q", qT[:], [b_qT], dslice("q", 512, tok0))
        dump("k", kT[slot][:], [b_kT[slot]], dslice("k", 512, tok0))

        hT_kt = hT[:].rearrange("p c (k t) -> p c t k", t=8)
        Utok_g = Utok[:].rearrange("k t c -> k (t c)").rearrange("k (g t c) -> k g t c", g=32, t=8)
        for half in range(2):
            slab, bslab = load_slab(t, 0, 6 + half)
            for tau in range(8):
                pu, pub, _ = MP.get()
                S.group("pe", [lambda p, c=c: p.matmul(
                    pu[0:NK, 0:256], hT_kt[:, c, tau, :], slab[:, c, :],
                    start=(c == 0), stop=(c == 7)) for c in range(8)],
                    reads=[bslab, b_hT], writes=[pub])
                S.op("act", lambda a: a.activation(
                    out=Utok_g[:, 16 * half:16 * half + 16, tau, :],
                    in_=pu[0:NK, 0:256].rearrange("k (g c) -> k g c", c=16), func=AF.Copy),
                    reads=[pub], writes=[b_Utok])
                yield 1.2

        if stage >= 4:
            pt, ptb, _ = MP.get()
            ptv = pt[:].bitcast(BF16).rearrange("p (g k) -> p g k", k=NK)
            S.group("pe", [lambda p, g=g: p.transpose(ptv[:, g, :], Utok_g[:, g].rearrange("k t c -> k (t c)"),
                                                      identb[0:NK, 0:NK]) for g in range(32)],
                    reads=[b_Utok, b_par], writes=[ptb])
            S.op("act", lambda a: a.activation(out=Ush[:], in_=ptv[:, 0:32, :], func=AF.Copy),
                 reads=[ptb], writes=[b_Ush])
            yield 2.0
            for ri, MB in enumerate((MBre, MBim)):
                pss, pssb, _ = MP.get()
                psv = pss[:, 0:16 * NK].rearrange("p (g k) -> p g k", k=NK)
                S.group("pe", [lambda p, g=g: p.matmul(
                    psv[64 * (g % 2):64 * (g % 2) + 64, g // 2, :], MB[:, g, :], Ush[:, g, :], start=True, stop=True)
                    for g in range(32)], reads=[b_Ush, b_tab], writes=[pssb])
                S.op("act", lambda a: a.activation(out=Sf[:, ri], in_=psv, func=AF.Copy),
                     reads=[pssb], writes=[b_Sf])
                yield 2.0
            if seq_t == 0:
                S.op("dve", lambda v: v.memset(Hf[:, :, :, 0:1], 0.0), writes=[b_Hf])
            else:
                S.op("dve", lambda v: v.tensor_copy(out=Hf[:, :, :, 0:1], in_=Hf[:, :, :, NK:NK + 1]),
                     reads=[b_Hf], writes=[b_Hf])
            c_re, c_im = Hf[:, 0, :, 0], Hf[:, 1, :, 0]

            def dv(fn, reads, writes):
                S.op("dve", fn, reads=reads, writes=writes)
            rw = [b_Hf, b_Sf, b_tab]
            dv(lambda v: v.tensor_tensor(out=cz[:, 0], in0=A8[:, 0], in1=c_re, op=ALU.mult), rw, [b_Sf])
            dv(lambda v: v.tensor_tensor(out=cz[:, 1], in0=A8[:, 1], in1=c_im, op=ALU.mult), rw, [b_Sf])
            dv(lambda v: v.tensor_tensor(out=cz[:, 2], in0=A8[:, 0], in1=c_im, op=ALU.mult), rw, [b_Sf])
            dv(lambda v: v.tensor_tensor(out=cz[:, 3], in0=A8[:, 1], in1=c_re, op=ALU.mult), rw, [b_Sf])
            dv(lambda v: v.tensor_tensor(out=cz[:, 0], in0=cz[:, 0], in1=cz[:, 1], op=ALU.subtract), rw, [b_Sf])
            dv(lambda v: v.tensor_tensor(out=cz[:, 2], in0=cz[:, 2], in1=cz[:, 3], op=ALU.add), rw, [b_Sf])
            dv(lambda v: v.tensor_tensor(out=Sf[:, 0, :, 0], in0=Sf[:, 0, :, 0], in1=cz[:, 0], op=ALU.add), rw, [b_Sf])
            dv(lambda v: v.tensor_tensor(out=Sf[:, 1, :, 0], in0=Sf[:, 1, :, 0], in1=cz[:, 2], op=ALU.add), rw, [b_Sf])
            yield 1.0
            dv(lambda v: v.tensor_tensor(out=Tf[:, 0], in0=Sf[:, 0], in1=Ere[:], op=ALU.mult), [b_Sf, b_tab], [b_Tf])
            dv(lambda v: v.tensor_tensor(out=Tf[:, 1], in0=Sf[:, 1], in1=Eim[:], op=ALU.mult), [b_Sf, b_tab], [b_Tf])
            dv(lambda v: v.tensor_tensor(out=Xf[:, 0], in0=Tf[:, 0], in1=Tf[:, 1], op=ALU.add), [b_Tf], [b_Xf])
            yield 1.0
            dv(lambda v: v.tensor_tensor(out=Tf[:, 0], in0=Sf[:, 1], in1=Ere[:], op=ALU.mult), [b_Sf, b_tab, b_Xf], [b_Tf])
            dv(lambda v: v.tensor_tensor(out=Tf[:, 1], in0=Sf[:, 0], in1=Eim[:], op=ALU.mult), [b_Sf, b_tab], [b_Tf])
            dv(lambda v: v.tensor_tensor(out=Xf[:, 1], in0=Tf[:, 0], in1=Tf[:, 1], op=ALU.subtract), [b_Tf], [b_Xf])
            yield 1.0
            Rflat = Rtab[:].rearrange("p g k -> p (g k)")
            for ri in range(2):
                dv(lambda v: v.tensor_tensor_scan(
                    out=Zf[:, ri].rearrange("p g k -> p (g k)"), data0=Rflat,
                    data1=Xf[:, ri].rearrange("p g k -> p (g k)"), initial=0.0, op0=ALU.mult, op1=ALU.add),
                    [b_Xf, b_tab], [b_Zf])
                yield 1.0
            dv(lambda v: v.tensor_tensor(out=Tf[:, 0], in0=Zf[:, 0], in1=Ere[:], op=ALU.mult), [b_Zf, b_tab], [b_Tf])
            dv(lambda v: v.tensor_tensor(out=Tf[:, 1], in0=Zf[:, 1], in1=Eim[:], op=ALU.mult), [b_Zf, b_tab], [b_Tf])
            dv(lambda v: v.tensor_tensor(out=Hf[:, 0, :, 1:NK + 1], in0=Tf[:, 0], in1=Tf[:, 1], op=ALU.subtract), [b_Tf], [b_Hf])
            yield 1.0
            dv(lambda v: v.tensor_tensor(out=Tf[:, 0], in0=Zf[:, 0], in1=Eim[:], op=ALU.mult), [b_Zf, b_tab, b_Hf], [b_Tf])
            dv(lambda v: v.tensor_tensor(out=Tf[:, 1], in0=Zf[:, 1], in1=Ere[:], op=ALU.mult), [b_Zf, b_tab], [b_Tf])
            dv(lambda v: v.tensor_tensor(out=Hf[:, 1, :, 1:NK + 1], in0=Tf[:, 0], in1=Tf[:, 1], op=ALU.add), [b_Tf], [b_Hf])
            yield 1.0
            S.op("dve", lambda v: v.tensor_copy(out=Hb[:], in_=Hf[:, :, :, 0:NK]), reads=[b_Hf], writes=[b_Hb])
            yield 1.0

        for j in range(8):
            slab, bslab = load_slab(t, 0, 8 + j)
            for m in range(2):
                ci = j * 2 + m
                pg, pgb, _ = proj_fm(slab, bslab, m * 128, hT, b_hT)
                S.op("act", lambda a: a.activation(
                    out=gateT[:, ci, :], in_=pg[:, :TT], func=AF.Tanh, bias=hbias[:, ci:ci + 1], scale=0.5),
                    reads=[pgb, b_par], writes=[b_gate])
                yield 1.2
        if stage < 3:
            return

        for blk in range(NB):
            m_abs = seq_t * NB + blk
            kbs = [kb for kb in range(5) if m_abs - 4 + kb >= 0]
            for hg in range(2):
                po, pob, poi = MP.get(hold=True)
                po4 = po[:, 0:260].rearrange("p (h d) -> p h d", h=4)
                pend = []
                first = [True]

                def pv_step(ip, ks, kblk):
                    fns = []
                    for j in range(4):
                        h = 2 * j + hg
                        fns.append(lambda p, j=j, h=h, st=(first[0] and j == 0): p.matmul(
                            po[:, j * 65:(j + 1) * 65], PT[ip][:, j * 128:(j + 1) * 128],
                            Vaug[ks][:, kblk, h, :], start=st, stop=True, skip_group_check=True))
                    S.group("pe", fns, reads=[b_PT[ip], b_V[ks]], writes=[pob])
                    first[0] = False
                for kb in kbs:
                    ab = m_abs - 4 + kb
                    kt_ = (t - seq_t) + ab // NB
                    ks = kt_ % NSLOT
                    kblk = ab % NB
                    pst, psb, _ = MP.get()
                    fns = []
                    for j in range(4):
                        h = 2 * j + hg
                        mchunk, hb = h // 2, 64 * (h % 2)
                        fns.append(lambda p, j=j, mchunk=mchunk, hb=hb: p.matmul(
                            pst[:, j * 128:(j + 1) * 128],
                            kT[ks][hb:hb + 64, mchunk, kblk * 128:(kblk + 1) * 128],
                            qT[hb:hb + 64, mchunk, blk * 128:(blk + 1) * 128], start=True, stop=True))
                    S.group("pe", fns, reads=[b_kT[ks], b_qT], writes=[psb])
                    ia = att_ctr[0] % 2
                    att_ctr[0] += 1
                    S.op("dve", lambda v: v.tensor_tensor(
                        out=att_tmp[ia][:].rearrange("p (h q) -> p h q", h=4),
                        in0=pst[:, :].rearrange("p (h q) -> p h q", h=4),
                        in1=biasT4[:, kb, :, :].rearrange("p (j two) q -> p j two q", two=2)[:, :, hg, :], op=ALU.add),
                        reads=[psb, b_bias], writes=[b_att_tmp[ia]])
                    ip = att_ctr[1] % 3
                    att_ctr[1] += 1
                    S.op("act", lambda a: a.activation(out=PT[ip][:], in_=att_tmp[ia][:], func=AF.Exp),
                         reads=[b_att_tmp[ia]], writes=[b_PT[ip]])
                    yield 0.4
                    if len(pend) >= 2:
                        pv_step(*pend.pop(0))
                    pend.append((ip, ks, kblk))
                while pend:
                    pv_step(*pend.pop(0))
                    yield 0.3
                S.op("dve", lambda v: v.reciprocal(out=rden[:, hg, :], in_=po4[:, :, 64]),
                     reads=[pob], writes=[b_rden[hg]])
                S.op("dve", lambda v: v.tensor_tensor(
                    out=yatt[:, blk, :].rearrange("p (j two d) -> p j two d", two=2, d=64)[:, :, hg, :],
                    in0=po4[:, :, 0:64],
                    in1=rden[:, hg, :].rearrange("p (h o) -> p h o", o=1).to_broadcast([128, 4, 64]),
                    op=ALU.mult), reads=[pob, b_rden[hg]], writes=[b_yatt[blk]])
                MP.release(poi)
            pt, ptb, _ = MP.get()
            ptv = pt[:].bitcast(BF16)
            S.group("pe", [lambda p, c=c: p.transpose(
                ptv[:, c * 128:(c + 1) * 128], yatt[:, blk, c * 128:(c + 1) * 128], identb[:]) for c in range(4)],
                reads=[b_yatt[blk], b_par], writes=[ptb])
            S.op("act", lambda a: a.activation(
                out=yaT[:, :, blk * 128:(blk + 1) * 128], in_=ptv[:, 0:512].rearrange("p (c t) -> p c t", c=4),
                func=AF.Copy), reads=[ptb], writes=[b_yaT])
            yield 0.3
        dump("ya", yaT[:], [b_yaT], dslice("ya", 512, tok0))
        if stage < 4:
            return

        for g0 in range(0, 32, 4):
            py, pyb, _ = MP.get()
            fns = []
            for gi in range(4):
                g = g0 + gi
                gp = g // 2
                o = py[0:NK, gi * 128:(gi + 1) * 128]
                fns.append(lambda p, o=o, g=g, st=(gi == 0): p.matmul(o, Ush[:, g, :], T_all[:, g, :], start=st, stop=False,
                                                                      skip_group_check=True))
                fns.append(lambda p, o=o, gp=gp, g=g: p.matmul(o, Hb[:, 0, gp, :], MCre[:, g, :],
                                                              start=False, stop=False, skip_group_check=True))
                fns.append(lambda p, o=o, gp=gp, g=g: p.matmul(o, Hb[:, 1, gp, :], MCim[:, g, :],
                                                              start=False, stop=True, skip_group_check=True))
            S.group("pe", fns, reads=[b_Ush, b_Hb, b_tab], writes=[pyb])
            S.op("act", lambda a: a.activation(
                out=Utok[:, :, g0 * 16:(g0 + 4) * 16].rearrange("k t (g c) -> k t g c", g=4),
                in_=py[0:NK, :].rearrange("k (g t c) -> k t g c", g=4, t=8), func=AF.Gelu_apprx_tanh),
                reads=[pyb, b_Ush], writes=[b_Utok])
            yield 0.8
        pt, ptb, _ = MP.get()
        ptz = pt[:].bitcast(BF16)[:, 0:4 * TT].rearrange("p (c t k) -> p c t k", c=4, t=8)
        S.group("pe", [lambda p, cc=cc, tau=tau: p.transpose(
            ptz[:, cc, tau, :], Utok[:, tau, cc * 128:(cc + 1) * 128], identb[0:NK, 0:NK])
            for cc in range(4) for tau in range(8)], reads=[b_Utok, b_par], writes=[ptb])
        S.op("act", lambda a: a.activation(out=zT[:].rearrange("p c (k t) -> p c t k", t=8), in_=ptz, func=AF.Copy, scale=0.5),
             reads=[ptb], writes=[b_zT])
        yield 2.0
        dump("z", zT[:], [b_zT], dslice("z", 512, tok0))
        for m in range(4):
            pg, pgb, _ = MP.get()
            S.group("pe", [lambda p, c=c: p.matmul(pg[:, :TT], wglu[:, c, m * 128:(m + 1) * 128], zT[:, c, :],
                                                   start=(c == 0), stop=(c == 3)) for c in range(4)],
                    reads=[b_wres, b_zT], writes=[pgb])
            S.op("act", lambda a: a.activation(out=sig[m % 2][:], in_=pg[:, :TT], func=AF.Tanh,
                                               bias=hbias[:, 16 + m:17 + m], scale=1.0),
                 reads=[pgb, b_par], writes=[b_sig[m % 2]])
            S.op("dve", lambda v: v.scalar_tensor_tensor(out=ysT[:, m, :], in0=sig[m % 2][:], scalar=1.0, in1=zT[:, m, :],
                                                         op0=ALU.add, op1=ALU.mult),
                 reads=[b_zT, b_sig[m % 2]], writes=[b_ysT])
            yield 0.6
        dump("ys", ysT[:], [b_ysT], dslice("ys", 512, tok0))
        if stage < 5:
            return

        for fc in range(8):
            if fc % 2 == 0:
                wbr, b_wbr = load_slab(t, 1, fc // 2)
            fo = (fc % 2) * 128
            pa, pab, _ = MP.get()
            S.group("pe", [lambda p, c=c: p.matmul(pa[:, :TT], wbr[:, c, fo:fo + 128], yaT[:, c, :],
                                                   start=(c == 0), stop=(c == 3)) for c in range(4)],
                    reads=[b_wbr, b_yaT], writes=[pab])
            pb, pbb, _ = MP.get()
            S.group("pe", [lambda p, c=c: p.matmul(pb[:, :TT], wbr[:, 4 + c, fo:fo + 128], ysT[:, c, :],
                                                   start=(c == 0), stop=(c == 3)) for c in range(4)],
                    reads=[b_wbr, b_ysT], writes=[pbb])
            S.op("dve", lambda v: v.scalar_tensor_tensor(out=m12[0][:], in0=gateT[:, fc, :], scalar=1.0, in1=pa[:, :TT],
                                                         op0=ALU.add, op1=ALU.mult),
                 reads=[pab, b_gate], writes=[b_m12[0]])
            S.op("dve", lambda v: v.scalar_tensor_tensor(out=m12[1][:], in0=gateT[:, 8 + fc, :], scalar=1.0, in1=pb[:, :TT],
                                                         op0=ALU.add, op1=ALU.mult),
                 reads=[pbb, b_gate], writes=[b_m12[1]])
            S.op("dve", lambda g_: g_.tensor_tensor(out=mT[:, fc, :], in0=m12[0][:], in1=m12[1][:], op=ALU.add),
                 reads=[b_m12[0], b_m12[1]], writes=[b_mT])
            yield 1.2
        for fc in range(8):
            if fc % 2 == 0:
                wout, b_wout = load_slab(t, 2, fc // 2)
            fo = (fc % 2) * 128
            po, pob, _ = MP.get()
            S.group("pe", [lambda p, c=c: p.matmul(po[:, :TT], wout[:, c, fo:fo + 128], mT[:, c, :],
                                                   start=(c == 0), stop=(c == 7)) for c in range(8)],
                    reads=[b_wout, b_mT], writes=[pob])
            S.op("dve", lambda v: v.scalar_tensor_tensor(out=x_sb[:, fc, :], in0=po[:, :TT], scalar=0.5, in1=x_sb[:, fc, :],
                                                         op0=ALU.mult, op1=ALU.add),
                 reads=[pob, b_x[fc]], writes=[b_x[fc]])
            yield 1.2
        dump("x1", x_sb[:], b_x, dslice("x1", 1024, tok0))

    def store_x(t):
        tok0 = t * TT
        x_sb, b_x = xs[t % 2], b_xs[t % 2]
        for c in range(8):
            S.dma("sp", lambda q: q.dma_start(out=outT_d[c * 128:(c + 1) * 128, tok0:tok0 + TT], in_=x_sb[:, c, :]),
                  b_x[c], reads=[b_x[c]])

    def moe_pre(t):
        tok0 = t * TT
        x_sb, b_x = xs[t % 2], b_xs[t % 2]
        if stage < 6:
            return
        yield from rmsnorm(MP, x_sb, b_x, g2_sb, h2T, b_h2T)
        pr_, prb, _ = MP.get()
        prv = pr_[:, 0:NB * 20].rearrange("p (b j) -> p b j", j=20)
        for blk in range(NB):
            S.group("pe", [lambda p, c=c: p.matmul(prv[:, blk, :], h2T[:, c, blk * 128:(blk + 1) * 128], wr[:, c, :],
                                                   start=(c == 0), stop=(c == 7)) for c in range(8)],
                    reads=[b_h2T, b_wres], writes=[prb])
        R = [b_rt, b_par]

        def rv(fn, reads=R, writes=(b_rt,)):
            S.op("dve", fn, reads=list(reads), writes=list(writes))
        rv(lambda v: v.tensor_tensor(out=Lr[:], in0=prv, in1=rbias_sb.rearrange("p (o j) -> p o j", o=1).to_broadcast([128, NB, 20]),
                                     op=ALU.add), reads=[prb, b_par, b_rt])
        G = Lr[:, :, 0:4]
        E4 = Lr[:, :, 4:20].rearrange("p b (g j) -> p b g j", g=4)
        gmax, gsum, gprob, m1, m2, dd, w1, w2 = [rt[:, i, :, 0:1] for i in range(8)]
        gmask, ge, ing, x2 = [rt[:, 8 + i] for i in range(4)]
        mask1 = rt16[:, :, 0:4]
        mask2 = rt16[:, :, 4:8]
        wj = rt16[:, :, 8:12]
        tj = rt16[:, :, 12:16]

        def b4(x):
            return x.to_broadcast([128, NB, 4])
        rv(lambda v: v.tensor_reduce(out=gmax, in_=G, axis=AX.X, op=ALU.max))
        rv(lambda v: v.tensor_tensor(out=ge, in0=G, in1=b4(gmax), op=ALU.subtract))
        S.op("act", lambda a: a.activation(out=ge, in_=ge, func=AF.Exp), reads=R, writes=[b_rt])
        rv(lambda v: v.tensor_reduce(out=gsum, in_=ge, axis=AX.X, op=ALU.add))
        rv(lambda v: v.reciprocal(out=gprob, in_=gsum))
        rv(lambda v: v.tensor_tensor(out=gmask, in0=G, in1=b4(gmax), op=ALU.is_equal))
        sel4 = gates[:].rearrange("p b (g j) -> p b g j", g=4)
        rv(lambda v: v.tensor_tensor(out=sel4, in0=E4, in1=gmask.rearrange("p b (g o) -> p b g o", o=1).to_broadcast([128, NB, 4, 4]),
                                     op=ALU.mult))
        rv(lambda v: v.tensor_reduce(out=ing, in_=sel4.rearrange("p b g j -> p b j g"), axis=AX.X, op=ALU.add))
        rv(lambda v: v.tensor_reduce(out=m1, in_=ing, axis=AX.X, op=ALU.max))
        rv(lambda v: v.tensor_tensor(out=mask1, in0=ing, in1=b4(m1), op=ALU.is_equal))
        rv(lambda v: v.scalar_tensor_tensor(out=x2, in0=mask1, scalar=-1e30, in1=ing, op0=ALU.mult, op1=ALU.add))
        rv(lambda v: v.tensor_reduce(out=m2, in_=x2, axis=AX.X, op=ALU.max))
        rv(lambda v: v.tensor_tensor(out=mask2, in0=x2, in1=b4(m2), op=ALU.is_equal))
        rv(lambda v: v.tensor_tensor(out=dd, in0=m2, in1=m1, op=ALU.subtract))
        S.op("act", lambda a: a.activation(out=dd, in_=dd, func=AF.Exp), reads=R, writes=[b_rt])
        rv(lambda v: v.tensor_scalar(out=w1, in0=dd, scalar1=1.0, scalar2=None, op0=ALU.add))
        rv(lambda v: v.reciprocal(out=w1, in_=w1))
        rv(lambda v: v.tensor_tensor(out=w2, in0=dd, in1=w1, op=ALU.mult))
        rv(lambda v: v.tensor_tensor(out=w1, in0=w1, in1=gprob, op=ALU.mult))
        rv(lambda v: v.tensor_tensor(out=w2, in0=w2, in1=gprob, op=ALU.mult))
        rv(lambda v: v.tensor_tensor(out=wj, in0=mask1, in1=b4(w1), op=ALU.mult))
        rv(lambda v: v.tensor_tensor(out=tj, in0=mask2, in1=b4(w2), op=ALU.mult))
        rv(lambda v: v.tensor_tensor(out=wj, in0=wj, in1=tj, op=ALU.add))
        rv(lambda v: v.tensor_tensor(out=sel4, in0=gmask.rearrange("p b (g o) -> p b g o", o=1).to_broadcast([128, NB, 4, 4]),
                                     in1=wj.rearrange("p b (o j) -> p b o j", o=1).to_broadcast([128, NB, 4, 4]), op=ALU.mult))
        yield 6.0
        pgt, pgtb, _ = MP.get()
        S.group("pe", [lambda p, blk=blk: p.transpose(pgt[0:16, blk * 128:(blk + 1) * 128], gates[:, blk, :], identf)
                       for blk in range(NB)], reads=[b_rt, b_par], writes=[pgtb])
        S.op("act", lambda a: a.activation(out=gatesT[:], in_=pgt[0:16, 0:TT], func=AF.Copy), reads=[pgtb], writes=[b_gatesT])
        if "gates" in dbg_d:
            dump("gates", gatesT[:], [b_gatesT], dbg_d["gates"][0:16, tok0:tok0 + TT])
        yield 1.0

    def moe(t):
        tok0 = t * TT
        x_sb, b_x = xs[t % 2], b_xs[t % 2]
        if stage < 7:
            store_x(t)
            return

        for i_ in range(NWD):
            wd_prefetch(t, i_)
        for i_ in range(NGU):
            wgu_prefetch(t, i_)

        def emit_gm(e):
            if e < NE:
                S.op("dve", lambda v: v.tensor_scalar(out=gm[e % 3][:], in0=gatesT[:], scalar1=identf[0:16, e:e + 1],
                                                      scalar2=0.5, op0=ALU.mult, op1=ALU.mult),
                     reads=[b_gatesT, b_par], writes=[b_gm[e % 3]])

        def emit_bcast(e):
            if e < NE:
                pgb_, pgbb, _ = EP.get()
                S.group("pe", [lambda p: p.matmul(pgb_[:, :TT], ones_bf[0:16, :], gm[e % 3][:], start=True, stop=True)],
                        reads=[b_gm[e % 3], b_par], writes=[pgbb])
                S.op("act", lambda a: a.activation(out=gbs[e % 2][:], in_=pgb_[:, :TT], func=AF.Copy),
                     reads=[pgbb], writes=[b_gbs[e % 2]])
        emit_gm(0)
        emit_gm(1)
        emit_bcast(0)
        for hf in range(4):
            for e in range(4 * hf, 4 * hf + 4):
                emit_gm(e + 2)
                for fh in range(2):
                    if fh == 1:
                        emit_bcast(e + 1)
                    kc = 2 * (e - 4 * hf) + fh
                    w_, bw_ = wgu_live.pop((t, 2 * e + fh))
                    pa, pab, _ = EP.get()
                    S.group("pe", [lambda p, c=c: p.matmul(pa[:, :TT], w_[:, 0, c, :], h2T[:, c, :], start=(c == 0), stop=(c == 7))
                                   for c in range(8)], reads=[bw_, b_h2T], writes=[pab])
                    pu, pub, _ = EP.get()
                    S.group("pe", [lambda p, c=c: p.matmul(pu[:, :TT], w_[:, 1, c, :], h2T[:, c, :], start=(c == 0), stop=(c == 7))
                                   for c in range(8)], reads=[bw_, b_h2T], writes=[pub])
                    i2 = kc % 2
                    S.op("act", lambda a: a.activation(out=sil[i2][:], in_=pa[:, :TT], func=AF.Tanh, scale=0.5),
                         reads=[pab], writes=[b_sil[i2]])
                    S.op("dve", lambda v: v.scalar_tensor_tensor(out=s2b[i2][:], in0=sil[i2][:], scalar=1.0, in1=pa[:, :TT],
                                                                 op0=ALU.add, op1=ALU.mult),
                         reads=[b_sil[i2], pab], writes=[b_s2[i2]])
                    S.op("dve", lambda v: v.tensor_tensor(out=gsb[i2][:], in0=s2b[i2][:], in1=gbs[e % 2][:], op=ALU.mult),
                         reads=[b_s2[i2], b_gbs[e % 2]], writes=[b_gs[i2]])
                    S.op("dve", lambda v: v.tensor_tensor(out=hid[:, kc, :], in0=gsb[i2][:], in1=pu[:, :TT], op=ALU.mult),
                         reads=[b_gs[i2], pub], writes=[b_hid[kc]])
                    wgu_prefetch(t, 2 * e + fh + NGU)
                    yield 2.3
            for fc in range(8):
                w_, bw_ = wd_live.pop((t, 8 * hf + fc))
                pd, pdb, _ = EP.get()
                S.group("pe", [lambda p, kc=kc: p.matmul(pd[:, :TT], w_[:, kc, :], hid[:, kc, :],
                                                         start=(kc == 0), stop=(kc == 7)) for kc in range(8)],
                        reads=[bw_] + b_hid, writes=[pdb])
                S.op("dve", lambda v: v.tensor_tensor(out=x_sb[:, fc, :], in0=pd[:, :TT], in1=x_sb[:, fc, :], op=ALU.add),
                     reads=[pdb, b_x[fc]], writes=[b_x[fc]])
                wd_prefetch(t, 8 * hf + fc + NWD)
                if hf == 3:
                    S.dma("sp", lambda q: q.dma_start(out=outT_d[fc * 128:(fc + 1) * 128, tok0:tok0 + TT], in_=x_sb[:, fc, :]),
                          b_x[fc], reads=[b_x[fc]])
                yield 1.15

    def run_interleaved(ga, gb):
        wa = wb_ = 0.0
        a_live, b_live = ga is not None, gb is not None
        if a_live and b_live:
            next(gb)
            wb_ = 8.0
        while a_live or b_live:
            if a_live and (not b_live or wa <= wb_):
                try:
                    wa += next(ga)
                except StopIteration:
                    a_live = False
            else:
                try:
                    wb_ += next(gb)
                except StopIteration:
                    b_live = False

    ntl = NT if ntiles is None else ntiles
    def mixer_full(t):
        yield from mixer(t)
        yield from moe_pre(t)

    run_interleaved(mixer_full(0), None)
    for t in range(ntl):
        run_interleaved(moe(t), mixer_full(t + 1) if t + 1 < ntl else None)

    for i in range(2):
        for c in range(8):
            for ev in list(b_xs[i][c].r.values()):
                S.wait_event("sp", ev)
    for bt in dbg_final:
        for ev in list(bt.r.values()):
            S.wait_event("sp", ev)
    if debug:
        print("ins per engine", S.nins, "counts", S.cnt)
    S.emit()
    es.close()
    return nc


def _consts():
    identf = np.eye(128, dtype=np.float32)
    s_idx = np.arange(128) // 16
    maskLT = (s_idx[None, :] >= s_idx[:, None]).astype(np.float32)
    hb = np.arange(128) // 64
    bd64 = (hb[:, None] == hb[None, :]).astype(np.float32) / 64.0
    return np.ascontiguousarray(np.concatenate([identf, maskLT, bd64], axis=1))


def _bias_index():
    k = np.arange(128)[:, None, None]
    kb = np.arange(5)[None, :, None]
    q = np.arange(128)[None, None, :]
    qpos = 512 + q
    kpos = kb * 128 + k
    idx = np.clip(qpos - kpos, -63, 256) + 63
    qchunk = qpos // 64
    kchunk = kpos // 64
    valid = (kchunk <= qchunk) & (kchunk >= qchunk - 8)
    return idx, valid


def prepare_inputs(inputs):
    f = lambda a: np.ascontiguousarray(np.asarray(a, dtype=np.float32))
    x = f(inputs["x"])
    L = 0
    vec = np.zeros((128, 64), np.float32)
    vec[:, 0:8] = f(inputs["mix_norm_gain"])[L].reshape(8, 128).T
    vec[:, 8:16] = f(inputs["ffn_norm_gain"])[L].reshape(8, 128).T
    vec[:, 16:32] = f(inputs["b_gate"])[L].reshape(16, 128).T
    vec[:, 32:36] = f(inputs["b_glu"])[L].reshape(4, 128).T
    vec[:, 36] = np.tile(f(inputs["q_gain"])[L], 2)
    vec[:, 37] = np.tile(f(inputs["k_gain"])[L], 2)
    vec[:, 40:44] = f(inputs["group_bias"])[L][None, :]
    vec[:, 44:60] = f(inputs["expert_bias"])[L][None, :]
    idx, valid = _bias_index()
    rb = f(inputs["rel_bias"])[L]
    bt = rb[:, idx]
    bt = np.where(valid[None], bt, np.float32(NEG)).astype(np.float32)
    biasT = np.ascontiguousarray(bt.transpose(1, 2, 0, 3)).reshape(128, 5 * 8 * 128)

    def gp_layout(a):
        return a.reshape(16, 2, 64).transpose(1, 2, 0).reshape(128, 16)
    small = np.zeros((128, 48), np.float32)
    small[:, 0:16] = gp_layout(f(inputs["ssm_lambda_re"])[L])
    small[:, 16:32] = gp_layout(f(inputs["ssm_lambda_im"])[L])
    small[:, 32:48] = gp_layout(np.broadcast_to(f(inputs["ssm_log_step"])[L][:, None], (32, 64)))
    bc = np.zeros((128, 4, 256), np.float32)
    for i, nm in enumerate(("ssm_b_re", "ssm_b_im")):
        a = f(inputs[nm])[L].reshape(16, 2, 64, 16).transpose(1, 2, 0, 3).reshape(128, 256)
        bc[:, i] = a
    for i, nm in enumerate(("ssm_c_re", "ssm_c_im")):
        a = f(inputs[nm])[L].reshape(16, 2, 16, 64).transpose(1, 3, 0, 2).reshape(128, 256)
        bc[:, 2 + i] = a
    drep = np.ascontiguousarray(np.tile(f(inputs["ssm_d"])[L].reshape(32, 16).T, (8, 1)))
    w_r = np.ascontiguousarray(np.concatenate([f(inputs["w_group_router"])[L], f(inputs["w_expert_router"])[L]], axis=1))
    common = {
        "w_in": f(inputs["w_in"])[L], "w_glu": f(inputs["w_glu"])[L], "w_branch": f(inputs["w_branch"])[L],
        "w_out": f(inputs["w_out"])[L], "w_e_gate": f(inputs["w_e_gate"])[L], "w_e_up": f(inputs["w_e_up"])[L],
        "w_e_down": f(inputs["w_e_down"])[L], "w_r": w_r, "vecs": vec, "biasT": biasT, "ssm_small": small,
        "ssm_bc": bc, "drep": drep, "consts": _consts(),
    }
    xs = x.reshape(NCORE, TOK, D)
    in_maps = []
    for i in range(NCORE):
        m = dict(common)
        m["xT"] = np.ascontiguousarray(xs[i].T)
        in_maps.append(m)
    return in_maps


_CACHE = {}


def kernel(**inputs):
    in_maps = prepare_inputs(inputs)
    if "nc" not in _CACHE:
        _CACHE["nc"] = build_program(TT=256)
    res = run_bass_kernel_spmd(_CACHE["nc"], in_maps, core_ids=list(range(NCORE)))
    out = np.stack([np.asarray(r["outT"]).T for r in res.results], axis=0)
    return np.ascontiguousarray(out.reshape(16, SEQ, D).astype(np.float32))
```

```python
import json
import math
import os
from contextlib import ExitStack

import numpy as np
import concourse.bass as bass
import concourse.mybir as mybir
from concourse.bass_utils import run_bass_kernel_spmd

F32 = mybir.dt.float32
BF16 = mybir.dt.bfloat16
I32 = mybir.dt.int32
AF = mybir.ActivationFunctionType
ALU = mybir.AluOpType
AX = mybir.AxisListType

D = 1024
SEQ = 2048
NCORE = 8
TOK = 4096
NH = 8
DH = 64
DA = 512
DS = 512
DIN = 4096
NE = 16
FE = 256
EPS = 1e-6
NEG = -30000.0


class Buf:
    __slots__ = ("name", "w", "r", "dsem", "dcnt")

    def __init__(self, name):
        self.name = name
        self.w = None
        self.r = {}
        self.dsem = None
        self.dcnt = 0


class Sched:
    ENG = ("pe", "act", "dve", "pool", "sp")

    def __init__(self, nc, es):
        self.nc = nc
        self.es = es
        self.sem = {e: es.enter_context(nc.semaphore("sem_" + e)) for e in self.ENG}
        self.cnt = {e: 0 for e in self.ENG}
        self.known = {e: {} for e in self.ENG}
        self.nins = {e: 0 for e in self.ENG}
        self.engines = {"pe": nc.tensor, "act": nc.scalar, "dve": nc.vector, "pool": nc.gpsimd, "sp": nc.sync}
        self.semname = {}
        self.nsem = 0

    def _key(self, sem):
        return id(sem)

    def _need(self, e, ev, waits):
        if ev is None:
            return
        k = self.known[e]
        key = self._key(ev[0])
        if k.get(key, 0) >= ev[1]:
            return
        k[key] = ev[1]
        waits.append(ev)

    def _waits(self, e, reads, writes):
        waits = []
        for b in reads:
            self._need(e, b.w, waits)
        for b in writes:
            self._need(e, b.w, waits)
            for ev in b.r.values():
                self._need(e, ev, waits)
        return waits

    def _commit(self, ev, reads, writes):
        key = self._key(ev[0])
        for b in reads:
            old = b.r.get(key)
            if old is None or old[1] < ev[1]:
                b.r[key] = ev
        for b in writes:
            b.w = ev
            b.r = {}

    def _emit(self, e, waits, fn, ev, inc):
        engine = self.engines[e]
        for (s_, v) in waits:
            engine.wait_ge(s_, v)
        self.nins[e] += 1
        if fn is None:
            return
        ins = fn(engine)
        if ev is not None:
            ins.then_inc(ev[0], inc)

    def op(self, e, fn, reads=(), writes=()):
        waits = self._waits(e, reads, writes)
        self.cnt[e] += 1
        ev = (self.sem[e], self.cnt[e])
        self._emit(e, waits, fn, ev, 1)
        self._commit(ev, reads, writes)
        return ev

    def group(self, e, fns, reads=(), writes=()):
        waits = self._waits(e, reads, writes)
        self.cnt[e] += 1
        ev = (self.sem[e], self.cnt[e])
        n = len(fns)
        for i, fn in enumerate(fns):
            self._emit(e, waits if i == 0 else [], fn, ev if i == n - 1 else None, 1)
        self._commit(ev, reads, writes)
        return ev

    def dma(self, e, fn, owner, reads=(), writes=()):
        if owner.dsem is None:
            owner.dsem = self.es.enter_context(self.nc.semaphore("dsem_%d" % self.nsem))
            self.nsem += 1
        waits = self._waits(e, reads, writes)
        owner.dcnt += 16
        ev = (owner.dsem, owner.dcnt)
        self._emit(e, waits, fn, ev, 16)
        self._commit(ev, reads, writes)
        return ev

    def wait_event(self, e, ev):
        waits = []
        self._need(e, ev, waits)
        if waits:
            self._emit(e, waits, None, None, 0)

    def barrier(self):
        last = {e: (self.sem[e], self.cnt[e]) for e in self.ENG if self.cnt[e] > 0}
        for e in self.ENG:
            waits = []
            for f, ev in last.items():
                if f != e:
                    self._need(e, ev, waits)
            if waits:
                self._emit(e, waits, None, None, 0)

    def emit(self):
        pass


TWO_PI = 2.0 * math.pi
TUNE = {"head": 14.0, "mix_scale": 0.8}
if os.environ.get("KTUNE"):
    TUNE.update(json.loads(os.environ["KTUNE"]))


def build_program(TT=256, debug=(), stage=99, ntiles=None):
    nc = bass.Bass("TRN2", target_bir_lowering=False)
    es = ExitStack()
    S = Sched(nc, es)
    NT = TOK // TT
    TPS = SEQ // TT
    NB = TT // 128
    NK = TT // 8
    NSLOT = 512 // TT + 1
    LV = int(math.log2(NK))

    def dram_in(name, shape, dt=F32):
        return nc.dram_tensor(name, list(shape), dt, kind="ExternalInput").ap()

    xT_d = dram_in("xT", [D, TOK])
    w_in_d = dram_in("w_in", [D, DIN])
    w_glu_d = dram_in("w_glu", [DS, DS])
    w_br_d = dram_in("w_branch", [D, D])
    w_out_d = dram_in("w_out", [D, D])
    weg_d = dram_in("w_e_gate", [NE, D, FE])
    weu_d = dram_in("w_e_up", [NE, D, FE])
    wed_d = dram_in("w_e_down", [NE, FE, D])
    w_r_d = dram_in("w_r", [D, 20])
    vec_d = dram_in("vecs", [128, 64])
    biasT_d = dram_in("biasT", [128, 5 * 8 * 128])
    ssm_d = dram_in("ssm_small", [128, 48])
    ssm_bc_d = dram_in("ssm_bc", [128, 4, 256])
    drep_d = dram_in("drep", [128, 32])
    cst_d = dram_in("consts", [128, 128 * 3])
    outT_d = nc.dram_tensor("outT", [D, TOK], F32, kind="ExternalOutput").ap()
    dbg_d = {}
    for name, shape in debug:
        dbg_d[name] = nc.dram_tensor("dbg_" + name, list(shape), F32, kind="ExternalOutput").ap()

    def sbt(stack, name, shape, dt=F32):
        return stack.enter_context(nc.sbuf_tensor("sb_" + name, list(shape), dt))

    def sb(name, shape, dt=F32):
        return sbt(es, name, shape, dt)

    PS = [es.enter_context(nc.psum_tensor("ps%d" % i, [128, 512], F32)) for i in range(8)]
    b_PS = [Buf("ps%d" % i) for i in range(8)]
    ps_ctr = [0]

    def next_ps():
        i = ps_ctr[0] % 8
        ps_ctr[0] += 1
        return PS[i], b_PS[i]

    w_in_s = nc.dram_tensor("w_in_bf", [D, DIN], BF16).ap()
    wgu_s = nc.dram_tensor("wgu_bf", [NE, 2, 128, 2, 8, 128], BF16).ap()
    wd_s = nc.dram_tensor("wd_bf", [4, 8, 128, 8, 128], BF16).ap()
    b_w_in_s = Buf("w_in_s")
    wbr_s = nc.dram_tensor("wbr_bf", [D, D], BF16).ap()
    wout_s = nc.dram_tensor("wout_bf", [D, D], BF16).ap()
    b_wbr_s = Buf("wbr_s")
    b_wout_s = Buf("wout_s")
    b_wgu_s = Buf("wgu_s")
    b_wd_s = Buf("wd_s")

    cst = sb("cst", [128, 128 * 3])
    identf = cst[:, 0:128]
    maskLT = cst[:, 128:256]
    bd64f = cst[:, 256:384]
    vec = sb("vec", [128, 64])
    b_par = Buf("params")
    S.dma("sp", lambda q: q.dma_start(out=cst[:], in_=cst_d), b_par, writes=[b_par])
    S.dma("sp", lambda q: q.dma_start(out=vec[:], in_=vec_d), b_par, writes=[b_par])
    g1_sb = vec[:, 0:8]
    g2_sb = vec[:, 8:16]
    bgate_sb = vec[:, 16:32]
    bglu_sb = vec[:, 32:36]
    rbias_sb = vec[:, 40:60]
    hbias = sb("hbias", [128, 20])
    S.op("dve", lambda v: v.tensor_scalar(out=hbias[:], in0=vec[:, 16:36], scalar1=0.5, scalar2=None, op0=ALU.mult),
         reads=[b_par], writes=[b_par])
    cq = sb("cq", [128, 1])
    S.op("dve", lambda v: v.tensor_scalar(out=cq[:], in0=vec[:, 36:37], scalar1=vec[:, 37:38],
                                          scalar2=0.125, op0=ALU.mult, op1=ALU.mult),
         reads=[b_par], writes=[b_par])
    ones_bf = sb("ones_bf", [128, 128], BF16)
    identb = sb("identb", [128, 128], BF16)
    bd64 = sb("bd64", [128, 128], BF16)
    ones16f = sb("ones16f", [16, 128])
    S.op("pool", lambda g: g.memset(ones_bf[:], 1.0), writes=[b_par])
    S.op("pool", lambda g: g.memset(ones16f[:], 1.0), writes=[b_par])
    S.op("dve", lambda v: v.tensor_copy(out=identb[:], in_=identf), reads=[b_par], writes=[b_par])
    S.op("dve", lambda v: v.tensor_copy(out=bd64[:], in_=bd64f), reads=[b_par], writes=[b_par])

    wglu = sb("wglu", [128, 4, DS], BF16)
    wr = sb("wr", [128, 8, 20], BF16)
    b_wres = Buf("wres")
    for (tl, src) in ((wglu, w_glu_d), (wr, w_r_d)):
        S.dma("pool", lambda q, tl=tl, src=src: q.dma_start(
            out=tl[:], in_=src.rearrange("(c p) f -> p c f", p=128)), b_wres, writes=[b_wres])

    biasT = sb("biasT", [128, 5 * 8 * 128], BF16)
    b_bias = Buf("biasT")
    S.dma("pool", lambda q: q.dma_start(out=biasT[:], in_=biasT_d), b_bias, writes=[b_bias])
    biasT4 = biasT[:].rearrange("p (kb h q) -> p kb h q", kb=5, h=8)

    T_all = sb("T_all", [128, 32, 128], BF16)
    MBre = sb("MBre", [128, 32, 64], BF16)
    MBim = sb("MBim", [128, 32, 64], BF16)
    MCre = sb("MCre", [128, 32, 128], BF16)
    MCim = sb("MCim", [128, 32, 128], BF16)
    Ere = sb("Ere", [128, 16, NK])
    Eim = sb("Eim", [128, 16, NK])
    Rtab = sb("Rtab", [128, 16, NK])
    A8 = sb("A8", [128, 2, 16])
    b_tab = Buf("ssm_tables")

    with ExitStack() as ps_:
        def tb(name, shape, dt=F32):
            return sbt(ps_, name, shape, dt)
        small = tb("ssm_small", [128, 48])
        bc = tb("ssm_bc", [128, 4, 16, 16])
        drep = tb("drep", [128, 32])
        b_p = Buf("prep")
        S.dma("sp", lambda q: q.dma_start(out=small[:], in_=ssm_d), b_p, writes=[b_p])
        S.dma("sp", lambda q: q.dma_start(out=bc[:], in_=ssm_bc_d.rearrange("p a (g c) -> p a g c", g=16)),
              b_p, writes=[b_p])
        S.dma("sp", lambda q: q.dma_start(out=drep[:], in_=drep_d), b_p, writes=[b_p])
        lre = small[:, 0:16]
        lim = small[:, 16:32]
        lst = small[:, 32:48]
        W = tb("wk", [128, 24, 16])
        (STEP, XR, MAG, MAGI, ANG, TQ, TF, RS, RC, SN, CS, ARE, AIM, IRE, IIM, NRE, DEN, RDEN,
         FRE, FIM, T1, T2, T3, T4) = [W[:, i, :] for i in range(24)]
        WI = tb("wki", [128, 16], I32)

        def P(eng, fn):
            S.op(eng, fn, reads=[b_p, b_par], writes=[b_p])

        def tt(out, a, b, op):
            P("dve", lambda v: v.tensor_tensor(out=out, in0=a, in1=b, op=op))

        def ts(out, a, s1, op0, s2=None, op1=None):
            if op1 is None:
                P("dve", lambda v: v.tensor_scalar(out=out, in0=a, scalar1=s1, scalar2=None, op0=op0))
            else:
                P("dve", lambda v: v.tensor_scalar(out=out, in0=a, scalar1=s1, scalar2=s2, op0=op0, op1=op1))

        def actf(out, a, func, scale=1.0, bias=0.0):
            P("act", lambda x: x.activation(out=out, in_=a, func=func, scale=scale, bias=bias))

        def cmul(ore, oim, xre, xim, yre, yim, t1, t2):
            tt(t1, xre, yre, ALU.mult)
            tt(t2, xim, yim, ALU.mult)
            tt(ore, t1, t2, ALU.subtract)
            tt(t1, xre, yim, ALU.mult)
            tt(t2, xim, yre, ALU.mult)
            tt(oim, t1, t2, ALU.add)

        actf(STEP, lst, AF.Exp)
        tt(XR, lre, STEP, ALU.mult)
        actf(MAG, XR, AF.Exp)
        actf(MAGI, XR, AF.Exp, scale=-1.0)
        tt(ANG, lim, STEP, ALU.mult)
        ts(TQ, ANG, 1.0 / TWO_PI, ALU.mult)
        P("dve", lambda v: v.tensor_copy(out=WI[:], in_=TQ))
        P("dve", lambda v: v.tensor_copy(out=TF, in_=WI[:]))
        P("dve", lambda v: v.scalar_tensor_tensor(out=RS, in0=TF, scalar=-TWO_PI, in1=ANG,
                                                  op0=ALU.mult, op1=ALU.add))

        def wrap(x):
            ts(T1, x, math.pi, ALU.is_gt, TWO_PI, ALU.mult)
            tt(x, x, T1, ALU.subtract)
            ts(T1, x, -math.pi, ALU.is_lt, TWO_PI, ALU.mult)
            tt(x, x, T1, ALU.add)

        wrap(RS)
        ts(RC, RS, math.pi / 2, ALU.add)
        wrap(RC)
        actf(SN, RS, AF.Sin)
        actf(CS, RC, AF.Sin)
        tt(ARE, MAG, CS, ALU.mult)
        tt(AIM, MAG, SN, ALU.mult)
        tt(IRE, MAGI, CS, ALU.mult)
        tt(T1, MAGI, SN, ALU.mult)
        ts(IIM, T1, -1.0, ALU.mult)
        ts(NRE, ARE, -1.0, ALU.add)
        tt(T1, lre, lre, ALU.mult)
        tt(T2, lim, lim, ALU.mult)
        tt(DEN, T1, T2, ALU.add)
        P("dve", lambda v: v.reciprocal(out=RDEN, in_=DEN))
        tt(T1, NRE, lre, ALU.mult)
        tt(T2, AIM, lim, ALU.mult)
        tt(T1, T1, T2, ALU.add)
        tt(FRE, T1, RDEN, ALU.mult)
        tt(T1, AIM, lre, ALU.mult)
        tt(T2, NRE, lim, ALU.mult)
        tt(T1, T1, T2, ALU.subtract)
        tt(FIM, T1, RDEN, ALU.mult)
        BB = tb("bb", [128, 2, 16, 16])
        TB = tb("tbb", [128, 2, 16, 16])
        bre_, bim_, cre_, cim_ = bc[:, 0], bc[:, 1], bc[:, 2], bc[:, 3]

        def bcg(x):
            return x.rearrange("p (g o) -> p g o", o=1).to_broadcast([128, 16, 16])

        tt(TB[:, 0], bre_, bcg(FRE), ALU.mult)
        tt(TB[:, 1], bim_, bcg(FIM), ALU.mult)
        tt(BB[:, 0], TB[:, 0], TB[:, 1], ALU.subtract)
        tt(TB[:, 0], bim_, bcg(FRE), ALU.mult)
        tt(TB[:, 1], bre_, bcg(FIM), ALU.mult)
        tt(BB[:, 1], TB[:, 0], TB[:, 1], ALU.add)
        Pre = tb("Pre", [128, 9, 16])
        Pim = tb("Pim", [128, 9, 16])
        Qre = tb("Qre", [128, 9, 16])
        Qim = tb("Qim", [128, 9, 16])
        for (pr, pi, xr_, xi_) in ((Pre, Pim, ARE, AIM), (Qre, Qim, IRE, IIM)):
            P("dve", lambda v, pr=pr: v.memset(pr[:, 0, :], 1.0))
            P("dve", lambda v, pi=pi: v.memset(pi[:, 0, :], 0.0))
            for d in range(1, 9):
                cmul(pr[:, d, :], pi[:, d, :], pr[:, d - 1, :], pi[:, d - 1, :], xr_, xi_, T1, T2)
        P("dve", lambda v: v.tensor_copy(out=A8[:, 0, :], in_=Pre[:, 8, :]))
        P("dve", lambda v: v.tensor_copy(out=A8[:, 1, :], in_=Pim[:, 8, :]))
        MCf = tb("MCf", [128, 2, 16, 128])
        XTf = tb("XTf", [128, 2, 16, 128])
        MBf = tb("MBf", [128, 2, 16, 128])
        TMP = tb("TMPf", [128, 2, 16, 16])

        def v4(x, i):
            return x[:, i].rearrange("p g (t c) -> p g t c", t=8)

        for tq in range(8):
            prb = bcg(Pre[:, tq + 1, :])
            pib = bcg(Pim[:, tq + 1, :])
            tt(TMP[:, 0], cre_, prb, ALU.mult)
            tt(TMP[:, 1], cim_, pib, ALU.mult)
            tt(v4(MCf, 0)[:, :, tq, :], TMP[:, 0], TMP[:, 1], ALU.subtract)
            tt(TMP[:, 0], cre_, pib, ALU.mult)
            tt(TMP[:, 1], cim_, prb, ALU.mult)
            tt(TMP[:, 0], TMP[:, 0], TMP[:, 1], ALU.add)
            ts(v4(MCf, 1)[:, :, tq, :], TMP[:, 0], -1.0, ALU.mult)
            for (dst, pr, pi, d) in ((MBf, Pre, Pim, 7 - tq), (XTf, Qre, Qim, tq + 1)):
                prb2 = bcg(pr[:, d, :])
                pib2 = bcg(pi[:, d, :])
                tt(TMP[:, 0], BB[:, 0], prb2, ALU.mult)
                tt(TMP[:, 1], BB[:, 1], pib2, ALU.mult)
                tt(v4(dst, 0)[:, :, tq, :], TMP[:, 0], TMP[:, 1], ALU.subtract)
                tt(TMP[:, 0], BB[:, 1], prb2, ALU.mult)
                tt(TMP[:, 1], BB[:, 0], pib2, ALU.mult)
                tt(v4(dst, 1)[:, :, tq, :], TMP[:, 0], TMP[:, 1], ALU.add)
        for g in range(32):
            for ri, MCt in enumerate((MCre, MCim)):
                S.op("dve", lambda v, g=g, ri=ri, MCt=MCt: v.tensor_scalar(
                    out=MCt[:, g, :], in0=MCf[:, ri, g // 2, :], scalar1=bd64f[:, 64 * (g % 2):64 * (g % 2) + 1],
                    scalar2=64.0, op0=ALU.mult, op1=ALU.mult), reads=[b_p, b_par], writes=[b_tab])
        TMPM = tb("TMPM", [128, 128])
        for g in range(32):
            gp, g2 = g // 2, g % 2
            lo, hi = 64 * g2, 64 * g2 + 64
            pst, psb = next_ps()
            S.group("pe", [
                lambda p, pst=pst, gp=gp, lo=lo, hi=hi: p.matmul(pst[:, 0:128], XTf[lo:hi, 0, gp, :], MCf[lo:hi, 0, gp, :],
                                                                 start=True, stop=False),
                lambda p, pst=pst, gp=gp, lo=lo, hi=hi: p.matmul(pst[:, 0:128], XTf[lo:hi, 1, gp, :], MCf[lo:hi, 1, gp, :],
                                                                 start=False, stop=True),
                lambda p, pst=pst, gp=gp, lo=lo, hi=hi: p.matmul(pst[:, 128:192], MBf[lo:hi, 0, gp, :], identf[lo:hi, lo:hi],
                                                                 start=True, stop=True),
                lambda p, pst=pst, gp=gp, lo=lo, hi=hi: p.matmul(pst[:, 192:256], MBf[lo:hi, 1, gp, :], identf[lo:hi, lo:hi],
                                                                 start=True, stop=True),
            ], reads=[b_p, b_par], writes=[psb])
            S.op("dve", lambda v, pst=pst: v.tensor_tensor(out=TMPM[:], in0=pst[:, 0:128], in1=maskLT, op=ALU.mult),
                 reads=[psb, b_par, b_p], writes=[b_p])
            S.op("dve", lambda v, g=g: v.scalar_tensor_tensor(out=T_all[:, g, :], in0=identf, scalar=drep[:, g:g + 1],
                                                             in1=TMPM[:], op0=ALU.mult, op1=ALU.add),
                 reads=[b_p, b_par], writes=[b_tab])
            S.op("act", lambda a, pst=pst, g=g: a.activation(out=MBre[:, g, :], in_=pst[:, 128:192], func=AF.Copy),
                 reads=[psb], writes=[b_tab])
            S.op("act", lambda a, pst=pst, g=g: a.activation(out=MBim[:, g, :], in_=pst[:, 192:256], func=AF.Copy),
                 reads=[psb], writes=[b_tab])
        M8I = T3
        actf(M8I, XR, AF.Exp, scale=-8.0)
        actf(T4, XR, AF.Exp, scale=8.0)
        P("dve", lambda v: v.tensor_copy(out=Rtab[:], in_=T4.rearrange("p (g o) -> p g o", o=1).to_broadcast([128, 16, NK])))
        P("dve", lambda v: v.memset(Rtab[:, :, 0:1], 0.0))
        tt(Ere[:, :, 0], Pre[:, 8, :], M8I, ALU.mult)
        tt(Eim[:, :, 0], Pim[:, 8, :], M8I, ALU.mult)
        ETr = tb("ETr", [128, 16, NK])
        ETi = tb("ETi", [128, 16, NK])
        m = 1
        while m < NK:
            ub_r = Ere[:, :, m - 1:m].to_broadcast([128, 16, m])
            ub_i = Eim[:, :, m - 1:m].to_broadcast([128, 16, m])
            tt(ETr[:, :, 0:m], Ere[:, :, 0:m], ub_r, ALU.mult)
            tt(ETi[:, :, 0:m], Eim[:, :, 0:m], ub_i, ALU.mult)
            tt(Ere[:, :, m:2 * m], ETr[:, :, 0:m], ETi[:, :, 0:m], ALU.subtract)
            tt(ETr[:, :, 0:m], Ere[:, :, 0:m], ub_i, ALU.mult)
            tt(ETi[:, :, 0:m], Eim[:, :, 0:m], ub_r, ALU.mult)
            tt(Eim[:, :, m:2 * m], ETr[:, :, 0:m], ETi[:, :, 0:m], ALU.add)
            m *= 2
        S.op("dve", lambda v: v.memset(TMPM[0:1, 0:1], 0.0), reads=[b_p], writes=[b_tab, b_p])
        S.barrier()

    xs = [sb("x_sb%d" % i, [128, 8, TT]) for i in range(2)]
    b_xs = [[Buf("x%d_%d" % (i, c)) for c in range(8)] for i in range(2)]
    sq_sb = [sb("sq%d" % i, [128, TT], BF16) for i in range(2)]
    b_sq = [Buf("sq%d" % i) for i in range(2)]
    hT = sb("hT", [128, 8, TT], BF16)
    b_hT = Buf("hT")
    h2T = sb("h2T", [128, 8, TT], BF16)
    b_h2T = Buf("h2T")
    ln_sb = [sb("ln%d" % i, [128, TT]) for i in range(2)]
    b_ln = [Buf("ln%d" % i) for i in range(2)]
    rstd = [sb("rstd%d" % i, [128, TT]) for i in range(2)]
    b_rstd = [Buf("rstd%d" % i) for i in range(2)]
    rs_ctr = [0]
    NSLAB = 3
    wslab = [sb("wslab%d" % i, [128, 8, 256], BF16) for i in range(NSLAB)]
    b_wslab = [Buf("wslab%d" % i) for i in range(NSLAB)]
    slab_ctr = [0]
    ms_all = sb("ms_all", [128, 8, TT], BF16)
    b_ms = Buf("ms_all")
    qT = sb("qT", [128, 4, TT], BF16)
    b_qT = Buf("qT")
    kT = [sb("kT%d" % i, [128, 4, TT], BF16) for i in range(NSLOT)]
    b_kT = [Buf("kT%d" % i) for i in range(NSLOT)]
    Vaug = [sb("Vaug%d" % i, [128, NB, NH, 65], BF16) for i in range(NSLOT)]
    b_V = [Buf("V%d" % i) for i in range(NSLOT)]
    for i in range(NSLOT):
        S.op("pool", lambda g, i=i: g.memset(Vaug[i][:], 1.0), writes=[b_V[i]])
    Utok = sb("Utok", [NK, 8, 512], BF16)
    b_Utok = Buf("Utok")
    Ush = sb("Ush", [128, 32, NK], BF16)
    b_Ush = Buf("Ush")
    gateT = sb("gateT", [128, 16, TT], BF16)
    b_gate = Buf("gateT")
    att_tmp = [sb("att_tmp%d" % i, [128, 512]) for i in range(2)]
    b_att_tmp = [Buf("att_tmp%d" % i) for i in range(2)]
    PT = [sb("PT%d" % i, [128, 512], BF16) for i in range(3)]
    b_PT = [Buf("PT%d" % i) for i in range(3)]
    att_ctr = [0, 0]
    rden = sb("rden", [128, 2, 4])
    b_rden = [Buf("rden0"), Buf("rden1")]
    yatt = sb("yatt", [128, NB, 512], BF16)
    b_yatt = [Buf("yatt%d" % i) for i in range(NB)]
    yaT = sb("yaT", [128, 4, TT], BF16)
    b_yaT = Buf("yaT")
    Sf = sb("Sf", [128, 2, 16, NK])
    b_Sf = Buf("Sf")
    Xf = sb("Xf", [128, 2, 16, NK])
    b_Xf = Buf("Xf")
    Zf, b_Zf = Sf, b_Sf
    Tf = sb("Tf", [128, 2, 16, NK])
    b_Tf = Buf("Tf")
    Hf = sb("Hf", [128, 2, 16, NK + 1])
    b_Hf = Buf("Hf")
    Hb = sb("Hb", [128, 2, 16, NK], BF16)
    b_Hb = Buf("Hb")
    cz = sb("cz", [128, 4, 16])
    zT = sb("zT", [128, 4, TT], BF16)
    b_zT = Buf("zT")
    sig = [sb("sig%d" % i, [128, TT], BF16) for i in range(2)]
    b_sig = [Buf("sig%d" % i) for i in range(2)]
    ysT = sb("ysT", [128, 4, TT], BF16)
    b_ysT = Buf("ysT")
    mT = sb("mT", [128, 8, TT], BF16)
    b_mT = Buf("mT")
    m12 = [sb("m12_%d" % i, [128, TT], BF16) for i in range(2)]
    b_m12 = [Buf("m12_%d" % i) for i in range(2)]
    Lr = sb("Lr", [128, NB, 20])
    rt = sb("rt", [128, 12, NB, 4])
    rt16 = sb("rt16", [128, NB, 16])
    gates = sb("gates", [128, NB, 16])
    b_rt = Buf("router")
    gatesT = sb("gatesT", [16, TT])
    b_gatesT = Buf("gatesT")
    gm = [sb("gm%d" % i, [16, TT], BF16) for i in range(3)]
    b_gm = [Buf("gm%d" % i) for i in range(3)]
    if stage < 7:
        dbgtmp = sb("dbgtmp", [128, 8 * TT])
        b_dbgtmp = Buf("dbgtmp")
    hid = sb("hid", [128, 8, TT] if stage >= 7 else [128, 2, 2], BF16)
    b_hid = [Buf("hid%d" % i) for i in range(8)]
    NGU = 4
    wgu = [sb("wgu%d" % i, [128, 2, 8, 128], BF16) for i in range(NGU)]
    b_wgu = [Buf("wgu%d" % i) for i in range(NGU)]
    NWD = 4
    wd = [sb("wd%d" % i, [128, 8, 128], BF16) for i in range(NWD)]
    b_wd = [Buf("wd%d" % i) for i in range(NWD)]
    sil = [sb("sil%d" % i, [128, TT], BF16) for i in range(2)]
    b_sil = [Buf("sil%d" % i) for i in range(2)]
    gsb = [sb("gs%d" % i, [128, TT], BF16) for i in range(2)]
    gbs = [sb("gbs%d" % i, [128, TT], BF16) for i in range(2)]
    b_gbs = [Buf("gbs%d" % i) for i in range(2)]
    s2b = [sb("s2_%d" % i, [128, TT], BF16) for i in range(2)]
    b_s2 = [Buf("s2_%d" % i) for i in range(2)]
    b_gs = [Buf("gs%d" % i) for i in range(2)]
    moe_ctr = [0, 0]

    b_s_in = [Buf("s_in%d" % j) for j in range(16)]
    b_s_br = [Buf("s_br%d" % j) for j in range(4)]
    b_s_out = [Buf("s_out%d" % j) for j in range(4)]
    b_s_gu = [[Buf("s_gu%d_%d" % (e, fh)) for fh in range(2)] for e in range(NE)]
    b_s_wd = [[Buf("s_wd%d_%d" % (hf, fc)) for fc in range(8)] for hf in range(4)]
    wed_v = wed_d.rearrange("e (fh p) (fc j) -> fc p (e fh) j", fh=2, j=128)

    class PsPool:
        def __init__(self, idxs):
            self.idxs, self.ctr, self.held = list(idxs), 0, set()

        def get(self, hold=False):
            while True:
                i = self.idxs[self.ctr % len(self.idxs)]
                self.ctr += 1
                if i not in self.held:
                    break
            if hold:
                self.held.add(i)
            return PS[i], b_PS[i], i

        def release(self, i):
            self.held.discard(i)

    MP = PsPool(range(0, 4))
    EP = PsPool(range(4, 8))

    def load_slab(t, which, j):
        src_f, src_s, bs = ((w_in_d, w_in_s, b_s_in), (w_br_d, wbr_s, b_s_br), (w_out_d, wout_s, b_s_out))[which]
        i = slab_ctr[0] % NSLAB
        slab_ctr[0] += 1
        t_, b_ = wslab[i], b_wslab[i]
        col0 = 256 * j
        if t == 0:
            S.dma("pool", lambda q: q.dma_start(
                out=t_[:], in_=src_f[:, col0:col0 + 256].rearrange("(c p) f -> p c f", p=128)), b_, writes=[b_])
            S.dma("sp", lambda q: q.dma_start(
                out=src_s[:, col0:col0 + 256].rearrange("(c p) f -> p c f", p=128), in_=t_[:]),
                b_, reads=[b_], writes=[bs[j]])
        else:
            S.dma("sp", lambda q: q.dma_start(
                out=t_[:], in_=src_s[:, col0:col0 + 256].rearrange("(c p) f -> p c f", p=128)),
                b_, reads=[bs[j]], writes=[b_])
        return t_, b_

    def load_wgu(t, e, fh):
        iw = moe_ctr[0] % NGU
        moe_ctr[0] += 1
        t_, b_ = wgu[iw], b_wgu[iw]
        if t == 0:
            S.dma("pool", lambda q: q.dma_start(
                out=t_[:, 0], in_=weg_d[e][:, fh * 128:(fh + 1) * 128].rearrange("(c p) f -> p c f", p=128)), b_, writes=[b_])
            S.dma("pool", lambda q: q.dma_start(
                out=t_[:, 1], in_=weu_d[e][:, fh * 128:(fh + 1) * 128].rearrange("(c p) f -> p c f", p=128)), b_, writes=[b_])
            S.dma("sp", lambda q: q.dma_start(out=wgu_s[e, fh], in_=t_[:]), b_, reads=[b_], writes=[b_s_gu[e][fh]])
        else:
            S.dma("pool", lambda q: q.dma_start(out=t_[:], in_=wgu_s[e, fh]), b_, reads=[b_s_gu[e][fh]], writes=[b_])
        return t_, b_

    wd_live = {}
    wgu_live = {}

    def wgu_prefetch(t, idx):
        if idx < 32:
            wgu_live[(t, idx)] = load_wgu(t, idx // 2, idx % 2)

    def wd_prefetch(t, idx):
        if idx < 32:
            wd_live[(t, idx)] = load_wd(t, idx // 8, idx % 8)

    def load_wd(t, hf, fc):
        iw = moe_ctr[1] % NWD
        moe_ctr[1] += 1
        t_, b_ = wd[iw], b_wd[iw]
        if t == 0:
            S.dma("pool", lambda q: q.dma_start(out=t_[:], in_=wed_v[fc][:, 8 * hf:8 * hf + 8, :]), b_, writes=[b_])
            S.dma("sp", lambda q: q.dma_start(out=wd_s[hf, fc], in_=t_[:]), b_, reads=[b_], writes=[b_s_wd[hf][fc]])
        else:
            S.dma("pool", lambda q: q.dma_start(out=t_[:], in_=wd_s[hf, fc]), b_, reads=[b_s_wd[hf][fc]], writes=[b_])
        return t_, b_

    def dump(name, src, bufs, dst_ap):
        if name not in dbg_d:
            return
        shape = [int(s_) for s_ in src.shape]
        n = 1
        for s_ in shape[1:]:
            n *= s_
        tv = dbgtmp[0:shape[0], 0:n]
        if len(shape) == 3:
            tv = tv.rearrange("p (a b) -> p a b", a=shape[1])
        bt = b_dbgtmp
        S.op("dve", lambda v: v.tensor_copy(out=tv, in_=src), reads=bufs, writes=[bt])
        S.dma("sp", lambda q: q.dma_start(out=dst_ap, in_=tv), bt, reads=[bt])
        if bt not in dbg_final:
            dbg_final.append(bt)

    dbg_final = []

    def dslice(name, nrows, tok0):
        return dbg_d[name][0:nrows, tok0:tok0 + TT].rearrange("(c p) t -> p c t", p=128) if name in dbg_d else None

    def rs_from_ps(pst, psb, scale):
        i = rs_ctr[0] % 2
        rs_ctr[0] += 1
        S.op("act", lambda a: a.activation(out=ln_sb[i][:], in_=pst[:, :TT], func=AF.Ln, scale=scale, bias=EPS),
             reads=[psb], writes=[b_ln[i]])
        S.op("act", lambda a: a.activation(out=rstd[i][:], in_=ln_sb[i][:], func=AF.Exp, scale=-0.5),
             reads=[b_ln[i]], writes=[b_rstd[i]])
        return rstd[i], b_rstd[i]

    def rmsnorm(pool, x_sb, b_x, gain_sb, out_t, out_b):
        pst, psb, _ = pool.get()
        S.op("act", lambda a: a.activation(out=out_t[:], in_=x_sb[:], func=AF.Square), reads=list(b_x), writes=[out_b])
        S.group("pe", [lambda p, c=c: p.matmul(pst[:, :TT], ones_bf[:], out_t[:, c, :], start=(c == 0), stop=(c == 7))
                       for c in range(8)], reads=[out_b, b_par], writes=[psb])
        r_t, r_b = rs_from_ps(pst, psb, 1.0 / D)
        for c in range(8):
            S.op("dve", lambda v: v.scalar_tensor_tensor(
                out=out_t[:, c, :], in0=x_sb[:, c, :], scalar=gain_sb[:, c:c + 1], in1=r_t[:],
                op0=ALU.mult, op1=ALU.mult), reads=[b_x[c], r_b, b_par], writes=[out_b])
        yield 1.3
        yield 6.0

    def proj_fm(slab, bslab, col, rhs_t, rhs_b, nchunk=8, hold=False):
        pst, psb, pi_ = MP.get(hold=hold)
        S.group("pe", [lambda p, c=c: p.matmul(pst[:, :TT], slab[:, c, col:col + 128], rhs_t[:, c, :],
                                               start=(c == 0), stop=(c == nchunk - 1)) for c in range(nchunk)],
                reads=[bslab, rhs_b], writes=[psb])
        return pst, psb, pi_

    def mixer(t):
        tok0 = t * TT
        seq_t = t % TPS
        slot = t % NSLOT
        x_sb, b_x = xs[t % 2], b_xs[t % 2]
        for c in range(8):
            S.dma("sp", lambda q: q.dma_start(out=x_sb[:, c, :], in_=xT_d[c * 128:(c + 1) * 128, tok0:tok0 + TT]),
                  b_x[c], writes=[b_x[c]])
        yield 0.0
        if stage < 1:
            return
        yield from rmsnorm(MP, x_sb, b_x, g1_sb, hT, b_hT)
        dump("h", hT[:], [b_hT], dslice("h", 1024, tok0))

        pend = []

        def qk_finish(i, sq, bq):
            pm, pmb, _ = MP.get()
            S.group("pe", [lambda p: p.matmul(pm[:, :TT], bd64[:], sq[:], start=True, stop=True)],
                    reads=[bq, b_par], writes=[pmb])
            S.op("dve", lambda v: v.tensor_copy(out=ms_all[:, i, :], in_=pm[:, :TT]), reads=[pmb], writes=[b_ms])

        def qk_norm():
            S.op("act", lambda a: a.activation(out=ms_all[:], in_=ms_all[:], func=AF.Ln, bias=EPS), reads=[b_ms], writes=[b_ms])
            S.op("act", lambda a: a.activation(out=ms_all[:], in_=ms_all[:], func=AF.Exp, scale=-0.5), reads=[b_ms], writes=[b_ms])
            S.op("dve", lambda v: v.scalar_tensor_tensor(out=qT[:], in0=qT[:], scalar=cq[:, 0:1], in1=ms_all[:, 0:4, :],
                                                         op0=ALU.mult, op1=ALU.mult), reads=[b_qT, b_ms, b_par], writes=[b_qT])
            S.op("dve", lambda v: v.tensor_tensor(out=kT[slot][:], in0=kT[slot][:], in1=ms_all[:, 4:8, :], op=ALU.mult),
                 reads=[b_kT[slot], b_ms], writes=[b_kT[slot]])
        for which in range(2):
            for half in range(2):
                slab, bslab = load_slab(t, 0, 2 * which + half)
                for m in range(2 * half, 2 * half + 2):
                    pq, pqb, pqi = proj_fm(slab, bslab, (m % 2) * 128, hT, b_hT)
                    sq, bq = sq_sb[m % 2], b_sq[m % 2]
                    S.op("act", lambda a: a.activation(out=sq[:], in_=pq[:, :TT], func=AF.Square),
                         reads=[pqb], writes=[bq])
                    if which == 0:
                        S.op("act", lambda a: a.activation(out=qT[:, m, :], in_=pq[:, :TT], func=AF.Copy),
                             reads=[pqb], writes=[b_qT])
                    else:
                        S.op("act", lambda a: a.activation(out=kT[slot][:, m, :], in_=pq[:, :TT], func=AF.Copy),
                             reads=[pqb], writes=[b_kT[slot]])
                    yield 1.2
                    if pend:
                        qk_finish(*pend.pop())
                    pend.append((4 * which + m, sq, bq))
        if stage < 2:
            qk_finish(*pend.pop())
            qk_norm()
            return
        for half in range(2):
            slab, bslab = load_slab(t, 0, 4 + half)
            for blk in range(NB):
                pv, pvb, _ = MP.get()
                S.group("pe", [lambda p, c=c: p.matmul(
                    pv[:, 0:256], hT[:, c, blk * 128:(blk + 1) * 128], slab[:, c, :],
                    start=(c == 0), stop=(c == 7)) for c in range(8)],
                    reads=[bslab, b_hT], writes=[pvb])
                S.op("act", lambda a: a.activation(
                    out=Vaug[slot][:, blk, 4 * half:4 * half + 4, 0:64],
                    in_=pv[:, 0:256].rearrange("p (h d) -> p h d", h=4), func=AF.Copy),
                    reads=[pvb], writes=[b_V[slot]])
                yield 1.2
                if pend:
                    qk_finish(*pend.pop())
                    qk_norm()
        dump("q", qT[:], [b_qT], dslice("q", 512, tok0))
        dump("k", kT[slot][:], [b_kT[slot]], dslice("k", 512, tok0))

        hT_kt = hT[:].rearrange("p c (k t) -> p c t k", t=8)
        Utok_g = Utok[:].rearrange("k t c -> k (t c)").rearrange("k (g t c) -> k g t c", g=32, t=8)
        for half in range(2):
            slab, bslab = load_slab(t, 0, 6 + half)
            for tau in range(8):
                pu, pub, _ = MP.get()
                S.group("pe", [lambda p, c=c: p.matmul(
                    pu[0:NK, 0:256], hT_kt[:, c, tau, :], slab[:, c, :],
                    start=(c == 0), stop=(c == 7)) for c in range(8)],
                    reads=[bslab, b_hT], writes=[pub])
                S.op("act", lambda a: a.activation(
                    out=Utok_g[:, 16 * half:16 * half + 16, tau, :],
                    in_=pu[0:NK, 0:256].rearrange("k (g c) -> k g c", c=16), func=AF.Copy),
                    reads=[pub], writes=[b_Utok])
                yield 1.2

        if stage >= 4:
            pt, ptb, _ = MP.get()
            ptv = pt[:].bitcast(BF16).rearrange("p (g k) -> p g k", k=NK)
            S.group("pe", [lambda p, g=g: p.transpose(ptv[:, g, :], Utok_g[:, g].rearrange("k t c -> k (t c)"),
                                                      identb[0:NK, 0:NK]) for g in range(32)],
                    reads=[b_Utok, b_par], writes=[ptb])
            S.op("act", lambda a: a.activation(out=Ush[:], in_=ptv[:, 0:32, :], func=AF.Copy),
                 reads=[ptb], writes=[b_Ush])
            yield 2.0
            for ri, MB in enumerate((MBre, MBim)):
                pss, pssb, _ = MP.get()
                psv = pss[:, 0:16 * NK].rearrange("p (g k) -> p g k", k=NK)
                S.group("pe", [lambda p, g=g: p.matmul(
                    psv[64 * (g % 2):64 * (g % 2) + 64, g // 2, :], MB[:, g, :], Ush[:, g, :], start=True, stop=True)
                    for g in range(32)], reads=[b_Ush, b_tab], writes=[pssb])
                S.op("act", lambda a: a.activation(out=Sf[:, ri], in_=psv, func=AF.Copy),
                     reads=[pssb], writes=[b_Sf])
                yield 2.0
            if seq_t == 0:
                S.op("dve", lambda v: v.memset(Hf[:, :, :, 0:1], 0.0), writes=[b_Hf])
            else:
                S.op("dve", lambda v: v.tensor_copy(out=Hf[:, :, :, 0:1], in_=Hf[:, :, :, NK:NK + 1]),
                     reads=[b_Hf], writes=[b_Hf])
            c_re, c_im = Hf[:, 0, :, 0], Hf[:, 1, :, 0]

            def dv(fn, reads, writes):
                S.op("dve", fn, reads=reads, writes=writes)
            rw = [b_Hf, b_Sf, b_tab]
            dv(lambda v: v.tensor_tensor(out=cz[:, 0], in0=A8[:, 0], in1=c_re, op=ALU.mult), rw, [b_Sf])
            dv(lambda v: v.tensor_tensor(out=cz[:, 1], in0=A8[:, 1], in1=c_im, op=ALU.mult), rw, [b_Sf])
            dv(lambda v: v.tensor_tensor(out=cz[:, 2], in0=A8[:, 0], in1=c_im, op=ALU.mult), rw, [b_Sf])
            dv(lambda v: v.tensor_tensor(out=cz[:, 3], in0=A8[:, 1], in1=c_re, op=ALU.mult), rw, [b_Sf])
            dv(lambda v: v.tensor_tensor(out=cz[:, 0], in0=cz[:, 0], in1=cz[:, 1], op=ALU.subtract), rw, [b_Sf])
            dv(lambda v: v.tensor_tensor(out=cz[:, 2], in0=cz[:, 2], in1=cz[:, 3], op=ALU.add), rw, [b_Sf])
            dv(lambda v: v.tensor_tensor(out=Sf[:, 0, :, 0], in0=Sf[:, 0, :, 0], in1=cz[:, 0], op=ALU.add), rw, [b_Sf])
            dv(lambda v: v.tensor_tensor(out=Sf[:, 1, :, 0], in0=Sf[:, 1, :, 0], in1=cz[:, 2], op=ALU.add), rw, [b_Sf])
            yield 1.0
            dv(lambda v: v.tensor_tensor(out=Tf[:, 0], in0=Sf[:, 0], in1=Ere[:], op=ALU.mult), [b_Sf, b_tab], [b_Tf])
            dv(lambda v: v.tensor_tensor(out=Tf[:, 1], in0=Sf[:, 1], in1=Eim[:], op=ALU.mult), [b_Sf, b_tab], [b_Tf])
            dv(lambda v: v.tensor_tensor(out=Xf[:, 0], in0=Tf[:, 0], in1=Tf[:, 1], op=ALU.add), [b_Tf], [b_Xf])
            yield 1.0
            dv(lambda v: v.tensor_tensor(out=Tf[:, 0], in0=Sf[:, 1], in1=Ere[:], op=ALU.mult), [b_Sf, b_tab, b_Xf], [b_Tf])
            dv(lambda v: v.tensor_tensor(out=Tf[:, 1], in0=Sf[:, 0], in1=Eim[:], op=ALU.mult), [b_Sf, b_tab], [b_Tf])
            dv(lambda v: v.tensor_tensor(out=Xf[:, 1], in0=Tf[:, 0], in1=Tf[:, 1], op=ALU.subtract), [b_Tf], [b_Xf])
            yield 1.0
            Rflat = Rtab[:].rearrange("p g k -> p (g k)")
            for ri in range(2):
                dv(lambda v: v.tensor_tensor_scan(
                    out=Zf[:, ri].rearrange("p g k -> p (g k)"), data0=Rflat,
                    data1=Xf[:, ri].rearrange("p g k -> p (g k)"), initial=0.0, op0=ALU.mult, op1=ALU.add),
                    [b_Xf, b_tab], [b_Zf])
                yield 1.0
            dv(lambda v: v.tensor_tensor(out=Tf[:, 0], in0=Zf[:, 0], in1=Ere[:], op=ALU.mult), [b_Zf, b_tab], [b_Tf])
            dv(lambda v: v.tensor_tensor(out=Tf[:, 1], in0=Zf[:, 1], in1=Eim[:], op=ALU.mult), [b_Zf, b_tab], [b_Tf])
            dv(lambda v: v.tensor_tensor(out=Hf[:, 0, :, 1:NK + 1], in0=Tf[:, 0], in1=Tf[:, 1], op=ALU.subtract), [b_Tf], [b_Hf])
            yield 1.0
            dv(lambda v: v.tensor_tensor(out=Tf[:, 0], in0=Zf[:, 0], in1=Eim[:], op=ALU.mult), [b_Zf, b_tab, b_Hf], [b_Tf])
            dv(lambda v: v.tensor_tensor(out=Tf[:, 1], in0=Zf[:, 1], in1=Ere[:], op=ALU.mult), [b_Zf, b_tab], [b_Tf])
            dv(lambda v: v.tensor_tensor(out=Hf[:, 1, :, 1:NK + 1], in0=Tf[:, 0], in1=Tf[:, 1], op=ALU.add), [b_Tf], [b_Hf])
            yield 1.0
            S.op("dve", lambda v: v.tensor_copy(out=Hb[:], in_=Hf[:, :, :, 0:NK]), reads=[b_Hf], writes=[b_Hb])
            yield 1.0

        for j in range(8):
            slab, bslab = load_slab(t, 0, 8 + j)
            for m in range(2):
                ci = j * 2 + m
                pg, pgb, _ = proj_fm(slab, bslab, m * 128, hT, b_hT)
                S.op("act", lambda a: a.activation(
                    out=gateT[:, ci, :], in_=pg[:, :TT], func=AF.Tanh, bias=hbias[:, ci:ci + 1], scale=0.5),
                    reads=[pgb, b_par], writes=[b_gate])
                yield 1.2
        if stage < 3:
            return

        for blk in range(NB):
            m_abs = seq_t * NB + blk
            kbs = [kb for kb in range(5) if m_abs - 4 + kb >= 0]
            for hg in range(2):
                po, pob, poi = MP.get(hold=True)
                po4 = po[:, 0:260].rearrange("p (h d) -> p h d", h=4)
                pend = []
                first = [True]

                def pv_step(ip, ks, kblk):
                    fns = []
                    for j in range(4):
                        h = 2 * j + hg
                        fns.append(lambda p, j=j, h=h, st=(first[0] and j == 0): p.matmul(
                            po[:, j * 65:(j + 1) * 65], PT[ip][:, j * 128:(j + 1) * 128],
                            Vaug[ks][:, kblk, h, :], start=st, stop=True, skip_group_check=True))
                    S.group("pe", fns, reads=[b_PT[ip], b_V[ks]], writes=[pob])
                    first[0] = False
                for kb in kbs:
                    ab = m_abs - 4 + kb
                    kt_ = (t - seq_t) + ab // NB
                    ks = kt_ % NSLOT
                    kblk = ab % NB
                    pst, psb, _ = MP.get()
                    fns = []
                    for j in range(4):
                        h = 2 * j + hg
                        mchunk, hb = h // 2, 64 * (h % 2)
                        fns.append(lambda p, j=j, mchunk=mchunk, hb=hb: p.matmul(
                            pst[:, j * 128:(j + 1) * 128],
                            kT[ks][hb:hb + 64, mchunk, kblk * 128:(kblk + 1) * 128],
                            qT[hb:hb + 64, mchunk, blk * 128:(blk + 1) * 128], start=True, stop=True))
                    S.group("pe", fns, reads=[b_kT[ks], b_qT], writes=[psb])
                    ia = att_ctr[0] % 2
                    att_ctr[0] += 1
                    S.op("dve", lambda v: v.tensor_tensor(
                        out=att_tmp[ia][:].rearrange("p (h q) -> p h q", h=4),
                        in0=pst[:, :].rearrange("p (h q) -> p h q", h=4),
                        in1=biasT4[:, kb, :, :].rearrange("p (j two) q -> p j two q", two=2)[:, :, hg, :], op=ALU.add),
                        reads=[psb, b_bias], writes=[b_att_tmp[ia]])
                    ip = att_ctr[1] % 3
                    att_ctr[1] += 1
                    S.op("act", lambda a: a.activation(out=PT[ip][:], in_=att_tmp[ia][:], func=AF.Exp),
                         reads=[b_att_tmp[ia]], writes=[b_PT[ip]])
                    yield 0.4
                    if len(pend) >= 2:
                        pv_step(*pend.pop(0))
                    pend.append((ip, ks, kblk))
                while pend:
                    pv_step(*pend.pop(0))
                    yield 0.3
                S.op("dve", lambda v: v.reciprocal(out=rden[:, hg, :], in_=po4[:, :, 64]),
                     reads=[pob], writes=[b_rden[hg]])
                S.op("dve", lambda v: v.tensor_tensor(
                    out=yatt[:, blk, :].rearrange("p (j two d) -> p j two d", two=2, d=64)[:, :, hg, :],
                    in0=po4[:, :, 0:64],
                    in1=rden[:, hg, :].rearrange("p (h o) -> p h o", o=1).to_broadcast([128, 4, 64]),
                    op=ALU.mult), reads=[pob, b_rden[hg]], writes=[b_yatt[blk]])
                MP.release(poi)
            pt, ptb, _ = MP.get()
            ptv = pt[:].bitcast(BF16)
            S.group("pe", [lambda p, c=c: p.transpose(
                ptv[:, c * 128:(c + 1) * 128], yatt[:, blk, c * 128:(c + 1) * 128], identb[:]) for c in range(4)],
                reads=[b_yatt[blk], b_par], writes=[ptb])
            S.op("act", lambda a: a.activation(
                out=yaT[:, :, blk * 128:(blk + 1) * 128], in_=ptv[:, 0:512].rearrange("p (c t) -> p c t", c=4),
                func=AF.Copy), reads=[ptb], writes=[b_yaT])
            yield 0.3
        dump("ya", yaT[:], [b_yaT], dslice("ya", 512, tok0))
        if stage < 4:
            return

        for g0 in range(0, 32, 4):
            py, pyb, _ = MP.get()
            fns = []
            for gi in range(4):
                g = g0 + gi
                gp = g // 2
                o = py[0:NK, gi * 128:(gi + 1) * 128]
                fns.append(lambda p, o=o, g=g, st=(gi == 0): p.matmul(o, Ush[:, g, :], T_all[:, g, :], start=st, stop=False,
                                                                      skip_group_check=True))
                fns.append(lambda p, o=o, gp=gp, g=g: p.matmul(o, Hb[:, 0, gp, :], MCre[:, g, :],
                                                              start=False, stop=False, skip_group_check=True))
                fns.append(lambda p, o=o, gp=gp, g=g: p.matmul(o, Hb[:, 1, gp, :], MCim[:, g, :],
                                                              start=False, stop=True, skip_group_check=True))
            S.group("pe", fns, reads=[b_Ush, b_Hb, b_tab], writes=[pyb])
            S.op("act", lambda a: a.activation(
                out=Utok[:, :, g0 * 16:(g0 + 4) * 16].rearrange("k t (g c) -> k t g c", g=4),
                in_=py[0:NK, :].rearrange("k (g t c) -> k t g c", g=4, t=8), func=AF.Gelu_apprx_tanh),
                reads=[pyb, b_Ush], writes=[b_Utok])
            yield 0.8
        pt, ptb, _ = MP.get()
        ptz = pt[:].bitcast(BF16)[:, 0:4 * TT].rearrange("p (c t k) -> p c t k", c=4, t=8)
        S.group("pe", [lambda p, cc=cc, tau=tau: p.transpose(
            ptz[:, cc, tau, :], Utok[:, tau, cc * 128:(cc + 1) * 128], identb[0:NK, 0:NK])
            for cc in range(4) for tau in range(8)], reads=[b_Utok, b_par], writes=[ptb])
        S.op("act", lambda a: a.activation(out=zT[:].rearrange("p c (k t) -> p c t k", t=8), in_=ptz, func=AF.Copy, scale=0.5),
             reads=[ptb], writes=[b_zT])
        yield 2.0
        dump("z", zT[:], [b_zT], dslice("z", 512, tok0))
        for m in range(4):
            pg, pgb, _ = MP.get()
            S.group("pe", [lambda p, c=c: p.matmul(pg[:, :TT], wglu[:, c, m * 128:(m + 1) * 128], zT[:, c, :],
                                                   start=(c == 0), stop=(c == 3)) for c in range(4)],
                    reads=[b_wres, b_zT], writes=[pgb])
            S.op("act", lambda a: a.activation(out=sig[m % 2][:], in_=pg[:, :TT], func=AF.Tanh,
                                               bias=hbias[:, 16 + m:17 + m], scale=1.0),
                 reads=[pgb, b_par], writes=[b_sig[m % 2]])
            S.op("dve", lambda v: v.scalar_tensor_tensor(out=ysT[:, m, :], in0=sig[m % 2][:], scalar=1.0, in1=zT[:, m, :],
                                                         op0=ALU.add, op1=ALU.mult),
                 reads=[b_zT, b_sig[m % 2]], writes=[b_ysT])
            yield 0.6
        dump("ys", ysT[:], [b_ysT], dslice("ys", 512, tok0))
        if stage < 5:
            return

        for fc in range(8):
            if fc % 2 == 0:
                wbr, b_wbr = load_slab(t, 1, fc // 2)
            fo = (fc % 2) * 128
            pa, pab, _ = MP.get()
            S.group("pe", [lambda p, c=c: p.matmul(pa[:, :TT], wbr[:, c, fo:fo + 128], yaT[:, c, :],
                                                   start=(c == 0), stop=(c == 3)) for c in range(4)],
                    reads=[b_wbr, b_yaT], writes=[pab])
            pb, pbb, _ = MP.get()
            S.group("pe", [lambda p, c=c: p.matmul(pb[:, :TT], wbr[:, 4 + c, fo:fo + 128], ysT[:, c, :],
                                                   start=(c == 0), stop=(c == 3)) for c in range(4)],
                    reads=[b_wbr, b_ysT], writes=[pbb])
            S.op("dve", lambda v: v.scalar_tensor_tensor(out=m12[0][:], in0=gateT[:, fc, :], scalar=1.0, in1=pa[:, :TT],
                                                         op0=ALU.add, op1=ALU.mult),
                 reads=[pab, b_gate], writes=[b_m12[0]])
            S.op("dve", lambda v: v.scalar_tensor_tensor(out=m12[1][:], in0=gateT[:, 8 + fc, :], scalar=1.0, in1=pb[:, :TT],
                                                         op0=ALU.add, op1=ALU.mult),
                 reads=[pbb, b_gate], writes=[b_m12[1]])
            S.op("dve", lambda g_: g_.tensor_tensor(out=mT[:, fc, :], in0=m12[0][:], in1=m12[1][:], op=ALU.add),
                 reads=[b_m12[0], b_m12[1]], writes=[b_mT])
            yield 1.2
        for fc in range(8):
            if fc % 2 == 0:
                wout, b_wout = load_slab(t, 2, fc // 2)
            fo = (fc % 2) * 128
            po, pob, _ = MP.get()
            S.group("pe", [lambda p, c=c: p.matmul(po[:, :TT], wout[:, c, fo:fo + 128], mT[:, c, :],
                                                   start=(c == 0), stop=(c == 7)) for c in range(8)],
                    reads=[b_wout, b_mT], writes=[pob])
            S.op("dve", lambda v: v.scalar_tensor_tensor(out=x_sb[:, fc, :], in0=po[:, :TT], scalar=0.5, in1=x_sb[:, fc, :],
                                                         op0=ALU.mult, op1=ALU.add),
                 reads=[pob, b_x[fc]], writes=[b_x[fc]])
            yield 1.2
        dump("x1", x_sb[:], b_x, dslice("x1", 1024, tok0))

    def store_x(t):
        tok0 = t * TT
        x_sb, b_x = xs[t % 2], b_xs[t % 2]
        for c in range(8):
            S.dma("sp", lambda q: q.dma_start(out=outT_d[c * 128:(c + 1) * 128, tok0:tok0 + TT], in_=x_sb[:, c, :]),
                  b_x[c], reads=[b_x[c]])

    def moe(t):
        tok0 = t * TT
        x_sb, b_x = xs[t % 2], b_xs[t % 2]
        if stage < 6:
            store_x(t)
            return
        yield from rmsnorm(EP, x_sb, b_x, g2_sb, h2T, b_h2T)
        pr_, prb, _ = EP.get()
        prv = pr_[:, 0:NB * 20].rearrange("p (b j) -> p b j", j=20)
        for blk in range(NB):
            S.group("pe", [lambda p, c=c: p.matmul(prv[:, blk, :], h2T[:, c, blk * 128:(blk + 1) * 128], wr[:, c, :],
                                                   start=(c == 0), stop=(c == 7)) for c in range(8)],
                    reads=[b_h2T, b_wres], writes=[prb])
        R = [b_rt, b_par]

        def rv(fn, reads=R, writes=(b_rt,)):
            S.op("dve", fn, reads=list(reads), writes=list(writes))
        rv(lambda v: v.tensor_tensor(out=Lr[:], in0=prv, in1=rbias_sb.rearrange("p (o j) -> p o j", o=1).to_broadcast([128, NB, 20]),
                                     op=ALU.add), reads=[prb, b_par, b_rt])
        G = Lr[:, :, 0:4]
        E4 = Lr[:, :, 4:20].rearrange("p b (g j) -> p b g j", g=4)
        gmax, gsum, gprob, m1, m2, dd, w1, w2 = [rt[:, i, :, 0:1] for i in range(8)]
        gmask, ge, ing, x2 = [rt[:, 8 + i] for i in range(4)]
        mask1 = rt16[:, :, 0:4]
        mask2 = rt16[:, :, 4:8]
        wj = rt16[:, :, 8:12]
        tj = rt16[:, :, 12:16]

        def b4(x):
            return x.to_broadcast([128, NB, 4])
        rv(lambda v: v.tensor_reduce(out=gmax, in_=G, axis=AX.X, op=ALU.max))
        rv(lambda v: v.tensor_tensor(out=ge, in0=G, in1=b4(gmax), op=ALU.subtract))
        S.op("act", lambda a: a.activation(out=ge, in_=ge, func=AF.Exp), reads=R, writes=[b_rt])
        rv(lambda v: v.tensor_reduce(out=gsum, in_=ge, axis=AX.X, op=ALU.add))
        rv(lambda v: v.reciprocal(out=gprob, in_=gsum))
        rv(lambda v: v.tensor_tensor(out=gmask, in0=G, in1=b4(gmax), op=ALU.is_equal))
        sel4 = gates[:].rearrange("p b (g j) -> p b g j", g=4)
        rv(lambda v: v.tensor_tensor(out=sel4, in0=E4, in1=gmask.rearrange("p b (g o) -> p b g o", o=1).to_broadcast([128, NB, 4, 4]),
                                     op=ALU.mult))
        rv(lambda v: v.tensor_reduce(out=ing, in_=sel4.rearrange("p b g j -> p b j g"), axis=AX.X, op=ALU.add))
        rv(lambda v: v.tensor_reduce(out=m1, in_=ing, axis=AX.X, op=ALU.max))
        rv(lambda v: v.tensor_tensor(out=mask1, in0=ing, in1=b4(m1), op=ALU.is_equal))
        rv(lambda v: v.scalar_tensor_tensor(out=x2, in0=mask1, scalar=-1e30, in1=ing, op0=ALU.mult, op1=ALU.add))
        rv(lambda v: v.tensor_reduce(out=m2, in_=x2, axis=AX.X, op=ALU.max))
        rv(lambda v: v.tensor_tensor(out=mask2, in0=x2, in1=b4(m2), op=ALU.is_equal))
        rv(lambda v: v.tensor_tensor(out=dd, in0=m2, in1=m1, op=ALU.subtract))
        S.op("act", lambda a: a.activation(out=dd, in_=dd, func=AF.Exp), reads=R, writes=[b_rt])
        rv(lambda v: v.tensor_scalar(out=w1, in0=dd, scalar1=1.0, scalar2=None, op0=ALU.add))
        rv(lambda v: v.reciprocal(out=w1, in_=w1))
        rv(lambda v: v.tensor_tensor(out=w2, in0=dd, in1=w1, op=ALU.mult))
        rv(lambda v: v.tensor_tensor(out=w1, in0=w1, in1=gprob, op=ALU.mult))
        rv(lambda v: v.tensor_tensor(out=w2, in0=w2, in1=gprob, op=ALU.mult))
        rv(lambda v: v.tensor_tensor(out=wj, in0=mask1, in1=b4(w1), op=ALU.mult))
        rv(lambda v: v.tensor_tensor(out=tj, in0=mask2, in1=b4(w2), op=ALU.mult))
        rv(lambda v: v.tensor_tensor(out=wj, in0=wj, in1=tj, op=ALU.add))
        rv(lambda v: v.tensor_tensor(out=sel4, in0=gmask.rearrange("p b (g o) -> p b g o", o=1).to_broadcast([128, NB, 4, 4]),
                                     in1=wj.rearrange("p b (o j) -> p b o j", o=1).to_broadcast([128, NB, 4, 4]), op=ALU.mult))
        yield 12.0
        pgt, pgtb, _ = EP.get()
        S.group("pe", [lambda p, blk=blk: p.transpose(pgt[0:16, blk * 128:(blk + 1) * 128], gates[:, blk, :], identf)
                       for blk in range(NB)], reads=[b_rt, b_par], writes=[pgtb])
        S.op("act", lambda a: a.activation(out=gatesT[:], in_=pgt[0:16, 0:TT], func=AF.Copy), reads=[pgtb], writes=[b_gatesT])
        if "gates" in dbg_d:
            dump("gates", gatesT[:], [b_gatesT], dbg_d["gates"][0:16, tok0:tok0 + TT])
        yield 3.0
        if stage < 7:
            store_x(t)
            return

        for i_ in range(NWD):
            wd_prefetch(t, i_)
        for i_ in range(NGU):
            wgu_prefetch(t, i_)

        def emit_gm(e):
            if e < NE:
                S.op("dve", lambda v: v.tensor_scalar(out=gm[e % 3][:], in0=gatesT[:], scalar1=identf[0:16, e:e + 1],
                                                      scalar2=0.5, op0=ALU.mult, op1=ALU.mult),
                     reads=[b_gatesT, b_par], writes=[b_gm[e % 3]])

        def emit_bcast(e):
            if e < NE:
                pgb_, pgbb, _ = EP.get()
                S.group("pe", [lambda p: p.matmul(pgb_[:, :TT], ones_bf[0:16, :], gm[e % 3][:], start=True, stop=True)],
                        reads=[b_gm[e % 3], b_par], writes=[pgbb])
                S.op("act", lambda a: a.activation(out=gbs[e % 2][:], in_=pgb_[:, :TT], func=AF.Copy),
                     reads=[pgbb], writes=[b_gbs[e % 2]])
        emit_gm(0)
        emit_gm(1)
        emit_bcast(0)
        for hf in range(4):
            for e in range(4 * hf, 4 * hf + 4):
                emit_gm(e + 2)
                for fh in range(2):
                    if fh == 1:
                        emit_bcast(e + 1)
                    kc = 2 * (e - 4 * hf) + fh
                    w_, bw_ = wgu_live.pop((t, 2 * e + fh))
                    pa, pab, _ = EP.get()
                    S.group("pe", [lambda p, c=c: p.matmul(pa[:, :TT], w_[:, 0, c, :], h2T[:, c, :], start=(c == 0), stop=(c == 7))
                                   for c in range(8)], reads=[bw_, b_h2T], writes=[pab])
                    pu, pub, _ = EP.get()
                    S.group("pe", [lambda p, c=c: p.matmul(pu[:, :TT], w_[:, 1, c, :], h2T[:, c, :], start=(c == 0), stop=(c == 7))
                                   for c in range(8)], reads=[bw_, b_h2T], writes=[pub])
                    i2 = kc % 2
                    S.op("act", lambda a: a.activation(out=sil[i2][:], in_=pa[:, :TT], func=AF.Tanh, scale=0.5),
                         reads=[pab], writes=[b_sil[i2]])
                    S.op("dve", lambda v: v.scalar_tensor_tensor(out=s2b[i2][:], in0=sil[i2][:], scalar=1.0, in1=pa[:, :TT],
                                                                 op0=ALU.add, op1=ALU.mult),
                         reads=[b_sil[i2], pab], writes=[b_s2[i2]])
                    S.op("dve", lambda v: v.tensor_tensor(out=gsb[i2][:], in0=s2b[i2][:], in1=gbs[e % 2][:], op=ALU.mult),
                         reads=[b_s2[i2], b_gbs[e % 2]], writes=[b_gs[i2]])
                    S.op("dve", lambda v: v.tensor_tensor(out=hid[:, kc, :], in0=gsb[i2][:], in1=pu[:, :TT], op=ALU.mult),
                         reads=[b_gs[i2], pub], writes=[b_hid[kc]])
                    wgu_prefetch(t, 2 * e + fh + NGU)
                    yield 2.3
            for fc in range(8):
                w_, bw_ = wd_live.pop((t, 8 * hf + fc))
                pd, pdb, _ = EP.get()
                S.group("pe", [lambda p, kc=kc: p.matmul(pd[:, :TT], w_[:, kc, :], hid[:, kc, :],
                                                         start=(kc == 0), stop=(kc == 7)) for kc in range(8)],
                        reads=[bw_] + b_hid, writes=[pdb])
                S.op("dve", lambda v: v.tensor_tensor(out=x_sb[:, fc, :], in0=pd[:, :TT], in1=x_sb[:, fc, :], op=ALU.add),
                     reads=[pdb, b_x[fc]], writes=[b_x[fc]])
                wd_prefetch(t, 8 * hf + fc + NWD)
                if hf == 3:
                    S.dma("sp", lambda q: q.dma_start(out=outT_d[fc * 128:(fc + 1) * 128, tok0:tok0 + TT], in_=x_sb[:, fc, :]),
                          b_x[fc], reads=[b_x[fc]])
                yield 1.15

    def run_interleaved(ga, gb):
        wa = wb_ = 0.0
        a_live, b_live = ga is not None, gb is not None
        if a_live and b_live:
            next(gb)
            wb_ = TUNE["head"]
        while a_live or b_live:
            if a_live and (not b_live or wa <= wb_):
                try:
                    wa += next(ga)
                except StopIteration:
                    a_live = False
            else:
                try:
                    wb_ += next(gb) * TUNE["mix_scale"]
                except StopIteration:
                    b_live = False

    ntl = NT if ntiles is None else ntiles
    run_interleaved(mixer(0), None)
    for t in range(ntl):
        run_interleaved(moe(t), mixer(t + 1) if t + 1 < ntl else None)

    for i in range(2):
        for c in range(8):
            for ev in list(b_xs[i][c].r.values()):
                S.wait_event("sp", ev)
    for bt in dbg_final:
        for ev in list(bt.r.values()):
            S.wait_event("sp", ev)
    if debug:
        print("ins per engine", S.nins, "counts", S.cnt)
    S.emit()
    es.close()
    return nc


def _consts():
    identf = np.eye(128, dtype=np.float32)
    s_idx = np.arange(128) // 16
    maskLT = (s_idx[None, :] >= s_idx[:, None]).astype(np.float32)
    hb = np.arange(128) // 64
    bd64 = (hb[:, None] == hb[None, :]).astype(np.float32) / 64.0
    return np.ascontiguousarray(np.concatenate([identf, maskLT, bd64], axis=1))


def _bias_index():
    k = np.arange(128)[:, None, None]
    kb = np.arange(5)[None, :, None]
    q = np.arange(128)[None, None, :]
    qpos = 512 + q
    kpos = kb * 128 + k
    idx = np.clip(qpos - kpos, -63, 256) + 63
    qchunk = qpos // 64
    kchunk = kpos // 64
    valid = (kchunk <= qchunk) & (kchunk >= qchunk - 8)
    return idx, valid


def prepare_inputs(inputs):
    f = lambda a: np.ascontiguousarray(np.asarray(a, dtype=np.float32))
    x = f(inputs["x"])
    L = 0
    vec = np.zeros((128, 64), np.float32)
    vec[:, 0:8] = f(inputs["mix_norm_gain"])[L].reshape(8, 128).T
    vec[:, 8:16] = f(inputs["ffn_norm_gain"])[L].reshape(8, 128).T
    vec[:, 16:32] = f(inputs["b_gate"])[L].reshape(16, 128).T
    vec[:, 32:36] = f(inputs["b_glu"])[L].reshape(4, 128).T
    vec[:, 36] = np.tile(f(inputs["q_gain"])[L], 2)
    vec[:, 37] = np.tile(f(inputs["k_gain"])[L], 2)
    vec[:, 40:44] = f(inputs["group_bias"])[L][None, :]
    vec[:, 44:60] = f(inputs["expert_bias"])[L][None, :]
    idx, valid = _bias_index()
    rb = f(inputs["rel_bias"])[L]
    bt = rb[:, idx]
    bt = np.where(valid[None], bt, np.float32(NEG)).astype(np.float32)
    biasT = np.ascontiguousarray(bt.transpose(1, 2, 0, 3)).reshape(128, 5 * 8 * 128)

    def gp_layout(a):
        return a.reshape(16, 2, 64).transpose(1, 2, 0).reshape(128, 16)
    small = np.zeros((128, 48), np.float32)
    small[:, 0:16] = gp_layout(f(inputs["ssm_lambda_re"])[L])
    small[:, 16:32] = gp_layout(f(inputs["ssm_lambda_im"])[L])
    small[:, 32:48] = gp_layout(np.broadcast_to(f(inputs["ssm_log_step"])[L][:, None], (32, 64)))
    bc = np.zeros((128, 4, 256), np.float32)
    for i, nm in enumerate(("ssm_b_re", "ssm_b_im")):
        a = f(inputs[nm])[L].reshape(16, 2, 64, 16).transpose(1, 2, 0, 3).reshape(128, 256)
        bc[:, i] = a
    for i, nm in enumerate(("ssm_c_re", "ssm_c_im")):
        a = f(inputs[nm])[L].reshape(16, 2, 16, 64).transpose(1, 3, 0, 2).reshape(128, 256)
        bc[:, 2 + i] = a
    drep = np.ascontiguousarray(np.tile(f(inputs["ssm_d"])[L].reshape(32, 16).T, (8, 1)))
    w_r = np.ascontiguousarray(np.concatenate([f(inputs["w_group_router"])[L], f(inputs["w_expert_router"])[L]], axis=1))
    common = {
        "w_in": f(inputs["w_in"])[L], "w_glu": f(inputs["w_glu"])[L], "w_branch": f(inputs["w_branch"])[L],
        "w_out": f(inputs["w_out"])[L], "w_e_gate": f(inputs["w_e_gate"])[L], "w_e_up": f(inputs["w_e_up"])[L],
        "w_e_down": f(inputs["w_e_down"])[L], "w_r": w_r, "vecs": vec, "biasT": biasT, "ssm_small": small,
        "ssm_bc": bc, "drep": drep, "consts": _consts(),
    }
    xs = x.reshape(NCORE, TOK, D)
    in_maps = []
    for i in range(NCORE):
        m = dict(common)
        m["xT"] = np.ascontiguousarray(xs[i].T)
        in_maps.append(m)
    return in_maps


_CACHE = {}


def kernel(**inputs):
    in_maps = prepare_inputs(inputs)
    if "nc" not in _CACHE:
        _CACHE["nc"] = build_program(TT=256)
    res = run_bass_kernel_spmd(_CACHE["nc"], in_maps, core_ids=list(range(NCORE)))
    out = np.stack([np.asarray(r["outT"]).T for r in res.results], axis=0)
    return np.ascontiguousarray(out.reshape(16, SEQ, D).astype(np.float32))
```

```python
import json
import math
import os
from contextlib import ExitStack

import numpy as np
import concourse.bass as bass
import concourse.mybir as mybir
from concourse.bass_utils import run_bass_kernel_spmd

F32 = mybir.dt.float32
BF16 = mybir.dt.bfloat16
I32 = mybir.dt.int32
AF = mybir.ActivationFunctionType
ALU = mybir.AluOpType
AX = mybir.AxisListType

D = 1024
SEQ = 2048
NCORE = 8
TOK = 4096
NH = 8
DH = 64
DA = 512
DS = 512
DIN = 4096
NE = 16
FE = 256
EPS = 1e-6
NEG = -30000.0


class Buf:
    __slots__ = ("name", "w", "r", "dsem", "dcnt")

    def __init__(self, name):
        self.name = name
        self.w = None
        self.r = {}
        self.dsem = None
        self.dcnt = 0


class Sched:
    ENG = ("pe", "act", "dve", "pool", "sp")

    def __init__(self, nc, es):
        self.nc = nc
        self.es = es
        self.sem = {e: es.enter_context(nc.semaphore("sem_" + e)) for e in self.ENG}
        self.cnt = {e: 0 for e in self.ENG}
        self.known = {e: {} for e in self.ENG}
        self.nins = {e: 0 for e in self.ENG}
        self.engines = {"pe": nc.tensor, "act": nc.scalar, "dve": nc.vector, "pool": nc.gpsimd, "sp": nc.sync}
        self.semname = {}
        self.nsem = 0

    def _key(self, sem):
        return id(sem)

    def _need(self, e, ev, waits):
        if ev is None:
            return
        k = self.known[e]
        key = self._key(ev[0])
        if k.get(key, 0) >= ev[1]:
            return
        k[key] = ev[1]
        waits.append(ev)

    def _waits(self, e, reads, writes):
        waits = []
        for b in reads:
            self._need(e, b.w, waits)
        for b in writes:
            self._need(e, b.w, waits)
            for ev in b.r.values():
                self._need(e, ev, waits)
        return waits

    def _commit(self, ev, reads, writes):
        key = self._key(ev[0])
        for b in reads:
            old = b.r.get(key)
            if old is None or old[1] < ev[1]:
                b.r[key] = ev
        for b in writes:
            b.w = ev
            b.r = {}

    def _emit(self, e, waits, fn, ev, inc):
        engine = self.engines[e]
        for (s_, v) in waits:
            engine.wait_ge(s_, v)
        self.nins[e] += 1
        if fn is None:
            return
        ins = fn(engine)
        if ev is not None:
            ins.then_inc(ev[0], inc)

    def op(self, e, fn, reads=(), writes=()):
        waits = self._waits(e, reads, writes)
        self.cnt[e] += 1
        ev = (self.sem[e], self.cnt[e])
        self._emit(e, waits, fn, ev, 1)
        self._commit(ev, reads, writes)
        return ev

    def group(self, e, fns, reads=(), writes=()):
        waits = self._waits(e, reads, writes)
        self.cnt[e] += 1
        ev = (self.sem[e], self.cnt[e])
        n = len(fns)
        for i, fn in enumerate(fns):
            self._emit(e, waits if i == 0 else [], fn, ev if i == n - 1 else None, 1)
        self._commit(ev, reads, writes)
        return ev

    def dma(self, e, fn, owner, reads=(), writes=()):
        if owner.dsem is None:
            owner.dsem = self.es.enter_context(self.nc.semaphore("dsem_%d" % self.nsem))
            self.nsem += 1
        waits = self._waits(e, reads, writes)
        owner.dcnt += 16
        ev = (owner.dsem, owner.dcnt)
        self._emit(e, waits, fn, ev, 16)
        self._commit(ev, reads, writes)
        return ev

    def wait_event(self, e, ev):
        waits = []
        self._need(e, ev, waits)
        if waits:
            self._emit(e, waits, None, None, 0)

    def barrier(self):
        last = {e: (self.sem[e], self.cnt[e]) for e in self.ENG if self.cnt[e] > 0}
        for e in self.ENG:
            waits = []
            for f, ev in last.items():
                if f != e:
                    self._need(e, ev, waits)
            if waits:
                self._emit(e, waits, None, None, 0)

    def emit(self):
        pass


TWO_PI = 2.0 * math.pi
TUNE = {"head": 14.0, "mix_scale": 0.8}
if os.environ.get("KTUNE"):
    TUNE.update(json.loads(os.environ["KTUNE"]))


def build_program(TT=256, debug=(), stage=99, ntiles=None):
    nc = bass.Bass("TRN2", target_bir_lowering=False)
    es = ExitStack()
    S = Sched(nc, es)
    NT = TOK // TT
    TPS = SEQ // TT
    NB = TT // 128
    NK = TT // 8
    NSLOT = 512 // TT + 1
    LV = int(math.log2(NK))

    def dram_in(name, shape, dt=F32):
        return nc.dram_tensor(name, list(shape), dt, kind="ExternalInput").ap()

    xT_d = dram_in("xT", [TOK // TT, 128, 8, TT])
    w_in_d = dram_in("w_in", [D, DIN])
    w_glu_d = dram_in("w_glu", [DS, DS])
    w_br_d = dram_in("w_branch", [D, D])
    w_out_d = dram_in("w_out", [D, D])
    weg_d = dram_in("w_e_gate", [NE, D, FE])
    weu_d = dram_in("w_e_up", [NE, D, FE])
    wed_d = dram_in("w_e_down", [NE, FE, D])
    w_r_d = dram_in("w_r", [D, 20])
    vec_d = dram_in("vecs", [128, 64])
    biasT_d = dram_in("biasT", [128, 5 * 8 * 128])
    ssm_d = dram_in("ssm_small", [128, 48])
    ssm_bc_d = dram_in("ssm_bc", [128, 4, 256])
    drep_d = dram_in("drep", [128, 32])
    cst_d = dram_in("consts", [128, 128 * 3])
    outT_d = nc.dram_tensor("outT", [TOK // TT, 128, 8, TT], F32, kind="ExternalOutput").ap()
    dbg_d = {}
    for name, shape in debug:
        dbg_d[name] = nc.dram_tensor("dbg_" + name, list(shape), F32, kind="ExternalOutput").ap()

    def sbt(stack, name, shape, dt=F32):
        return stack.enter_context(nc.sbuf_tensor("sb_" + name, list(shape), dt))

    def sb(name, shape, dt=F32):
        return sbt(es, name, shape, dt)

    PS = [es.enter_context(nc.psum_tensor("ps%d" % i, [128, 512], F32)) for i in range(8)]
    b_PS = [Buf("ps%d" % i) for i in range(8)]
    ps_ctr = [0]

    def next_ps():
        i = ps_ctr[0] % 8
        ps_ctr[0] += 1
        return PS[i], b_PS[i]

    w_in_s = nc.dram_tensor("w_in_bf", [16, 128, 8 * 256], BF16).ap()
    wgu_s = nc.dram_tensor("wgu_bf", [NE, 2, 128, 2, 8, 128], BF16).ap()
    wd_s = nc.dram_tensor("wd_bf", [4, 8, 128, 8, 128], BF16).ap()
    b_w_in_s = Buf("w_in_s")
    wbr_s = nc.dram_tensor("wbr_bf", [4, 128, 8 * 256], BF16).ap()
    wout_s = nc.dram_tensor("wout_bf", [4, 128, 8 * 256], BF16).ap()
    b_wbr_s = Buf("wbr_s")
    b_wout_s = Buf("wout_s")
    b_wgu_s = Buf("wgu_s")
    b_wd_s = Buf("wd_s")

    cst = sb("cst", [128, 128 * 3])
    identf = cst[:, 0:128]
    maskLT = cst[:, 128:256]
    bd64f = cst[:, 256:384]
    vec = sb("vec", [128, 64])
    b_par = Buf("params")
    S.dma("sp", lambda q: q.dma_start(out=cst[:], in_=cst_d), b_par, writes=[b_par])
    S.dma("sp", lambda q: q.dma_start(out=vec[:], in_=vec_d), b_par, writes=[b_par])
    g1_sb = vec[:, 0:8]
    g2_sb = vec[:, 8:16]
    bgate_sb = vec[:, 16:32]
    bglu_sb = vec[:, 32:36]
    rbias_sb = vec[:, 40:60]
    hbias = sb("hbias", [128, 20])
    S.op("dve", lambda v: v.tensor_scalar(out=hbias[:], in0=vec[:, 16:36], scalar1=0.5, scalar2=None, op0=ALU.mult),
         reads=[b_par], writes=[b_par])
    cq = sb("cq", [128, 1])
    S.op("dve", lambda v: v.tensor_scalar(out=cq[:], in0=vec[:, 36:37], scalar1=vec[:, 37:38],
                                          scalar2=0.125, op0=ALU.mult, op1=ALU.mult),
         reads=[b_par], writes=[b_par])
    ones_bf = sb("ones_bf", [128, 128], BF16)
    identb = sb("identb", [128, 128], BF16)
    bd64 = sb("bd64", [128, 128], BF16)
    ones16f = sb("ones16f", [16, 128])
    S.op("pool", lambda g: g.memset(ones_bf[:], 1.0), writes=[b_par])
    S.op("pool", lambda g: g.memset(ones16f[:], 1.0), writes=[b_par])
    S.op("dve", lambda v: v.tensor_copy(out=identb[:], in_=identf), reads=[b_par], writes=[b_par])
    S.op("dve", lambda v: v.tensor_copy(out=bd64[:], in_=bd64f), reads=[b_par], writes=[b_par])

    wglu = sb("wglu", [128, 4, DS], BF16)
    wr = sb("wr", [128, 8, 20], BF16)
    b_wres = Buf("wres")
    for (tl, src) in ((wglu, w_glu_d), (wr, w_r_d)):
        S.dma("pool", lambda q, tl=tl, src=src: q.dma_start(
            out=tl[:], in_=src.rearrange("(c p) f -> p c f", p=128)), b_wres, writes=[b_wres])

    biasT = sb("biasT", [128, 5 * 8 * 128], BF16)
    b_bias = Buf("biasT")
    S.dma("pool", lambda q: q.dma_start(out=biasT[:], in_=biasT_d), b_bias, writes=[b_bias])
    biasT4 = biasT[:].rearrange("p (kb h q) -> p kb h q", kb=5, h=8)

    T_all = sb("T_all", [128, 32, 128], BF16)
    MBre = sb("MBre", [128, 32, 64], BF16)
    MBim = sb("MBim", [128, 32, 64], BF16)
    MCre = sb("MCre", [128, 32, 128], BF16)
    MCim = sb("MCim", [128, 32, 128], BF16)
    Ere = sb("Ere", [128, 16, NK])
    Eim = sb("Eim", [128, 16, NK])
    Rtab = sb("Rtab", [128, 16, NK])
    A8 = sb("A8", [128, 2, 16])
    b_tab = Buf("ssm_tables")

    with ExitStack() as ps_:
        def tb(name, shape, dt=F32):
            return sbt(ps_, name, shape, dt)
        small = tb("ssm_small", [128, 48])
        bc = tb("ssm_bc", [128, 4, 16, 16])
        drep = tb("drep", [128, 32])
        b_p = Buf("prep")
        S.dma("sp", lambda q: q.dma_start(out=small[:], in_=ssm_d), b_p, writes=[b_p])
        S.dma("sp", lambda q: q.dma_start(out=bc[:], in_=ssm_bc_d.rearrange("p a (g c) -> p a g c", g=16)),
              b_p, writes=[b_p])
        S.dma("sp", lambda q: q.dma_start(out=drep[:], in_=drep_d), b_p, writes=[b_p])
        lre = small[:, 0:16]
        lim = small[:, 16:32]
        lst = small[:, 32:48]
        W = tb("wk", [128, 24, 16])
        (STEP, XR, MAG, MAGI, ANG, TQ, TF, RS, RC, SN, CS, ARE, AIM, IRE, IIM, NRE, DEN, RDEN,
         FRE, FIM, T1, T2, T3, T4) = [W[:, i, :] for i in range(24)]
        WI = tb("wki", [128, 16], I32)

        def P(eng, fn):
            S.op(eng, fn, reads=[b_p, b_par], writes=[b_p])

        def tt(out, a, b, op):
            P("dve", lambda v: v.tensor_tensor(out=out, in0=a, in1=b, op=op))

        def ts(out, a, s1, op0, s2=None, op1=None):
            if op1 is None:
                P("dve", lambda v: v.tensor_scalar(out=out, in0=a, scalar1=s1, scalar2=None, op0=op0))
            else:
                P("dve", lambda v: v.tensor_scalar(out=out, in0=a, scalar1=s1, scalar2=s2, op0=op0, op1=op1))

        def actf(out, a, func, scale=1.0, bias=0.0):
            P("act", lambda x: x.activation(out=out, in_=a, func=func, scale=scale, bias=bias))

        def cmul(ore, oim, xre, xim, yre, yim, t1, t2):
            tt(t1, xre, yre, ALU.mult)
            tt(t2, xim, yim, ALU.mult)
            tt(ore, t1, t2, ALU.subtract)
            tt(t1, xre, yim, ALU.mult)
            tt(t2, xim, yre, ALU.mult)
            tt(oim, t1, t2, ALU.add)

        actf(STEP, lst, AF.Exp)
        tt(XR, lre, STEP, ALU.mult)
        actf(MAG, XR, AF.Exp)
        actf(MAGI, XR, AF.Exp, scale=-1.0)
        tt(ANG, lim, STEP, ALU.mult)
        ts(TQ, ANG, 1.0 / TWO_PI, ALU.mult)
        P("dve", lambda v: v.tensor_copy(out=WI[:], in_=TQ))
        P("dve", lambda v: v.tensor_copy(out=TF, in_=WI[:]))
        P("dve", lambda v: v.scalar_tensor_tensor(out=RS, in0=TF, scalar=-TWO_PI, in1=ANG,
                                                  op0=ALU.mult, op1=ALU.add))

        def wrap(x):
            ts(T1, x, math.pi, ALU.is_gt, TWO_PI, ALU.mult)
            tt(x, x, T1, ALU.subtract)
            ts(T1, x, -math.pi, ALU.is_lt, TWO_PI, ALU.mult)
            tt(x, x, T1, ALU.add)

        wrap(RS)
        ts(RC, RS, math.pi / 2, ALU.add)
        wrap(RC)
        actf(SN, RS, AF.Sin)
        actf(CS, RC, AF.Sin)
        tt(ARE, MAG, CS, ALU.mult)
        tt(AIM, MAG, SN, ALU.mult)
        tt(IRE, MAGI, CS, ALU.mult)
        tt(T1, MAGI, SN, ALU.mult)
        ts(IIM, T1, -1.0, ALU.mult)
        ts(NRE, ARE, -1.0, ALU.add)
        tt(T1, lre, lre, ALU.mult)
        tt(T2, lim, lim, ALU.mult)
        tt(DEN, T1, T2, ALU.add)
        P("dve", lambda v: v.reciprocal(out=RDEN, in_=DEN))
        tt(T1, NRE, lre, ALU.mult)
        tt(T2, AIM, lim, ALU.mult)
        tt(T1, T1, T2, ALU.add)
        tt(FRE, T1, RDEN, ALU.mult)
        tt(T1, AIM, lre, ALU.mult)
        tt(T2, NRE, lim, ALU.mult)
        tt(T1, T1, T2, ALU.subtract)
        tt(FIM, T1, RDEN, ALU.mult)
        BB = tb("bb", [128, 2, 16, 16])
        TB = tb("tbb", [128, 2, 16, 16])
        bre_, bim_, cre_, cim_ = bc[:, 0], bc[:, 1], bc[:, 2], bc[:, 3]

        def bcg(x):
            return x.rearrange("p (g o) -> p g o", o=1).to_broadcast([128, 16, 16])

        tt(TB[:, 0], bre_, bcg(FRE), ALU.mult)
        tt(TB[:, 1], bim_, bcg(FIM), ALU.mult)
        tt(BB[:, 0], TB[:, 0], TB[:, 1], ALU.subtract)
        tt(TB[:, 0], bim_, bcg(FRE), ALU.mult)
        tt(TB[:, 1], bre_, bcg(FIM), ALU.mult)
        tt(BB[:, 1], TB[:, 0], TB[:, 1], ALU.add)
        Pre = tb("Pre", [128, 9, 16])
        Pim = tb("Pim", [128, 9, 16])
        Qre = tb("Qre", [128, 9, 16])
        Qim = tb("Qim", [128, 9, 16])
        for (pr, pi, xr_, xi_) in ((Pre, Pim, ARE, AIM), (Qre, Qim, IRE, IIM)):
            P("dve", lambda v, pr=pr: v.memset(pr[:, 0, :], 1.0))
            P("dve", lambda v, pi=pi: v.memset(pi[:, 0, :], 0.0))
            for d in range(1, 9):
                cmul(pr[:, d, :], pi[:, d, :], pr[:, d - 1, :], pi[:, d - 1, :], xr_, xi_, T1, T2)
        P("dve", lambda v: v.tensor_copy(out=A8[:, 0, :], in_=Pre[:, 8, :]))
        P("dve", lambda v: v.tensor_copy(out=A8[:, 1, :], in_=Pim[:, 8, :]))
        MCf = tb("MCf", [128, 2, 16, 128])
        XTf = tb("XTf", [128, 2, 16, 128])
        MBf = tb("MBf", [128, 2, 16, 128])
        TMP = tb("TMPf", [128, 2, 16, 16])

        def v4(x, i):
            return x[:, i].rearrange("p g (t c) -> p g t c", t=8)

        for tq in range(8):
            prb = bcg(Pre[:, tq + 1, :])
            pib = bcg(Pim[:, tq + 1, :])
            tt(TMP[:, 0], cre_, prb, ALU.mult)
            tt(TMP[:, 1], cim_, pib, ALU.mult)
            tt(v4(MCf, 0)[:, :, tq, :], TMP[:, 0], TMP[:, 1], ALU.subtract)
            tt(TMP[:, 0], cre_, pib, ALU.mult)
            tt(TMP[:, 1], cim_, prb, ALU.mult)
            tt(TMP[:, 0], TMP[:, 0], TMP[:, 1], ALU.add)
            ts(v4(MCf, 1)[:, :, tq, :], TMP[:, 0], -1.0, ALU.mult)
            for (dst, pr, pi, d) in ((MBf, Pre, Pim, 7 - tq), (XTf, Qre, Qim, tq + 1)):
                prb2 = bcg(pr[:, d, :])
                pib2 = bcg(pi[:, d, :])
                tt(TMP[:, 0], BB[:, 0], prb2, ALU.mult)
                tt(TMP[:, 1], BB[:, 1], pib2, ALU.mult)
                tt(v4(dst, 0)[:, :, tq, :], TMP[:, 0], TMP[:, 1], ALU.subtract)
                tt(TMP[:, 0], BB[:, 1], prb2, ALU.mult)
                tt(TMP[:, 1], BB[:, 0], pib2, ALU.mult)
                tt(v4(dst, 1)[:, :, tq, :], TMP[:, 0], TMP[:, 1], ALU.add)
        for g in range(32):
            for ri, MCt in enumerate((MCre, MCim)):
                S.op("dve", lambda v, g=g, ri=ri, MCt=MCt: v.tensor_scalar(
                    out=MCt[:, g, :], in0=MCf[:, ri, g // 2, :], scalar1=bd64f[:, 64 * (g % 2):64 * (g % 2) + 1],
                    scalar2=64.0, op0=ALU.mult, op1=ALU.mult), reads=[b_p, b_par], writes=[b_tab])
        TMPM = tb("TMPM", [128, 128])
        for g in range(32):
            gp, g2 = g // 2, g % 2
            lo, hi = 64 * g2, 64 * g2 + 64
            pst, psb = next_ps()
            S.group("pe", [
                lambda p, pst=pst, gp=gp, lo=lo, hi=hi: p.matmul(pst[:, 0:128], XTf[lo:hi, 0, gp, :], MCf[lo:hi, 0, gp, :],
                                                                 start=True, stop=False),
                lambda p, pst=pst, gp=gp, lo=lo, hi=hi: p.matmul(pst[:, 0:128], XTf[lo:hi, 1, gp, :], MCf[lo:hi, 1, gp, :],
                                                                 start=False, stop=True),
                lambda p, pst=pst, gp=gp, lo=lo, hi=hi: p.matmul(pst[:, 128:192], MBf[lo:hi, 0, gp, :], identf[lo:hi, lo:hi],
                                                                 start=True, stop=True),
                lambda p, pst=pst, gp=gp, lo=lo, hi=hi: p.matmul(pst[:, 192:256], MBf[lo:hi, 1, gp, :], identf[lo:hi, lo:hi],
                                                                 start=True, stop=True),
            ], reads=[b_p, b_par], writes=[psb])
            S.op("dve", lambda v, pst=pst: v.tensor_tensor(out=TMPM[:], in0=pst[:, 0:128], in1=maskLT, op=ALU.mult),
                 reads=[psb, b_par, b_p], writes=[b_p])
            S.op("dve", lambda v, g=g: v.scalar_tensor_tensor(out=T_all[:, g, :], in0=identf, scalar=drep[:, g:g + 1],
                                                             in1=TMPM[:], op0=ALU.mult, op1=ALU.add),
                 reads=[b_p, b_par], writes=[b_tab])
            S.op("act", lambda a, pst=pst, g=g: a.activation(out=MBre[:, g, :], in_=pst[:, 128:192], func=AF.Copy),
                 reads=[psb], writes=[b_tab])
            S.op("act", lambda a, pst=pst, g=g: a.activation(out=MBim[:, g, :], in_=pst[:, 192:256], func=AF.Copy),
                 reads=[psb], writes=[b_tab])
        M8I = T3
        actf(M8I, XR, AF.Exp, scale=-8.0)
        actf(T4, XR, AF.Exp, scale=8.0)
        P("dve", lambda v: v.tensor_copy(out=Rtab[:], in_=T4.rearrange("p (g o) -> p g o", o=1).to_broadcast([128, 16, NK])))
        P("dve", lambda v: v.memset(Rtab[:, :, 0:1], 0.0))
        tt(Ere[:, :, 0], Pre[:, 8, :], M8I, ALU.mult)
        tt(Eim[:, :, 0], Pim[:, 8, :], M8I, ALU.mult)
        ETr = tb("ETr", [128, 16, NK])
        ETi = tb("ETi", [128, 16, NK])
        m = 1
        while m < NK:
            ub_r = Ere[:, :, m - 1:m].to_broadcast([128, 16, m])
            ub_i = Eim[:, :, m - 1:m].to_broadcast([128, 16, m])
            tt(ETr[:, :, 0:m], Ere[:, :, 0:m], ub_r, ALU.mult)
            tt(ETi[:, :, 0:m], Eim[:, :, 0:m], ub_i, ALU.mult)
            tt(Ere[:, :, m:2 * m], ETr[:, :, 0:m], ETi[:, :, 0:m], ALU.subtract)
            tt(ETr[:, :, 0:m], Ere[:, :, 0:m], ub_i, ALU.mult)
            tt(ETi[:, :, 0:m], Eim[:, :, 0:m], ub_r, ALU.mult)
            tt(Eim[:, :, m:2 * m], ETr[:, :, 0:m], ETi[:, :, 0:m], ALU.add)
            m *= 2
        S.op("dve", lambda v: v.memset(TMPM[0:1, 0:1], 0.0), reads=[b_p], writes=[b_tab, b_p])
        S.barrier()

    xs = [sb("x_sb%d" % i, [128, 8, TT]) for i in range(2)]
    b_xs = [[Buf("x%d_%d" % (i, c)) for c in range(8)] for i in range(2)]
    sq_sb = [sb("sq%d" % i, [128, TT], BF16) for i in range(2)]
    b_sq = [Buf("sq%d" % i) for i in range(2)]
    hT = sb("hT", [128, 8, TT], BF16)
    b_hT = Buf("hT")
    h2T = sb("h2T", [128, 8, TT], BF16)
    b_h2T = Buf("h2T")
    ln_sb = [sb("ln%d" % i, [128, TT]) for i in range(2)]
    b_ln = [Buf("ln%d" % i) for i in range(2)]
    rstd = [sb("rstd%d" % i, [128, TT]) for i in range(2)]
    b_rstd = [Buf("rstd%d" % i) for i in range(2)]
    rs_ctr = [0]
    NSLAB = 3
    wslab = [sb("wslab%d" % i, [128, 8, 256], BF16) for i in range(NSLAB)]
    b_wslab = [Buf("wslab%d" % i) for i in range(NSLAB)]
    slab_ctr = [0]
    ms_all = sb("ms_all", [128, 8, TT], BF16)
    b_ms = Buf("ms_all")
    qT = sb("qT", [128, 4, TT], BF16)
    b_qT = Buf("qT")
    kT = [sb("kT%d" % i, [128, 4, TT], BF16) for i in range(NSLOT)]
    b_kT = [Buf("kT%d" % i) for i in range(NSLOT)]
    Vaug = [sb("Vaug%d" % i, [128, NB, NH, 65], BF16) for i in range(NSLOT)]
    b_V = [Buf("V%d" % i) for i in range(NSLOT)]
    for i in range(NSLOT):
        S.op("pool", lambda g, i=i: g.memset(Vaug[i][:], 1.0), writes=[b_V[i]])
    Utok = sb("Utok", [NK, 8, 512], BF16)
    b_Utok = Buf("Utok")
    Ush = sb("Ush", [128, 32, NK], BF16)
    b_Ush = Buf("Ush")
    gateT = sb("gateT", [128, 16, TT], BF16)
    b_gate = Buf("gateT")
    att_tmp = [sb("att_tmp%d" % i, [128, 512]) for i in range(2)]
    b_att_tmp = [Buf("att_tmp%d" % i) for i in range(2)]
    PT = [sb("PT%d" % i, [128, 512], BF16) for i in range(3)]
    b_PT = [Buf("PT%d" % i) for i in range(3)]
    att_ctr = [0, 0]
    rden = sb("rden", [128, 2, 4])
    b_rden = [Buf("rden0"), Buf("rden1")]
    yatt = sb("yatt", [128, NB, 512], BF16)
    b_yatt = [Buf("yatt%d" % i) for i in range(NB)]
    yaT = sb("yaT", [128, 4, TT], BF16)
    b_yaT = Buf("yaT")
    Sf = sb("Sf", [128, 2, 16, NK])
    b_Sf = Buf("Sf")
    Xf = sb("Xf", [128, 2, 16, NK])
    b_Xf = Buf("Xf")
    Zf, b_Zf = Sf, b_Sf
    Tf = sb("Tf", [128, 2, 16, NK])
    b_Tf = Buf("Tf")
    Hf = sb("Hf", [128, 2, 16, NK + 1])
    b_Hf = Buf("Hf")
    Hb = sb("Hb", [128, 2, 16, NK], BF16)
    b_Hb = Buf("Hb")
    cz = sb("cz", [128, 4, 16])
    zT = sb("zT", [128, 4, TT], BF16)
    b_zT = Buf("zT")
    sig = [sb("sig%d" % i, [128, TT], BF16) for i in range(2)]
    b_sig = [Buf("sig%d" % i) for i in range(2)]
    ysT = sb("ysT", [128, 4, TT], BF16)
    b_ysT = Buf("ysT")
    mT = sb("mT", [128, 8, TT], BF16)
    b_mT = Buf("mT")
    m12 = [sb("m12_%d" % i, [128, TT], BF16) for i in range(2)]
    b_m12 = [Buf("m12_%d" % i) for i in range(2)]
    Lr = sb("Lr", [128, NB, 20])
    rt = sb("rt", [128, 12, NB, 4])
    rt16 = sb("rt16", [128, NB, 16])
    gates = sb("gates", [128, NB, 16])
    b_rt = Buf("router")
    gatesT = sb("gatesT", [16, TT])
    b_gatesT = Buf("gatesT")
    gm = [sb("gm%d" % i, [16, TT], BF16) for i in range(3)]
    b_gm = [Buf("gm%d" % i) for i in range(3)]
    if stage < 7:
        dbgtmp = sb("dbgtmp", [128, 8 * TT])
        b_dbgtmp = Buf("dbgtmp")
    hid = sb("hid", [128, 8, TT] if stage >= 7 else [128, 2, 2], BF16)
    b_hid = [Buf("hid%d" % i) for i in range(8)]
    NGU = 4
    wgu = [sb("wgu%d" % i, [128, 2, 8, 128], BF16) for i in range(NGU)]
    b_wgu = [Buf("wgu%d" % i) for i in range(NGU)]
    NWD = 4
    wd = [sb("wd%d" % i, [128, 8, 128], BF16) for i in range(NWD)]
    b_wd = [Buf("wd%d" % i) for i in range(NWD)]
    sil = [sb("sil%d" % i, [128, TT], BF16) for i in range(2)]
    b_sil = [Buf("sil%d" % i) for i in range(2)]
    gsb = [sb("gs%d" % i, [128, TT], BF16) for i in range(2)]
    gbs = [sb("gbs%d" % i, [128, TT], BF16) for i in range(2)]
    b_gbs = [Buf("gbs%d" % i) for i in range(2)]
    s2b = [sb("s2_%d" % i, [128, TT], BF16) for i in range(2)]
    b_s2 = [Buf("s2_%d" % i) for i in range(2)]
    b_gs = [Buf("gs%d" % i) for i in range(2)]
    moe_ctr = [0, 0]

    b_s_in = [Buf("s_in%d" % j) for j in range(16)]
    b_s_br = [Buf("s_br%d" % j) for j in range(4)]
    b_s_out = [Buf("s_out%d" % j) for j in range(4)]
    b_s_gu = [[Buf("s_gu%d_%d" % (e, fh)) for fh in range(2)] for e in range(NE)]
    b_s_wd = [[Buf("s_wd%d_%d" % (hf, fc)) for fc in range(8)] for hf in range(4)]
    wed_v = wed_d.rearrange("e (fh p) (fc j) -> fc p (e fh) j", fh=2, j=128)

    class PsPool:
        def __init__(self, idxs):
            self.idxs, self.ctr, self.held = list(idxs), 0, set()

        def get(self, hold=False):
            while True:
                i = self.idxs[self.ctr % len(self.idxs)]
                self.ctr += 1
                if i not in self.held:
                    break
            if hold:
                self.held.add(i)
            return PS[i], b_PS[i], i

        def release(self, i):
            self.held.discard(i)

    MP = PsPool(range(0, 4))
    EP = PsPool(range(4, 8))

    def load_slab(t, which, j):
        src_f, src_s, bs = ((w_in_d, w_in_s, b_s_in), (w_br_d, wbr_s, b_s_br), (w_out_d, wout_s, b_s_out))[which]
        i = slab_ctr[0] % NSLAB
        slab_ctr[0] += 1
        t_, b_ = wslab[i], b_wslab[i]
        col0 = 256 * j
        if t == 0:
            S.dma("pool", lambda q: q.dma_start(
                out=t_[:], in_=src_f[:, col0:col0 + 256].rearrange("(c p) f -> p c f", p=128)), b_, writes=[b_])
            S.dma("sp", lambda q: q.dma_start(out=src_s[j], in_=t_[:].rearrange("p c f -> p (c f)")),
                  b_, reads=[b_], writes=[bs[j]])
        else:
            S.dma("sp", lambda q: q.dma_start(out=t_[:].rearrange("p c f -> p (c f)"), in_=src_s[j]),
                  b_, reads=[bs[j]], writes=[b_])
        return t_, b_

    def load_wgu(t, e, fh):
        iw = moe_ctr[0] % NGU
        moe_ctr[0] += 1
        t_, b_ = wgu[iw], b_wgu[iw]
        if t == 0:
            S.dma("pool", lambda q: q.dma_start(
                out=t_[:, 0], in_=weg_d[e][:, fh * 128:(fh + 1) * 128].rearrange("(c p) f -> p c f", p=128)), b_, writes=[b_])
            S.dma("pool", lambda q: q.dma_start(
                out=t_[:, 1], in_=weu_d[e][:, fh * 128:(fh + 1) * 128].rearrange("(c p) f -> p c f", p=128)), b_, writes=[b_])
            S.dma("sp", lambda q: q.dma_start(out=wgu_s[e, fh], in_=t_[:]), b_, reads=[b_], writes=[b_s_gu[e][fh]])
        else:
            S.dma("pool", lambda q: q.dma_start(out=t_[:], in_=wgu_s[e, fh]), b_, reads=[b_s_gu[e][fh]], writes=[b_])
        return t_, b_

    wd_live = {}
    wgu_live = {}

    def wgu_prefetch(t, idx):
        if idx < 32:
            wgu_live[(t, idx)] = load_wgu(t, idx // 2, idx % 2)

    def wd_prefetch(t, idx):
        if idx < 32:
            wd_live[(t, idx)] = load_wd(t, idx // 8, idx % 8)

    def load_wd(t, hf, fc):
        iw = moe_ctr[1] % NWD
        moe_ctr[1] += 1
        t_, b_ = wd[iw], b_wd[iw]
        if t == 0:
            S.dma("pool", lambda q: q.dma_start(out=t_[:], in_=wed_v[fc][:, 8 * hf:8 * hf + 8, :]), b_, writes=[b_])
            S.dma("sp", lambda q: q.dma_start(out=wd_s[hf, fc], in_=t_[:]), b_, reads=[b_], writes=[b_s_wd[hf][fc]])
        else:
            S.dma("pool", lambda q: q.dma_start(out=t_[:], in_=wd_s[hf, fc]), b_, reads=[b_s_wd[hf][fc]], writes=[b_])
        return t_, b_

    def dump(name, src, bufs, dst_ap):
        if name not in dbg_d:
            return
        shape = [int(s_) for s_ in src.shape]
        n = 1
        for s_ in shape[1:]:
            n *= s_
        tv = dbgtmp[0:shape[0], 0:n]
        if len(shape) == 3:
            tv = tv.rearrange("p (a b) -> p a b", a=shape[1])
        bt = b_dbgtmp
        S.op("dve", lambda v: v.tensor_copy(out=tv, in_=src), reads=bufs, writes=[bt])
        S.dma("sp", lambda q: q.dma_start(out=dst_ap, in_=tv), bt, reads=[bt])
        if bt not in dbg_final:
            dbg_final.append(bt)

    dbg_final = []

    def dslice(name, nrows, tok0):
        return dbg_d[name][0:nrows, tok0:tok0 + TT].rearrange("(c p) t -> p c t", p=128) if name in dbg_d else None

    def rs_from_ps(pst, psb, scale):
        i = rs_ctr[0] % 2
        rs_ctr[0] += 1
        S.op("act", lambda a: a.activation(out=ln_sb[i][:], in_=pst[:, :TT], func=AF.Ln, scale=scale, bias=EPS),
             reads=[psb], writes=[b_ln[i]])
        S.op("act", lambda a: a.activation(out=rstd[i][:], in_=ln_sb[i][:], func=AF.Exp, scale=-0.5),
             reads=[b_ln[i]], writes=[b_rstd[i]])
        return rstd[i], b_rstd[i]

    def rmsnorm(pool, x_sb, b_x, gain_sb, out_t, out_b):
        pst, psb, _ = pool.get()
        S.op("act", lambda a: a.activation(out=out_t[:], in_=x_sb[:], func=AF.Square), reads=list(b_x), writes=[out_b])
        S.group("pe", [lambda p, c=c: p.matmul(pst[:, :TT], ones_bf[:], out_t[:, c, :], start=(c == 0), stop=(c == 7))
                       for c in range(8)], reads=[out_b, b_par], writes=[psb])
        r_t, r_b = rs_from_ps(pst, psb, 1.0 / D)
        for c in range(8):
            S.op("dve", lambda v: v.scalar_tensor_tensor(
                out=out_t[:, c, :], in0=x_sb[:, c, :], scalar=gain_sb[:, c:c + 1], in1=r_t[:],
                op0=ALU.mult, op1=ALU.mult), reads=[b_x[c], r_b, b_par], writes=[out_b])
        yield 1.3
        yield 6.0

    def proj_fm(slab, bslab, col, rhs_t, rhs_b, nchunk=8, hold=False):
        pst, psb, pi_ = MP.get(hold=hold)
        S.group("pe", [lambda p, c=c: p.matmul(pst[:, :TT], slab[:, c, col:col + 128], rhs_t[:, c, :],
                                               start=(c == 0), stop=(c == nchunk - 1)) for c in range(nchunk)],
                reads=[bslab, rhs_b], writes=[psb])
        return pst, psb, pi_

    def mixer(t):
        tok0 = t * TT
        seq_t = t % TPS
        slot = t % NSLOT
        x_sb, b_x = xs[t % 2], b_xs[t % 2]
        S.dma("sp", lambda q: q.dma_start(out=x_sb[:], in_=xT_d[t]), b_x[0], writes=list(b_x))
        yield 0.0
        if stage < 1:
            return
        yield from rmsnorm(MP, x_sb, b_x, g1_sb, hT, b_hT)
        dump("h", hT[:], [b_hT], dslice("h", 1024, tok0))

        pend = []

        def qk_finish(i, sq, bq):
            pm, pmb, _ = MP.get()
            S.group("pe", [lambda p: p.matmul(pm[:, :TT], bd64[:], sq[:], start=True, stop=True)],
                    reads=[bq, b_par], writes=[pmb])
            S.op("dve", lambda v: v.tensor_copy(out=ms_all[:, i, :], in_=pm[:, :TT]), reads=[pmb], writes=[b_ms])

        def qk_norm():
            S.op("act", lambda a: a.activation(out=ms_all[:], in_=ms_all[:], func=AF.Ln, bias=EPS), reads=[b_ms], writes=[b_ms])
            S.op("act", lambda a: a.activation(out=ms_all[:], in_=ms_all[:], func=AF.Exp, scale=-0.5), reads=[b_ms], writes=[b_ms])
            S.op("dve", lambda v: v.scalar_tensor_tensor(out=qT[:], in0=qT[:], scalar=cq[:, 0:1], in1=ms_all[:, 0:4, :],
                                                         op0=ALU.mult, op1=ALU.mult), reads=[b_qT, b_ms, b_par], writes=[b_qT])
            S.op("dve", lambda v: v.tensor_tensor(out=kT[slot][:], in0=kT[slot][:], in1=ms_all[:, 4:8, :], op=ALU.mult),
                 reads=[b_kT[slot], b_ms], writes=[b_kT[slot]])
        for which in range(2):
            for half in range(2):
                slab, bslab = load_slab(t, 0, 2 * which + half)
                for m in range(2 * half, 2 * half + 2):
                    pq, pqb, pqi = proj_fm(slab, bslab, (m % 2) * 128, hT, b_hT)
                    sq, bq = sq_sb[m % 2], b_sq[m % 2]
                    S.op("act", lambda a: a.activation(out=sq[:], in_=pq[:, :TT], func=AF.Square),
                         reads=[pqb], writes=[bq])
                    if which == 0:
                        S.op("act", lambda a: a.activation(out=qT[:, m, :], in_=pq[:, :TT], func=AF.Copy),
                             reads=[pqb], writes=[b_qT])
                    else:
                        S.op("act", lambda a: a.activation(out=kT[slot][:, m, :], in_=pq[:, :TT], func=AF.Copy),
                             reads=[pqb], writes=[b_kT[slot]])
                    yield 1.2
                    if pend:
                        qk_finish(*pend.pop())
                    pend.append((4 * which + m, sq, bq))
        if stage < 2:
            qk_finish(*pend.pop())
            qk_norm()
            return
        for half in range(2):
            slab, bslab = load_slab(t, 0, 4 + half)
            for blk in range(NB):
                pv, pvb, _ = MP.get()
                S.group("pe", [lambda p, c=c: p.matmul(
                    pv[:, 0:256], hT[:, c, blk * 128:(blk + 1) * 128], slab[:, c, :],
                    start=(c == 0), stop=(c == 7)) for c in range(8)],
                    reads=[bslab, b_hT], writes=[pvb])
                S.op("act", lambda a: a.activation(
                    out=Vaug[slot][:, blk, 4 * half:4 * half + 4, 0:64],
                    in_=pv[:, 0:256].rearrange("p (h d) -> p h d", h=4), func=AF.Copy),
                    reads=[pvb], writes=[b_V[slot]])
                yield 1.2
                if pend:
                    qk_finish(*pend.pop())
                    qk_norm()
        dump("q", qT[:], [b_qT], dslice("q", 512, tok0))
        dump("k", kT[slot][:], [b_kT[slot]], dslice("k", 512, tok0))

        hT_kt = hT[:].rearrange("p c (k t) -> p c t k", t=8)
        Utok_g = Utok[:].rearrange("k t c -> k (t c)").rearrange("k (g t c) -> k g t c", g=32, t=8)
        for half in range(2):
            slab, bslab = load_slab(t, 0, 6 + half)
            for tau in range(8):
                pu, pub, _ = MP.get()
                S.group("pe", [lambda p, c=c: p.matmul(
                    pu[0:NK, 0:256], hT_kt[:, c, tau, :], slab[:, c, :],
                    start=(c == 0), stop=(c == 7)) for c in range(8)],
                    reads=[bslab, b_hT], writes=[pub])
                S.op("act", lambda a: a.activation(
                    out=Utok_g[:, 16 * half:16 * half + 16, tau, :],
                    in_=pu[0:NK, 0:256].rearrange("k (g c) -> k g c", c=16), func=AF.Copy),
                    reads=[pub], writes=[b_Utok])
                yield 1.2

        if stage >= 4:
            pt, ptb, _ = MP.get()
            ptv = pt[:].bitcast(BF16).rearrange("p (g k) -> p g k", k=NK)
            S.group("pe", [lambda p, g=g: p.transpose(ptv[:, g, :], Utok_g[:, g].rearrange("k t c -> k (t c)"),
                                                      identb[0:NK, 0:NK]) for g in range(32)],
                    reads=[b_Utok, b_par], writes=[ptb])
            S.op("act", lambda a: a.activation(out=Ush[:], in_=ptv[:, 0:32, :], func=AF.Copy),
                 reads=[ptb], writes=[b_Ush])
            yield 2.0
            for ri, MB in enumerate((MBre, MBim)):
                pss, pssb, _ = MP.get()
                psv = pss[:, 0:16 * NK].rearrange("p (g k) -> p g k", k=NK)
                S.group("pe", [lambda p, g=g: p.matmul(
                    psv[64 * (g % 2):64 * (g % 2) + 64, g // 2, :], MB[:, g, :], Ush[:, g, :], start=True, stop=True)
                    for g in range(32)], reads=[b_Ush, b_tab], writes=[pssb])
                S.op("act", lambda a: a.activation(out=Sf[:, ri], in_=psv, func=AF.Copy),
                     reads=[pssb], writes=[b_Sf])
                yield 2.0
            if seq_t == 0:
                S.op("dve", lambda v: v.memset(Hf[:, :, :, 0:1], 0.0), writes=[b_Hf])
            else:
                S.op("dve", lambda v: v.tensor_copy(out=Hf[:, :, :, 0:1], in_=Hf[:, :, :, NK:NK + 1]),
                     reads=[b_Hf], writes=[b_Hf])
            c_re, c_im = Hf[:, 0, :, 0], Hf[:, 1, :, 0]

            def dv(fn, reads, writes):
                S.op("dve", fn, reads=reads, writes=writes)
            rw = [b_Hf, b_Sf, b_tab]
            dv(lambda v: v.tensor_tensor(out=cz[:, 0], in0=A8[:, 0], in1=c_re, op=ALU.mult), rw, [b_Sf])
            dv(lambda v: v.tensor_tensor(out=cz[:, 1], in0=A8[:, 1], in1=c_im, op=ALU.mult), rw, [b_Sf])
            dv(lambda v: v.tensor_tensor(out=cz[:, 2], in0=A8[:, 0], in1=c_im, op=ALU.mult), rw, [b_Sf])
            dv(lambda v: v.tensor_tensor(out=cz[:, 3], in0=A8[:, 1], in1=c_re, op=ALU.mult), rw, [b_Sf])
            dv(lambda v: v.tensor_tensor(out=cz[:, 0], in0=cz[:, 0], in1=cz[:, 1], op=ALU.subtract), rw, [b_Sf])
            dv(lambda v: v.tensor_tensor(out=cz[:, 2], in0=cz[:, 2], in1=cz[:, 3], op=ALU.add), rw, [b_Sf])
            dv(lambda v: v.tensor_tensor(out=Sf[:, 0, :, 0], in0=Sf[:, 0, :, 0], in1=cz[:, 0], op=ALU.add), rw, [b_Sf])
            dv(lambda v: v.tensor_tensor(out=Sf[:, 1, :, 0], in0=Sf[:, 1, :, 0], in1=cz[:, 2], op=ALU.add), rw, [b_Sf])
            yield 1.0
            dv(lambda v: v.tensor_tensor(out=Tf[:, 0], in0=Sf[:, 0], in1=Ere[:], op=ALU.mult), [b_Sf, b_tab], [b_Tf])
            dv(lambda v: v.tensor_tensor(out=Tf[:, 1], in0=Sf[:, 1], in1=Eim[:], op=ALU.mult), [b_Sf, b_tab], [b_Tf])
            dv(lambda v: v.tensor_tensor(out=Xf[:, 0], in0=Tf[:, 0], in1=Tf[:, 1], op=ALU.add), [b_Tf], [b_Xf])
            yield 1.0
            dv(lambda v: v.tensor_tensor(out=Tf[:, 0], in0=Sf[:, 1], in1=Ere[:], op=ALU.mult), [b_Sf, b_tab, b_Xf], [b_Tf])
            dv(lambda v: v.tensor_tensor(out=Tf[:, 1], in0=Sf[:, 0], in1=Eim[:], op=ALU.mult), [b_Sf, b_tab], [b_Tf])
            dv(lambda v: v.tensor_tensor(out=Xf[:, 1], in0=Tf[:, 0], in1=Tf[:, 1], op=ALU.subtract), [b_Tf], [b_Xf])
            yield 1.0
            Rflat = Rtab[:].rearrange("p g k -> p (g k)")
            for ri in range(2):
                dv(lambda v: v.tensor_tensor_scan(
                    out=Zf[:, ri].rearrange("p g k -> p (g k)"), data0=Rflat,
                    data1=Xf[:, ri].rearrange("p g k -> p (g k)"), initial=0.0, op0=ALU.mult, op1=ALU.add),
                    [b_Xf, b_tab], [b_Zf])
                yield 1.0
            dv(lambda v: v.tensor_tensor(out=Tf[:, 0], in0=Zf[:, 0], in1=Ere[:], op=ALU.mult), [b_Zf, b_tab], [b_Tf])
            dv(lambda v: v.tensor_tensor(out=Tf[:, 1], in0=Zf[:, 1], in1=Eim[:], op=ALU.mult), [b_Zf, b_tab], [b_Tf])
            dv(lambda v: v.tensor_tensor(out=Hf[:, 0, :, 1:NK + 1], in0=Tf[:, 0], in1=Tf[:, 1], op=ALU.subtract), [b_Tf], [b_Hf])
            yield 1.0
            dv(lambda v: v.tensor_tensor(out=Tf[:, 0], in0=Zf[:, 0], in1=Eim[:], op=ALU.mult), [b_Zf, b_tab, b_Hf], [b_Tf])
            dv(lambda v: v.tensor_tensor(out=Tf[:, 1], in0=Zf[:, 1], in1=Ere[:], op=ALU.mult), [b_Zf, b_tab], [b_Tf])
            dv(lambda v: v.tensor_tensor(out=Hf[:, 1, :, 1:NK + 1], in0=Tf[:, 0], in1=Tf[:, 1], op=ALU.add), [b_Tf], [b_Hf])
            yield 1.0
            S.op("dve", lambda v: v.tensor_copy(out=Hb[:], in_=Hf[:, :, :, 0:NK]), reads=[b_Hf], writes=[b_Hb])
            yield 1.0

        for j in range(8):
            slab, bslab = load_slab(t, 0, 8 + j)
            for m in range(2):
                ci = j * 2 + m
                pg, pgb, _ = proj_fm(slab, bslab, m * 128, hT, b_hT)
                S.op("act", lambda a: a.activation(
                    out=gateT[:, ci, :], in_=pg[:, :TT], func=AF.Tanh, bias=hbias[:, ci:ci + 1], scale=0.5),
                    reads=[pgb, b_par], writes=[b_gate])
                yield 1.2
        if stage < 3:
            return

        for blk in range(NB):
            m_abs = seq_t * NB + blk
            kbs = [kb for kb in range(5) if m_abs - 4 + kb >= 0]
            for hg in range(2):
                po, pob, poi = MP.get(hold=True)
                po4 = po[:, 0:260].rearrange("p (h d) -> p h d", h=4)
                pend = []
                first = [True]

                def pv_step(ip, ks, kblk):
                    fns = []
                    for j in range(4):
                        h = 2 * j + hg
                        fns.append(lambda p, j=j, h=h, st=(first[0] and j == 0): p.matmul(
                            po[:, j * 65:(j + 1) * 65], PT[ip][:, j * 128:(j + 1) * 128],
                            Vaug[ks][:, kblk, h, :], start=st, stop=True, skip_group_check=True))
                    S.group("pe", fns, reads=[b_PT[ip], b_V[ks]], writes=[pob])
                    first[0] = False
                for kb in kbs:
                    ab = m_abs - 4 + kb
                    kt_ = (t - seq_t) + ab // NB
                    ks = kt_ % NSLOT
                    kblk = ab % NB
                    pst, psb, _ = MP.get()
                    fns = []
                    for j in range(4):
                        h = 2 * j + hg
                        mchunk, hb = h // 2, 64 * (h % 2)
                        fns.append(lambda p, j=j, mchunk=mchunk, hb=hb: p.matmul(
                            pst[:, j * 128:(j + 1) * 128],
                            kT[ks][hb:hb + 64, mchunk, kblk * 128:(kblk + 1) * 128],
                            qT[hb:hb + 64, mchunk, blk * 128:(blk + 1) * 128], start=True, stop=True))
                    S.group("pe", fns, reads=[b_kT[ks], b_qT], writes=[psb])
                    ia = att_ctr[0] % 2
                    att_ctr[0] += 1
                    S.op("dve", lambda v: v.tensor_tensor(
                        out=att_tmp[ia][:].rearrange("p (h q) -> p h q", h=4),
                        in0=pst[:, :].rearrange("p (h q) -> p h q", h=4),
                        in1=biasT4[:, kb, :, :].rearrange("p (j two) q -> p j two q", two=2)[:, :, hg, :], op=ALU.add),
                        reads=[psb, b_bias], writes=[b_att_tmp[ia]])
                    ip = att_ctr[1] % 3
                    att_ctr[1] += 1
                    S.op("act", lambda a: a.activation(out=PT[ip][:], in_=att_tmp[ia][:], func=AF.Exp),
                         reads=[b_att_tmp[ia]], writes=[b_PT[ip]])
                    yield 0.4
                    if len(pend) >= 2:
                        pv_step(*pend.pop(0))
                    pend.append((ip, ks, kblk))
                while pend:
                    pv_step(*pend.pop(0))
                    yield 0.3
                S.op("dve", lambda v: v.reciprocal(out=rden[:, hg, :], in_=po4[:, :, 64]),
                     reads=[pob], writes=[b_rden[hg]])
                S.op("dve", lambda v: v.tensor_tensor(
                    out=yatt[:, blk, :].rearrange("p (j two d) -> p j two d", two=2, d=64)[:, :, hg, :],
                    in0=po4[:, :, 0:64],
                    in1=rden[:, hg, :].rearrange("p (h o) -> p h o", o=1).to_broadcast([128, 4, 64]),
                    op=ALU.mult), reads=[pob, b_rden[hg]], writes=[b_yatt[blk]])
                MP.release(poi)
            pt, ptb, _ = MP.get()
            ptv = pt[:].bitcast(BF16)
            S.group("pe", [lambda p, c=c: p.transpose(
                ptv[:, c * 128:(c + 1) * 128], yatt[:, blk, c * 128:(c + 1) * 128], identb[:]) for c in range(4)],
                reads=[b_yatt[blk], b_par], writes=[ptb])
            S.op("act", lambda a: a.activation(
                out=yaT[:, :, blk * 128:(blk + 1) * 128], in_=ptv[:, 0:512].rearrange("p (c t) -> p c t", c=4),
                func=AF.Copy), reads=[ptb], writes=[b_yaT])
            yield 0.3
        dump("ya", yaT[:], [b_yaT], dslice("ya", 512, tok0))
        if stage < 4:
            return

        for g0 in range(0, 32, 4):
            py, pyb, _ = MP.get()
            fns = []
            for gi in range(4):
                g = g0 + gi
                gp = g // 2
                o = py[0:NK, gi * 128:(gi + 1) * 128]
                fns.append(lambda p, o=o, g=g, st=(gi == 0): p.matmul(o, Ush[:, g, :], T_all[:, g, :], start=st, stop=False,
                                                                      skip_group_check=True))
                fns.append(lambda p, o=o, gp=gp, g=g: p.matmul(o, Hb[:, 0, gp, :], MCre[:, g, :],
                                                              start=False, stop=False, skip_group_check=True))
                fns.append(lambda p, o=o, gp=gp, g=g: p.matmul(o, Hb[:, 1, gp, :], MCim[:, g, :],
                                                              start=False, stop=True, skip_group_check=True))
            S.group("pe", fns, reads=[b_Ush, b_Hb, b_tab], writes=[pyb])
            S.op("act", lambda a: a.activation(
                out=Utok[:, :, g0 * 16:(g0 + 4) * 16].rearrange("k t (g c) -> k t g c", g=4),
                in_=py[0:NK, :].rearrange("k (g t c) -> k t g c", g=4, t=8), func=AF.Gelu_apprx_tanh),
                reads=[pyb, b_Ush], writes=[b_Utok])
            yield 0.8
        pt, ptb, _ = MP.get()
        ptz = pt[:].bitcast(BF16)[:, 0:4 * TT].rearrange("p (c t k) -> p c t k", c=4, t=8)
        S.group("pe", [lambda p, cc=cc, tau=tau: p.transpose(
            ptz[:, cc, tau, :], Utok[:, tau, cc * 128:(cc + 1) * 128], identb[0:NK, 0:NK])
            for cc in range(4) for tau in range(8)], reads=[b_Utok, b_par], writes=[ptb])
        S.op("act", lambda a: a.activation(out=zT[:].rearrange("p c (k t) -> p c t k", t=8), in_=ptz, func=AF.Copy, scale=0.5),
             reads=[ptb], writes=[b_zT])
        yield 2.0
        dump("z", zT[:], [b_zT], dslice("z", 512, tok0))
        for m in range(4):
            pg, pgb, _ = MP.get()
            S.group("pe", [lambda p, c=c: p.matmul(pg[:, :TT], wglu[:, c, m * 128:(m + 1) * 128], zT[:, c, :],
                                                   start=(c == 0), stop=(c == 3)) for c in range(4)],
                    reads=[b_wres, b_zT], writes=[pgb])
            S.op("act", lambda a: a.activation(out=sig[m % 2][:], in_=pg[:, :TT], func=AF.Tanh,
                                               bias=hbias[:, 16 + m:17 + m], scale=1.0),
                 reads=[pgb, b_par], writes=[b_sig[m % 2]])
            S.op("dve", lambda v: v.scalar_tensor_tensor(out=ysT[:, m, :], in0=sig[m % 2][:], scalar=1.0, in1=zT[:, m, :],
                                                         op0=ALU.add, op1=ALU.mult),
                 reads=[b_zT, b_sig[m % 2]], writes=[b_ysT])
            yield 0.6
        dump("ys", ysT[:], [b_ysT], dslice("ys", 512, tok0))
        if stage < 5:
            return

        for fc in range(8):
            if fc % 2 == 0:
                wbr, b_wbr = load_slab(t, 1, fc // 2)
            fo = (fc % 2) * 128
            pa, pab, _ = MP.get()
            S.group("pe", [lambda p, c=c: p.matmul(pa[:, :TT], wbr[:, c, fo:fo + 128], yaT[:, c, :],
                                                   start=(c == 0), stop=(c == 3)) for c in range(4)],
                    reads=[b_wbr, b_yaT], writes=[pab])
            pb, pbb, _ = MP.get()
            S.group("pe", [lambda p, c=c: p.matmul(pb[:, :TT], wbr[:, 4 + c, fo:fo + 128], ysT[:, c, :],
                                                   start=(c == 0), stop=(c == 3)) for c in range(4)],
                    reads=[b_wbr, b_ysT], writes=[pbb])
            S.op("dve", lambda v: v.scalar_tensor_tensor(out=m12[0][:], in0=gateT[:, fc, :], scalar=1.0, in1=pa[:, :TT],
                                                         op0=ALU.add, op1=ALU.mult),
                 reads=[pab, b_gate], writes=[b_m12[0]])
            S.op("dve", lambda v: v.scalar_tensor_tensor(out=m12[1][:], in0=gateT[:, 8 + fc, :], scalar=1.0, in1=pb[:, :TT],
                                                         op0=ALU.add, op1=ALU.mult),
                 reads=[pbb, b_gate], writes=[b_m12[1]])
            S.op("dve", lambda g_: g_.tensor_tensor(out=mT[:, fc, :], in0=m12[0][:], in1=m12[1][:], op=ALU.add),
                 reads=[b_m12[0], b_m12[1]], writes=[b_mT])
            yield 1.2
        for fc in range(8):
            if fc % 2 == 0:
                wout, b_wout = load_slab(t, 2, fc // 2)
            fo = (fc % 2) * 128
            po, pob, _ = MP.get()
            S.group("pe", [lambda p, c=c: p.matmul(po[:, :TT], wout[:, c, fo:fo + 128], mT[:, c, :],
                                                   start=(c == 0), stop=(c == 7)) for c in range(8)],
                    reads=[b_wout, b_mT], writes=[pob])
            S.op("dve", lambda v: v.scalar_tensor_tensor(out=x_sb[:, fc, :], in0=po[:, :TT], scalar=0.5, in1=x_sb[:, fc, :],
                                                         op0=ALU.mult, op1=ALU.add),
                 reads=[pob, b_x[fc]], writes=[b_x[fc]])
            yield 1.2
        dump("x1", x_sb[:], b_x, dslice("x1", 1024, tok0))

    def store_x(t):
        tok0 = t * TT
        x_sb, b_x = xs[t % 2], b_xs[t % 2]
        for c in range(8):
            S.dma("sp", lambda q: q.dma_start(out=outT_d[t, :, c, :], in_=x_sb[:, c, :]),
                  b_x[c], reads=[b_x[c]])

    def moe(t):
        tok0 = t * TT
        x_sb, b_x = xs[t % 2], b_xs[t % 2]
        if stage < 6:
            store_x(t)
            return
        yield from rmsnorm(EP, x_sb, b_x, g2_sb, h2T, b_h2T)
        pr_, prb, _ = EP.get()
        prv = pr_[:, 0:NB * 20].rearrange("p (b j) -> p b j", j=20)
        for blk in range(NB):
            S.group("pe", [lambda p, c=c: p.matmul(prv[:, blk, :], h2T[:, c, blk * 128:(blk + 1) * 128], wr[:, c, :],
                                                   start=(c == 0), stop=(c == 7)) for c in range(8)],
                    reads=[b_h2T, b_wres], writes=[prb])
        R = [b_rt, b_par]

        def rv(fn, reads=R, writes=(b_rt,)):
            S.op("dve", fn, reads=list(reads), writes=list(writes))
        rv(lambda v: v.tensor_tensor(out=Lr[:], in0=prv, in1=rbias_sb.rearrange("p (o j) -> p o j", o=1).to_broadcast([128, NB, 20]),
                                     op=ALU.add), reads=[prb, b_par, b_rt])
        G = Lr[:, :, 0:4]
        E4 = Lr[:, :, 4:20].rearrange("p b (g j) -> p b g j", g=4)
        gmax, gsum, gprob, m1, m2, dd, w1, w2 = [rt[:, i, :, 0:1] for i in range(8)]
        gmask, ge, ing, x2 = [rt[:, 8 + i] for i in range(4)]
        mask1 = rt16[:, :, 0:4]
        mask2 = rt16[:, :, 4:8]
        wj = rt16[:, :, 8:12]
        tj = rt16[:, :, 12:16]

        def b4(x):
            return x.to_broadcast([128, NB, 4])
        rv(lambda v: v.tensor_reduce(out=gmax, in_=G, axis=AX.X, op=ALU.max))
        rv(lambda v: v.tensor_tensor(out=ge, in0=G, in1=b4(gmax), op=ALU.subtract))
        S.op("act", lambda a: a.activation(out=ge, in_=ge, func=AF.Exp), reads=R, writes=[b_rt])
        rv(lambda v: v.tensor_reduce(out=gsum, in_=ge, axis=AX.X, op=ALU.add))
        rv(lambda v: v.reciprocal(out=gprob, in_=gsum))
        rv(lambda v: v.tensor_tensor(out=gmask, in0=G, in1=b4(gmax), op=ALU.is_equal))
        sel4 = gates[:].rearrange("p b (g j) -> p b g j", g=4)
        rv(lambda v: v.tensor_tensor(out=sel4, in0=E4, in1=gmask.rearrange("p b (g o) -> p b g o", o=1).to_broadcast([128, NB, 4, 4]),
                                     op=ALU.mult))
        rv(lambda v: v.tensor_reduce(out=ing, in_=sel4.rearrange("p b g j -> p b j g"), axis=AX.X, op=ALU.add))
        rv(lambda v: v.tensor_reduce(out=m1, in_=ing, axis=AX.X, op=ALU.max))
        rv(lambda v: v.tensor_tensor(out=mask1, in0=ing, in1=b4(m1), op=ALU.is_equal))
        rv(lambda v: v.scalar_tensor_tensor(out=x2, in0=mask1, scalar=-1e30, in1=ing, op0=ALU.mult, op1=ALU.add))
        rv(lambda v: v.tensor_reduce(out=m2, in_=x2, axis=AX.X, op=ALU.max))
        rv(lambda v: v.tensor_tensor(out=mask2, in0=x2, in1=b4(m2), op=ALU.is_equal))
        rv(lambda v: v.tensor_tensor(out=dd, in0=m2, in1=m1, op=ALU.subtract))
        S.op("act", lambda a: a.activation(out=dd, in_=dd, func=AF.Exp), reads=R, writes=[b_rt])
        rv(lambda v: v.tensor_scalar(out=w1, in0=dd, scalar1=1.0, scalar2=None, op0=ALU.add))
        rv(lambda v: v.reciprocal(out=w1, in_=w1))
        rv(lambda v: v.tensor_tensor(out=w2, in0=dd, in1=w1, op=ALU.mult))
        rv(lambda v: v.tensor_tensor(out=w1, in0=w1, in1=gprob, op=ALU.mult))
        rv(lambda v: v.tensor_tensor(out=w2, in0=w2, in1=gprob, op=ALU.mult))
        rv(lambda v: v.tensor_tensor(out=wj, in0=mask1, in1=b4(w1), op=ALU.mult))
        rv(lambda v: v.tensor_tensor(out=tj, in0=mask2, in1=b4(w2), op=ALU.mult))
        rv(lambda v: v.tensor_tensor(out=wj, in0=wj, in1=tj, op=ALU.add))
        rv(lambda v: v.tensor_tensor(out=sel4, in0=gmask.rearrange("p b (g o) -> p b g o", o=1).to_broadcast([128, NB, 4, 4]),
                                     in1=wj.rearrange("p b (o j) -> p b o j", o=1).to_broadcast([128, NB, 4, 4]), op=ALU.mult))
        yield 12.0
        pgt, pgtb, _ = EP.get()
        S.group("pe", [lambda p, blk=blk: p.transpose(pgt[0:16, blk * 128:(blk + 1) * 128], gates[:, blk, :], identf)
                       for blk in range(NB)], reads=[b_rt, b_par], writes=[pgtb])
        S.op("act", lambda a: a.activation(out=gatesT[:], in_=pgt[0:16, 0:TT], func=AF.Copy), reads=[pgtb], writes=[b_gatesT])
        if "gates" in dbg_d:
            dump("gates", gatesT[:], [b_gatesT], dbg_d["gates"][0:16, tok0:tok0 + TT])
        yield 3.0
        if stage < 7:
            store_x(t)
            return

        for i_ in range(NWD):
            wd_prefetch(t, i_)
        for i_ in range(NGU):
            wgu_prefetch(t, i_)

        def emit_gm(e):
            if e < NE:
                S.op("dve", lambda v: v.tensor_scalar(out=gm[e % 3][:], in0=gatesT[:], scalar1=identf[0:16, e:e + 1],
                                                      scalar2=0.5, op0=ALU.mult, op1=ALU.mult),
                     reads=[b_gatesT, b_par], writes=[b_gm[e % 3]])

        def emit_bcast(e):
            if e < NE:
                pgb_, pgbb, _ = EP.get()
                S.group("pe", [lambda p: p.matmul(pgb_[:, :TT], ones_bf[0:16, :], gm[e % 3][:], start=True, stop=True)],
                        reads=[b_gm[e % 3], b_par], writes=[pgbb])
                S.op("act", lambda a: a.activation(out=gbs[e % 2][:], in_=pgb_[:, :TT], func=AF.Copy),
                     reads=[pgbb], writes=[b_gbs[e % 2]])
        emit_gm(0)
        emit_gm(1)
        emit_bcast(0)
        for hf in range(4):
            for e in range(4 * hf, 4 * hf + 4):
                emit_gm(e + 2)
                for fh in range(2):
                    if fh == 1:
                        emit_bcast(e + 1)
                    kc = 2 * (e - 4 * hf) + fh
                    w_, bw_ = wgu_live.pop((t, 2 * e + fh))
                    pa, pab, _ = EP.get()
                    S.group("pe", [lambda p, c=c: p.matmul(pa[:, :TT], w_[:, 0, c, :], h2T[:, c, :], start=(c == 0), stop=(c == 7))
                                   for c in range(8)], reads=[bw_, b_h2T], writes=[pab])
                    pu, pub, _ = EP.get()
                    S.group("pe", [lambda p, c=c: p.matmul(pu[:, :TT], w_[:, 1, c, :], h2T[:, c, :], start=(c == 0), stop=(c == 7))
                                   for c in range(8)], reads=[bw_, b_h2T], writes=[pub])
                    i2 = kc % 2
                    S.op("act", lambda a: a.activation(out=sil[i2][:], in_=pa[:, :TT], func=AF.Tanh, scale=0.5),
                         reads=[pab], writes=[b_sil[i2]])
                    S.op("dve", lambda v: v.scalar_tensor_tensor(out=s2b[i2][:], in0=sil[i2][:], scalar=1.0, in1=pa[:, :TT],
                                                                 op0=ALU.add, op1=ALU.mult),
                         reads=[b_sil[i2], pab], writes=[b_s2[i2]])
                    S.op("dve", lambda v: v.tensor_tensor(out=gsb[i2][:], in0=s2b[i2][:], in1=gbs[e % 2][:], op=ALU.mult),
                         reads=[b_s2[i2], b_gbs[e % 2]], writes=[b_gs[i2]])
                    S.op("dve", lambda v: v.tensor_tensor(out=hid[:, kc, :], in0=gsb[i2][:], in1=pu[:, :TT], op=ALU.mult),
                         reads=[b_gs[i2], pub], writes=[b_hid[kc]])
                    wgu_prefetch(t, 2 * e + fh + NGU)
                    yield 2.3
            for fc in range(8):
                w_, bw_ = wd_live.pop((t, 8 * hf + fc))
                pd, pdb, _ = EP.get()
                S.group("pe", [lambda p, kc=kc: p.matmul(pd[:, :TT], w_[:, kc, :], hid[:, kc, :],
                                                         start=(kc == 0), stop=(kc == 7)) for kc in range(8)],
                        reads=[bw_] + b_hid, writes=[pdb])
                S.op("dve", lambda v: v.tensor_tensor(out=x_sb[:, fc, :], in0=pd[:, :TT], in1=x_sb[:, fc, :], op=ALU.add),
                     reads=[pdb, b_x[fc]], writes=[b_x[fc]])
                wd_prefetch(t, 8 * hf + fc + NWD)
                if hf == 3:
                    S.dma("sp", lambda q: q.dma_start(out=outT_d[t, :, fc, :], in_=x_sb[:, fc, :]),
                          b_x[fc], reads=[b_x[fc]])
                yield 1.15

    def run_interleaved(ga, gb):
        wa = wb_ = 0.0
        a_live, b_live = ga is not None, gb is not None
        if a_live and b_live:
            next(gb)
            wb_ = TUNE["head"]
        while a_live or b_live:
            if a_live and (not b_live or wa <= wb_):
                try:
                    wa += next(ga)
                except StopIteration:
                    a_live = False
            else:
                try:
                    wb_ += next(gb) * TUNE["mix_scale"]
                except StopIteration:
                    b_live = False

    ntl = NT if ntiles is None else ntiles
    run_interleaved(mixer(0), None)
    for t in range(ntl):
        run_interleaved(moe(t), mixer(t + 1) if t + 1 < ntl else None)

    for i in range(2):
        for c in range(8):
            for ev in list(b_xs[i][c].r.values()):
                S.wait_event("sp", ev)
    for bt in dbg_final:
        for ev in list(bt.r.values()):
            S.wait_event("sp", ev)
    if debug:
        print("ins per engine", S.nins, "counts", S.cnt)
    S.emit()
    es.close()
    return nc


def _consts():
    identf = np.eye(128, dtype=np.float32)
    s_idx = np.arange(128) // 16
    maskLT = (s_idx[None, :] >= s_idx[:, None]).astype(np.float32)
    hb = np.arange(128) // 64
    bd64 = (hb[:, None] == hb[None, :]).astype(np.float32) / 64.0
    return np.ascontiguousarray(np.concatenate([identf, maskLT, bd64], axis=1))


def _bias_index():
    k = np.arange(128)[:, None, None]
    kb = np.arange(5)[None, :, None]
    q = np.arange(128)[None, None, :]
    qpos = 512 + q
    kpos = kb * 128 + k
    idx = np.clip(qpos - kpos, -63, 256) + 63
    qchunk = qpos // 64
    kchunk = kpos // 64
    valid = (kchunk <= qchunk) & (kchunk >= qchunk - 8)
    return idx, valid


def prepare_inputs(inputs):
    f = lambda a: np.ascontiguousarray(np.asarray(a, dtype=np.float32))
    x = f(inputs["x"])
    L = 0
    vec = np.zeros((128, 64), np.float32)
    vec[:, 0:8] = f(inputs["mix_norm_gain"])[L].reshape(8, 128).T
    vec[:, 8:16] = f(inputs["ffn_norm_gain"])[L].reshape(8, 128).T
    vec[:, 16:32] = f(inputs["b_gate"])[L].reshape(16, 128).T
    vec[:, 32:36] = f(inputs["b_glu"])[L].reshape(4, 128).T
    vec[:, 36] = np.tile(f(inputs["q_gain"])[L], 2)
    vec[:, 37] = np.tile(f(inputs["k_gain"])[L], 2)
    vec[:, 40:44] = f(inputs["group_bias"])[L][None, :]
    vec[:, 44:60] = f(inputs["expert_bias"])[L][None, :]
    idx, valid = _bias_index()
    rb = f(inputs["rel_bias"])[L]
    bt = rb[:, idx]
    bt = np.where(valid[None], bt, np.float32(NEG)).astype(np.float32)
    biasT = np.ascontiguousarray(bt.transpose(1, 2, 0, 3)).reshape(128, 5 * 8 * 128)

    def gp_layout(a):
        return a.reshape(16, 2, 64).transpose(1, 2, 0).reshape(128, 16)
    small = np.zeros((128, 48), np.float32)
    small[:, 0:16] = gp_layout(f(inputs["ssm_lambda_re"])[L])
    small[:, 16:32] = gp_layout(f(inputs["ssm_lambda_im"])[L])
    small[:, 32:48] = gp_layout(np.broadcast_to(f(inputs["ssm_log_step"])[L][:, None], (32, 64)))
    bc = np.zeros((128, 4, 256), np.float32)
    for i, nm in enumerate(("ssm_b_re", "ssm_b_im")):
        a = f(inputs[nm])[L].reshape(16, 2, 64, 16).transpose(1, 2, 0, 3).reshape(128, 256)
        bc[:, i] = a
    for i, nm in enumerate(("ssm_c_re", "ssm_c_im")):
        a = f(inputs[nm])[L].reshape(16, 2, 16, 64).transpose(1, 3, 0, 2).reshape(128, 256)
        bc[:, 2 + i] = a
    drep = np.ascontiguousarray(np.tile(f(inputs["ssm_d"])[L].reshape(32, 16).T, (8, 1)))
    w_r = np.ascontiguousarray(np.concatenate([f(inputs["w_group_router"])[L], f(inputs["w_expert_router"])[L]], axis=1))
    common = {
        "w_in": f(inputs["w_in"])[L], "w_glu": f(inputs["w_glu"])[L], "w_branch": f(inputs["w_branch"])[L],
        "w_out": f(inputs["w_out"])[L], "w_e_gate": f(inputs["w_e_gate"])[L], "w_e_up": f(inputs["w_e_up"])[L],
        "w_e_down": f(inputs["w_e_down"])[L], "w_r": w_r, "vecs": vec, "biasT": biasT, "ssm_small": small,
        "ssm_bc": bc, "drep": drep, "consts": _consts(),
    }
    xs = x.reshape(NCORE, TOK, D)
    in_maps = []
    for i in range(NCORE):
        m = dict(common)
        m["xT"] = np.ascontiguousarray(xs[i].reshape(TOK // 256, 256, 8, 128).transpose(0, 3, 2, 1))
        in_maps.append(m)
    return in_maps


_CACHE = {}


def kernel(**inputs):
    in_maps = prepare_inputs(inputs)
    if "nc" not in _CACHE:
        _CACHE["nc"] = build_program(TT=256)
    res = run_bass_kernel_spmd(_CACHE["nc"], in_maps, core_ids=list(range(NCORE)))
    out = np.stack([np.asarray(r["outT"]).transpose(0, 3, 2, 1).reshape(TOK, D) for r in res.results], axis=0)
    return np.ascontiguousarray(out.reshape(16, SEQ, D).astype(np.float32))
```

```python
import json
import math
import os
from contextlib import ExitStack

import numpy as np
import concourse.bass as bass
import concourse.mybir as mybir
from concourse.bass_utils import run_bass_kernel_spmd

F32 = mybir.dt.float32
BF16 = mybir.dt.bfloat16
I32 = mybir.dt.int32
AF = mybir.ActivationFunctionType
ALU = mybir.AluOpType
AX = mybir.AxisListType

D = 1024
SEQ = 2048
NCORE = 8
TOK = 4096
NH = 8
DH = 64
DA = 512
DS = 512
DIN = 4096
NE = 16
FE = 256
EPS = 1e-6
NEG = -30000.0


class Buf:
    __slots__ = ("name", "w", "r", "dsem", "dcnt")

    def __init__(self, name):
        self.name = name
        self.w = None
        self.r = {}
        self.dsem = {}
        self.dcnt = {}


class Sched:
    ENG = ("pe", "act", "dve", "pool", "sp")

    def __init__(self, nc, es):
        self.nc = nc
        self.es = es
        self.sem = {e: es.enter_context(nc.semaphore("sem_" + e)) for e in self.ENG}
        self.cnt = {e: 0 for e in self.ENG}
        self.known = {e: {} for e in self.ENG}
        self.nins = {e: 0 for e in self.ENG}
        self.engines = {"pe": nc.tensor, "act": nc.scalar, "dve": nc.vector, "pool": nc.gpsimd, "sp": nc.sync}
        self.semname = {}
        self.nsem = 0

    def _key(self, sem):
        return id(sem)

    def _need(self, e, ev, waits):
        if ev is None:
            return
        k = self.known[e]
        key = self._key(ev[0])
        if k.get(key, 0) >= ev[1]:
            return
        k[key] = ev[1]
        waits.append(ev)

    def _waits(self, e, reads, writes):
        waits = []
        for b in reads:
            self._need(e, b.w, waits)
        for b in writes:
            self._need(e, b.w, waits)
            for ev in b.r.values():
                self._need(e, ev, waits)
        return waits

    def _commit(self, ev, reads, writes):
        key = self._key(ev[0])
        for b in reads:
            old = b.r.get(key)
            if old is None or old[1] < ev[1]:
                b.r[key] = ev
        for b in writes:
            b.w = ev
            b.r = {}

    def _emit(self, e, waits, fn, ev, inc):
        engine = self.engines[e]
        for (s_, v) in waits:
            engine.wait_ge(s_, v)
        self.nins[e] += 1
        if fn is None:
            return
        ins = fn(engine)
        if ev is not None:
            ins.then_inc(ev[0], inc)

    def op(self, e, fn, reads=(), writes=()):
        waits = self._waits(e, reads, writes)
        self.cnt[e] += 1
        ev = (self.sem[e], self.cnt[e])
        self._emit(e, waits, fn, ev, 1)
        self._commit(ev, reads, writes)
        return ev

    def group(self, e, fns, reads=(), writes=()):
        waits = self._waits(e, reads, writes)
        self.cnt[e] += 1
        ev = (self.sem[e], self.cnt[e])
        n = len(fns)
        for i, fn in enumerate(fns):
            self._emit(e, waits if i == 0 else [], fn, ev if i == n - 1 else None, 1)
        self._commit(ev, reads, writes)
        return ev

    def dma(self, e, fn, owner, reads=(), writes=()):
        kind = "sw" if e == "pool" else "hw"
        if kind not in owner.dsem:
            owner.dsem[kind] = self.es.enter_context(self.nc.semaphore("dsem_%d" % self.nsem))
            owner.dcnt[kind] = 0
            self.nsem += 1
        waits = self._waits(e, reads, writes)
        owner.dcnt[kind] += 16
        ev = (owner.dsem[kind], owner.dcnt[kind])
        self._emit(e, waits, fn, ev, 16)
        self._commit(ev, reads, writes)
        return ev

    def wait_event(self, e, ev):
        waits = []
        self._need(e, ev, waits)
        if waits:
            self._emit(e, waits, None, None, 0)

    def barrier(self):
        last = {e: (self.sem[e], self.cnt[e]) for e in self.ENG if self.cnt[e] > 0}
        for e in self.ENG:
            waits = []
            for f, ev in last.items():
                if f != e:
                    self._need(e, ev, waits)
            if waits:
                self._emit(e, waits, None, None, 0)

    def emit(self):
        pass


TWO_PI = 2.0 * math.pi
TUNE = {"head": 14.0, "mix_scale": 0.8}
if os.environ.get("KTUNE"):
    TUNE.update(json.loads(os.environ["KTUNE"]))


def build_program(TT=256, debug=(), stage=99, ntiles=None):
    nc = bass.Bass("TRN2", target_bir_lowering=False)
    es = ExitStack()
    S = Sched(nc, es)
    NT = TOK // TT
    TPS = SEQ // TT
    NB = TT // 128
    NK = TT // 8
    NSLOT = 512 // TT + 1
    LV = int(math.log2(NK))

    def dram_in(name, shape, dt=F32):
        return nc.dram_tensor(name, list(shape), dt, kind="ExternalInput").ap()

    xT_d = dram_in("xT", [TOK // TT, 128, 8, TT])
    w_in_d = dram_in("w_in", [D, DIN])
    w_glu_d = dram_in("w_glu", [DS, DS])
    w_br_d = dram_in("w_branch", [D, D])
    w_out_d = dram_in("w_out", [D, D])
    weg_d = dram_in("w_e_gate", [NE, D, FE])
    weu_d = dram_in("w_e_up", [NE, D, FE])
    wed_d = dram_in("w_e_down", [NE, FE, D])
    w_r_d = dram_in("w_r", [D, 20])
    vec_d = dram_in("vecs", [128, 64])
    biasT_d = dram_in("biasT", [128, 5 * 8 * 128])
    ssm_d = dram_in("ssm_small", [128, 48])
    ssm_bc_d = dram_in("ssm_bc", [128, 4, 256])
    drep_d = dram_in("drep", [128, 32])
    cst_d = dram_in("consts", [128, 128 * 3])
    outT_d = nc.dram_tensor("outT", [TOK // TT, 128, 8, TT], F32, kind="ExternalOutput").ap()
    dbg_d = {}
    for name, shape in debug:
        dbg_d[name] = nc.dram_tensor("dbg_" + name, list(shape), F32, kind="ExternalOutput").ap()

    def sbt(stack, name, shape, dt=F32):
        return stack.enter_context(nc.sbuf_tensor("sb_" + name, list(shape), dt))

    def sb(name, shape, dt=F32):
        return sbt(es, name, shape, dt)

    PS = [es.enter_context(nc.psum_tensor("ps%d" % i, [128, 512], F32)) for i in range(8)]
    b_PS = [Buf("ps%d" % i) for i in range(8)]
    ps_ctr = [0]

    def next_ps():
        i = ps_ctr[0] % 8
        ps_ctr[0] += 1
        return PS[i], b_PS[i]

    w_in_s = nc.dram_tensor("w_in_bf", [16, 128, 8 * 256], BF16).ap()
    wgu_s = nc.dram_tensor("wgu_bf", [NE, 2, 128, 2, 8, 128], BF16).ap()
    wd_s = nc.dram_tensor("wd_bf", [4, 8, 128, 8, 128], BF16).ap()
    b_w_in_s = Buf("w_in_s")
    wbr_s = nc.dram_tensor("wbr_bf", [4, 128, 8 * 256], BF16).ap()
    wout_s = nc.dram_tensor("wout_bf", [4, 128, 8 * 256], BF16).ap()
    b_wbr_s = Buf("wbr_s")
    b_wout_s = Buf("wout_s")
    b_wgu_s = Buf("wgu_s")
    b_wd_s = Buf("wd_s")

    cst = sb("cst", [128, 128 * 3])
    identf = cst[:, 0:128]
    maskLT = cst[:, 128:256]
    bd64f = cst[:, 256:384]
    vec = sb("vec", [128, 64])
    b_par = Buf("params")
    S.dma("sp", lambda q: q.dma_start(out=cst[:], in_=cst_d), b_par, writes=[b_par])
    S.dma("sp", lambda q: q.dma_start(out=vec[:], in_=vec_d), b_par, writes=[b_par])
    g1_sb = vec[:, 0:8]
    g2_sb = vec[:, 8:16]
    bgate_sb = vec[:, 16:32]
    bglu_sb = vec[:, 32:36]
    rbias_sb = vec[:, 40:60]
    hbias = sb("hbias", [128, 20])
    S.op("dve", lambda v: v.tensor_scalar(out=hbias[:], in0=vec[:, 16:36], scalar1=0.5, scalar2=None, op0=ALU.mult),
         reads=[b_par], writes=[b_par])
    cq = sb("cq", [128, 1])
    S.op("dve", lambda v: v.tensor_scalar(out=cq[:], in0=vec[:, 36:37], scalar1=vec[:, 37:38],
                                          scalar2=0.125, op0=ALU.mult, op1=ALU.mult),
         reads=[b_par], writes=[b_par])
    ones_bf = sb("ones_bf", [128, 128], BF16)
    identb = sb("identb", [128, 128], BF16)
    bd64 = sb("bd64", [128, 128], BF16)
    ones16f = sb("ones16f", [16, 128])
    S.op("pool", lambda g: g.memset(ones_bf[:], 1.0), writes=[b_par])
    S.op("pool", lambda g: g.memset(ones16f[:], 1.0), writes=[b_par])
    S.op("dve", lambda v: v.tensor_copy(out=identb[:], in_=identf), reads=[b_par], writes=[b_par])
    S.op("dve", lambda v: v.tensor_copy(out=bd64[:], in_=bd64f), reads=[b_par], writes=[b_par])

    wglu = sb("wglu", [128, 4, DS], BF16)
    wr = sb("wr", [128, 8, 20], BF16)
    b_wres = Buf("wres")
    for (tl, src) in ((wglu, w_glu_d), (wr, w_r_d)):
        S.dma("pool", lambda q, tl=tl, src=src: q.dma_start(
            out=tl[:], in_=src.rearrange("(c p) f -> p c f", p=128)), b_wres, writes=[b_wres])

    biasT = sb("biasT", [128, 5 * 8 * 128], BF16)
    b_bias = Buf("biasT")
    S.dma("pool", lambda q: q.dma_start(out=biasT[:], in_=biasT_d), b_bias, writes=[b_bias])
    biasT4 = biasT[:].rearrange("p (kb h q) -> p kb h q", kb=5, h=8)

    T_all = sb("T_all", [128, 32, 128], BF16)
    MBre = sb("MBre", [128, 32, 64], BF16)
    MBim = sb("MBim", [128, 32, 64], BF16)
    MCre = sb("MCre", [128, 32, 128], BF16)
    MCim = sb("MCim", [128, 32, 128], BF16)
    Ere = sb("Ere", [128, 16, NK])
    Eim = sb("Eim", [128, 16, NK])
    Rtab = sb("Rtab", [128, 16, NK])
    A8 = sb("A8", [128, 2, 16])
    b_tab = Buf("ssm_tables")

    with ExitStack() as ps_:
        def tb(name, shape, dt=F32):
            return sbt(ps_, name, shape, dt)
        small = tb("ssm_small", [128, 48])
        bc = tb("ssm_bc", [128, 4, 16, 16])
        drep = tb("drep", [128, 32])
        b_p = Buf("prep")
        S.dma("sp", lambda q: q.dma_start(out=small[:], in_=ssm_d), b_p, writes=[b_p])
        S.dma("sp", lambda q: q.dma_start(out=bc[:], in_=ssm_bc_d.rearrange("p a (g c) -> p a g c", g=16)),
              b_p, writes=[b_p])
        S.dma("sp", lambda q: q.dma_start(out=drep[:], in_=drep_d), b_p, writes=[b_p])
        lre = small[:, 0:16]
        lim = small[:, 16:32]
        lst = small[:, 32:48]
        W = tb("wk", [128, 24, 16])
        (STEP, XR, MAG, MAGI, ANG, TQ, TF, RS, RC, SN, CS, ARE, AIM, IRE, IIM, NRE, DEN, RDEN,
         FRE, FIM, T1, T2, T3, T4) = [W[:, i, :] for i in range(24)]
        WI = tb("wki", [128, 16], I32)

        def P(eng, fn):
            S.op(eng, fn, reads=[b_p, b_par], writes=[b_p])

        def tt(out, a, b, op):
            P("dve", lambda v: v.tensor_tensor(out=out, in0=a, in1=b, op=op))

        def ts(out, a, s1, op0, s2=None, op1=None):
            if op1 is None:
                P("dve", lambda v: v.tensor_scalar(out=out, in0=a, scalar1=s1, scalar2=None, op0=op0))
            else:
                P("dve", lambda v: v.tensor_scalar(out=out, in0=a, scalar1=s1, scalar2=s2, op0=op0, op1=op1))

        def actf(out, a, func, scale=1.0, bias=0.0):
            P("act", lambda x: x.activation(out=out, in_=a, func=func, scale=scale, bias=bias))

        def cmul(ore, oim, xre, xim, yre, yim, t1, t2):
            tt(t1, xre, yre, ALU.mult)
            tt(t2, xim, yim, ALU.mult)
            tt(ore, t1, t2, ALU.subtract)
            tt(t1, xre, yim, ALU.mult)
            tt(t2, xim, yre, ALU.mult)
            tt(oim, t1, t2, ALU.add)

        actf(STEP, lst, AF.Exp)
        tt(XR, lre, STEP, ALU.mult)
        actf(MAG, XR, AF.Exp)
        actf(MAGI, XR, AF.Exp, scale=-1.0)
        tt(ANG, lim, STEP, ALU.mult)
        ts(TQ, ANG, 1.0 / TWO_PI, ALU.mult)
        P("dve", lambda v: v.tensor_copy(out=WI[:], in_=TQ))
        P("dve", lambda v: v.tensor_copy(out=TF, in_=WI[:]))
        P("dve", lambda v: v.scalar_tensor_tensor(out=RS, in0=TF, scalar=-TWO_PI, in1=ANG,
                                                  op0=ALU.mult, op1=ALU.add))

        def wrap(x):
            ts(T1, x, math.pi, ALU.is_gt, TWO_PI, ALU.mult)
            tt(x, x, T1, ALU.subtract)
            ts(T1, x, -math.pi, ALU.is_lt, TWO_PI, ALU.mult)
            tt(x, x, T1, ALU.add)

        wrap(RS)
        ts(RC, RS, math.pi / 2, ALU.add)
        wrap(RC)
        actf(SN, RS, AF.Sin)
        actf(CS, RC, AF.Sin)
        tt(ARE, MAG, CS, ALU.mult)
        tt(AIM, MAG, SN, ALU.mult)
        tt(IRE, MAGI, CS, ALU.mult)
        tt(T1, MAGI, SN, ALU.mult)
        ts(IIM, T1, -1.0, ALU.mult)
        ts(NRE, ARE, -1.0, ALU.add)
        tt(T1, lre, lre, ALU.mult)
        tt(T2, lim, lim, ALU.mult)
        tt(DEN, T1, T2, ALU.add)
        P("dve", lambda v: v.reciprocal(out=RDEN, in_=DEN))
        tt(T1, NRE, lre, ALU.mult)
        tt(T2, AIM, lim, ALU.mult)
        tt(T1, T1, T2, ALU.add)
        tt(FRE, T1, RDEN, ALU.mult)
        tt(T1, AIM, lre, ALU.mult)
        tt(T2, NRE, lim, ALU.mult)
        tt(T1, T1, T2, ALU.subtract)
        tt(FIM, T1, RDEN, ALU.mult)
        BB = tb("bb", [128, 2, 16, 16])
        TB = tb("tbb", [128, 2, 16, 16])
        bre_, bim_, cre_, cim_ = bc[:, 0], bc[:, 1], bc[:, 2], bc[:, 3]

        def bcg(x):
            return x.rearrange("p (g o) -> p g o", o=1).to_broadcast([128, 16, 16])

        tt(TB[:, 0], bre_, bcg(FRE), ALU.mult)
        tt(TB[:, 1], bim_, bcg(FIM), ALU.mult)
        tt(BB[:, 0], TB[:, 0], TB[:, 1], ALU.subtract)
        tt(TB[:, 0], bim_, bcg(FRE), ALU.mult)
        tt(TB[:, 1], bre_, bcg(FIM), ALU.mult)
        tt(BB[:, 1], TB[:, 0], TB[:, 1], ALU.add)
        Pre = tb("Pre", [128, 9, 16])
        Pim = tb("Pim", [128, 9, 16])
        Qre = tb("Qre", [128, 9, 16])
        Qim = tb("Qim", [128, 9, 16])
        for (pr, pi, xr_, xi_) in ((Pre, Pim, ARE, AIM), (Qre, Qim, IRE, IIM)):
            P("dve", lambda v, pr=pr: v.memset(pr[:, 0, :], 1.0))
            P("dve", lambda v, pi=pi: v.memset(pi[:, 0, :], 0.0))
            for d in range(1, 9):
                cmul(pr[:, d, :], pi[:, d, :], pr[:, d - 1, :], pi[:, d - 1, :], xr_, xi_, T1, T2)
        P("dve", lambda v: v.tensor_copy(out=A8[:, 0, :], in_=Pre[:, 8, :]))
        P("dve", lambda v: v.tensor_copy(out=A8[:, 1, :], in_=Pim[:, 8, :]))
        MCf = tb("MCf", [128, 2, 16, 128])
        XTf = tb("XTf", [128, 2, 16, 128])
        MBf = tb("MBf", [128, 2, 16, 128])
        TMP = tb("TMPf", [128, 2, 16, 16])

        def v4(x, i):
            return x[:, i].rearrange("p g (t c) -> p g t c", t=8)

        for tq in range(8):
            prb = bcg(Pre[:, tq + 1, :])
            pib = bcg(Pim[:, tq + 1, :])
            tt(TMP[:, 0], cre_, prb, ALU.mult)
            tt(TMP[:, 1], cim_, pib, ALU.mult)
            tt(v4(MCf, 0)[:, :, tq, :], TMP[:, 0], TMP[:, 1], ALU.subtract)
            tt(TMP[:, 0], cre_, pib, ALU.mult)
            tt(TMP[:, 1], cim_, prb, ALU.mult)
            tt(TMP[:, 0], TMP[:, 0], TMP[:, 1], ALU.add)
            ts(v4(MCf, 1)[:, :, tq, :], TMP[:, 0], -1.0, ALU.mult)
            for (dst, pr, pi, d) in ((MBf, Pre, Pim, 7 - tq), (XTf, Qre, Qim, tq + 1)):
                prb2 = bcg(pr[:, d, :])
                pib2 = bcg(pi[:, d, :])
                tt(TMP[:, 0], BB[:, 0], prb2, ALU.mult)
                tt(TMP[:, 1], BB[:, 1], pib2, ALU.mult)
                tt(v4(dst, 0)[:, :, tq, :], TMP[:, 0], TMP[:, 1], ALU.subtract)
                tt(TMP[:, 0], BB[:, 1], prb2, ALU.mult)
                tt(TMP[:, 1], BB[:, 0], pib2, ALU.mult)
                tt(v4(dst, 1)[:, :, tq, :], TMP[:, 0], TMP[:, 1], ALU.add)
        for g in range(32):
            for ri, MCt in enumerate((MCre, MCim)):
                S.op("dve", lambda v, g=g, ri=ri, MCt=MCt: v.tensor_scalar(
                    out=MCt[:, g, :], in0=MCf[:, ri, g // 2, :], scalar1=bd64f[:, 64 * (g % 2):64 * (g % 2) + 1],
                    scalar2=64.0, op0=ALU.mult, op1=ALU.mult), reads=[b_p, b_par], writes=[b_tab])
        TMPM = tb("TMPM", [128, 128])
        for g in range(32):
            gp, g2 = g // 2, g % 2
            lo, hi = 64 * g2, 64 * g2 + 64
            pst, psb = next_ps()
            S.group("pe", [
                lambda p, pst=pst, gp=gp, lo=lo, hi=hi: p.matmul(pst[:, 0:128], XTf[lo:hi, 0, gp, :], MCf[lo:hi, 0, gp, :],
                                                                 start=True, stop=False),
                lambda p, pst=pst, gp=gp, lo=lo, hi=hi: p.matmul(pst[:, 0:128], XTf[lo:hi, 1, gp, :], MCf[lo:hi, 1, gp, :],
                                                                 start=False, stop=True),
                lambda p, pst=pst, gp=gp, lo=lo, hi=hi: p.matmul(pst[:, 128:192], MBf[lo:hi, 0, gp, :], identf[lo:hi, lo:hi],
                                                                 start=True, stop=True),
                lambda p, pst=pst, gp=gp, lo=lo, hi=hi: p.matmul(pst[:, 192:256], MBf[lo:hi, 1, gp, :], identf[lo:hi, lo:hi],
                                                                 start=True, stop=True),
            ], reads=[b_p, b_par], writes=[psb])
            S.op("dve", lambda v, pst=pst: v.tensor_tensor(out=TMPM[:], in0=pst[:, 0:128], in1=maskLT, op=ALU.mult),
                 reads=[psb, b_par, b_p], writes=[b_p])
            S.op("dve", lambda v, g=g: v.scalar_tensor_tensor(out=T_all[:, g, :], in0=identf, scalar=drep[:, g:g + 1],
                                                             in1=TMPM[:], op0=ALU.mult, op1=ALU.add),
                 reads=[b_p, b_par], writes=[b_tab])
            S.op("act", lambda a, pst=pst, g=g: a.activation(out=MBre[:, g, :], in_=pst[:, 128:192], func=AF.Copy),
                 reads=[psb], writes=[b_tab])
            S.op("act", lambda a, pst=pst, g=g: a.activation(out=MBim[:, g, :], in_=pst[:, 192:256], func=AF.Copy),
                 reads=[psb], writes=[b_tab])
        M8I = T3
        actf(M8I, XR, AF.Exp, scale=-8.0)
        actf(T4, XR, AF.Exp, scale=8.0)
        P("dve", lambda v: v.tensor_copy(out=Rtab[:], in_=T4.rearrange("p (g o) -> p g o", o=1).to_broadcast([128, 16, NK])))
        P("dve", lambda v: v.memset(Rtab[:, :, 0:1], 0.0))
        tt(Ere[:, :, 0], Pre[:, 8, :], M8I, ALU.mult)
        tt(Eim[:, :, 0], Pim[:, 8, :], M8I, ALU.mult)
        ETr = tb("ETr", [128, 16, NK])
        ETi = tb("ETi", [128, 16, NK])
        m = 1
        while m < NK:
            ub_r = Ere[:, :, m - 1:m].to_broadcast([128, 16, m])
            ub_i = Eim[:, :, m - 1:m].to_broadcast([128, 16, m])
            tt(ETr[:, :, 0:m], Ere[:, :, 0:m], ub_r, ALU.mult)
            tt(ETi[:, :, 0:m], Eim[:, :, 0:m], ub_i, ALU.mult)
            tt(Ere[:, :, m:2 * m], ETr[:, :, 0:m], ETi[:, :, 0:m], ALU.subtract)
            tt(ETr[:, :, 0:m], Ere[:, :, 0:m], ub_i, ALU.mult)
            tt(ETi[:, :, 0:m], Eim[:, :, 0:m], ub_r, ALU.mult)
            tt(Eim[:, :, m:2 * m], ETr[:, :, 0:m], ETi[:, :, 0:m], ALU.add)
            m *= 2
        S.op("dve", lambda v: v.memset(TMPM[0:1, 0:1], 0.0), reads=[b_p], writes=[b_tab, b_p])
        S.barrier()

    xs = [sb("x_sb%d" % i, [128, 8, TT]) for i in range(2)]
    b_xs = [[Buf("x%d_%d" % (i, c)) for c in range(8)] for i in range(2)]
    sq_sb = [sb("sq%d" % i, [128, TT], BF16) for i in range(2)]
    b_sq = [Buf("sq%d" % i) for i in range(2)]
    hT = sb("hT", [128, 8, TT], BF16)
    b_hT = Buf("hT")
    h2T = sb("h2T", [128, 8, TT], BF16)
    b_h2T = Buf("h2T")
    ln_sb = [sb("ln%d" % i, [128, TT]) for i in range(2)]
    b_ln = [Buf("ln%d" % i) for i in range(2)]
    rstd = [sb("rstd%d" % i, [128, TT]) for i in range(2)]
    b_rstd = [Buf("rstd%d" % i) for i in range(2)]
    rs_ctr = [0]
    NSLAB = 3
    wslab = [sb("wslab%d" % i, [128, 8, 256], BF16) for i in range(NSLAB)]
    b_wslab = [Buf("wslab%d" % i) for i in range(NSLAB)]
    slab_ctr = [0]
    ms_all = sb("ms_all", [128, 8, TT], BF16)
    b_ms = Buf("ms_all")
    qT = sb("qT", [128, 4, TT], BF16)
    b_qT = Buf("qT")
    kT = [sb("kT%d" % i, [128, 4, TT], BF16) for i in range(NSLOT)]
    b_kT = [Buf("kT%d" % i) for i in range(NSLOT)]
    Vaug = [sb("Vaug%d" % i, [128, NB, NH, 65], BF16) for i in range(NSLOT)]
    b_V = [Buf("V%d" % i) for i in range(NSLOT)]
    for i in range(NSLOT):
        S.op("pool", lambda g, i=i: g.memset(Vaug[i][:], 1.0), writes=[b_V[i]])
    Utok = sb("Utok", [NK, 8, 512], BF16)
    b_Utok = Buf("Utok")
    Ush = sb("Ush", [128, 32, NK], BF16)
    b_Ush = Buf("Ush")
    gateT = sb("gateT", [128, 16, TT], BF16)
    b_gate = Buf("gateT")
    att_tmp = [sb("att_tmp%d" % i, [128, 512]) for i in range(2)]
    b_att_tmp = [Buf("att_tmp%d" % i) for i in range(2)]
    PT = [sb("PT%d" % i, [128, 512], BF16) for i in range(3)]
    b_PT = [Buf("PT%d" % i) for i in range(3)]
    att_ctr = [0, 0]
    rden = sb("rden", [128, 2, 4])
    b_rden = [Buf("rden0"), Buf("rden1")]
    yatt = sb("yatt", [128, NB, 512], BF16)
    b_yatt = [Buf("yatt%d" % i) for i in range(NB)]
    yaT = sb("yaT", [128, 4, TT], BF16)
    b_yaT = Buf("yaT")
    Sf = sb("Sf", [128, 2, 16, NK])
    b_Sf = Buf("Sf")
    Xf = sb("Xf", [128, 2, 16, NK])
    b_Xf = Buf("Xf")
    Zf, b_Zf = Sf, b_Sf
    Tf = sb("Tf", [128, 2, 16, NK])
    b_Tf = Buf("Tf")
    Hf = sb("Hf", [128, 2, 16, NK + 1])
    b_Hf = Buf("Hf")
    Hb = sb("Hb", [128, 2, 16, NK], BF16)
    b_Hb = Buf("Hb")
    cz = sb("cz", [128, 4, 16])
    zT = sb("zT", [128, 4, TT], BF16)
    b_zT = Buf("zT")
    sig = [sb("sig%d" % i, [128, TT], BF16) for i in range(2)]
    b_sig = [Buf("sig%d" % i) for i in range(2)]
    ysT = sb("ysT", [128, 4, TT], BF16)
    b_ysT = Buf("ysT")
    mT = sb("mT", [128, 8, TT], BF16)
    b_mT = Buf("mT")
    m12 = [sb("m12_%d" % i, [128, TT], BF16) for i in range(2)]
    b_m12 = [Buf("m12_%d" % i) for i in range(2)]
    Lr = sb("Lr", [128, NB, 20])
    rt = sb("rt", [128, 12, NB, 4])
    rt16 = sb("rt16", [128, NB, 16])
    gates = sb("gates", [128, NB, 16])
    b_rt = Buf("router")
    gatesT = sb("gatesT", [16, TT])
    b_gatesT = Buf("gatesT")
    gm = [sb("gm%d" % i, [16, TT], BF16) for i in range(3)]
    b_gm = [Buf("gm%d" % i) for i in range(3)]
    if stage < 7:
        dbgtmp = sb("dbgtmp", [128, 8 * TT])
        b_dbgtmp = Buf("dbgtmp")
    hid = sb("hid", [128, 8, TT] if stage >= 7 else [128, 2, 2], BF16)
    b_hid = [Buf("hid%d" % i) for i in range(8)]
    NGU = 3
    wgu = [sb("wgu%d" % i, [128, 2, 8, 128], BF16) for i in range(NGU)]
    b_wgu = [Buf("wgu%d" % i) for i in range(NGU)]
    NWD = 6
    wd = [sb("wd%d" % i, [128, 8, 128], BF16) for i in range(NWD)]
    b_wd = [Buf("wd%d" % i) for i in range(NWD)]
    sil = [sb("sil%d" % i, [128, TT], BF16) for i in range(2)]
    b_sil = [Buf("sil%d" % i) for i in range(2)]
    gsb = [sb("gs%d" % i, [128, TT], BF16) for i in range(2)]
    gbs = [sb("gbs%d" % i, [128, TT], BF16) for i in range(2)]
    b_gbs = [Buf("gbs%d" % i) for i in range(2)]
    s2b = [sb("s2_%d" % i, [128, TT], BF16) for i in range(2)]
    b_s2 = [Buf("s2_%d" % i) for i in range(2)]
    b_gs = [Buf("gs%d" % i) for i in range(2)]
    moe_ctr = [0, 0]

    b_s_in = [Buf("s_in%d" % j) for j in range(16)]
    b_s_br = [Buf("s_br%d" % j) for j in range(4)]
    b_s_out = [Buf("s_out%d" % j) for j in range(4)]
    b_s_gu = [[Buf("s_gu%d_%d" % (e, fh)) for fh in range(2)] for e in range(NE)]
    b_s_wd = [[Buf("s_wd%d_%d" % (hf, fc)) for fc in range(8)] for hf in range(4)]
    wed_v = wed_d.rearrange("e (fh p) (fc j) -> fc p (e fh) j", fh=2, j=128)

    class PsPool:
        def __init__(self, idxs):
            self.idxs, self.ctr, self.held = list(idxs), 0, set()

        def get(self, hold=False):
            while True:
                i = self.idxs[self.ctr % len(self.idxs)]
                self.ctr += 1
                if i not in self.held:
                    break
            if hold:
                self.held.add(i)
            return PS[i], b_PS[i], i

        def release(self, i):
            self.held.discard(i)

    MP = PsPool(range(0, 4))
    EP = PsPool(range(4, 8))

    def load_slab(t, which, j):
        src_f, src_s, bs = ((w_in_d, w_in_s, b_s_in), (w_br_d, wbr_s, b_s_br), (w_out_d, wout_s, b_s_out))[which]
        i = slab_ctr[0] % NSLAB
        slab_ctr[0] += 1
        t_, b_ = wslab[i], b_wslab[i]
        col0 = 256 * j
        if t == 0:
            S.dma("pool", lambda q: q.dma_start(
                out=t_[:], in_=src_f[:, col0:col0 + 256].rearrange("(c p) f -> p c f", p=128)), b_, writes=[b_])
            S.dma("sp", lambda q: q.dma_start(out=src_s[j], in_=t_[:].rearrange("p c f -> p (c f)")),
                  b_, reads=[b_], writes=[bs[j]])
        else:
            S.dma("sp", lambda q: q.dma_start(out=t_[:].rearrange("p c f -> p (c f)"), in_=src_s[j]),
                  b_, reads=[bs[j]], writes=[b_])
        return t_, b_

    def load_wgu(t, e, fh):
        iw = moe_ctr[0] % NGU
        moe_ctr[0] += 1
        t_, b_ = wgu[iw], b_wgu[iw]
        if t == 0:
            S.dma("pool", lambda q: q.dma_start(
                out=t_[:, 0], in_=weg_d[e][:, fh * 128:(fh + 1) * 128].rearrange("(c p) f -> p c f", p=128)), b_, writes=[b_])
            S.dma("pool", lambda q: q.dma_start(
                out=t_[:, 1], in_=weu_d[e][:, fh * 128:(fh + 1) * 128].rearrange("(c p) f -> p c f", p=128)), b_, writes=[b_])
            S.dma("sp", lambda q: q.dma_start(out=wgu_s[e, fh], in_=t_[:]), b_, reads=[b_], writes=[b_s_gu[e][fh]])
        else:
            S.dma("pool", lambda q: q.dma_start(out=t_[:], in_=wgu_s[e, fh]), b_, reads=[b_s_gu[e][fh]], writes=[b_])
        return t_, b_

    wd_live = {}
    wgu_live = {}

    def wgu_prefetch(t, idx):
        if idx < 32:
            wgu_live[(t, idx)] = load_wgu(t, idx // 2, idx % 2)

    def wd_prefetch(t, idx):
        if idx < 32:
            wd_live[(t, idx)] = load_wd(t, idx // 8, idx % 8)

    def load_wd(t, hf, fc):
        iw = moe_ctr[1] % NWD
        moe_ctr[1] += 1
        t_, b_ = wd[iw], b_wd[iw]
        if t == 0:
            S.dma("pool", lambda q: q.dma_start(out=t_[:], in_=wed_v[fc][:, 8 * hf:8 * hf + 8, :]), b_, writes=[b_])
            S.dma("sp", lambda q: q.dma_start(out=wd_s[hf, fc], in_=t_[:]), b_, reads=[b_], writes=[b_s_wd[hf][fc]])
        else:
            S.dma("pool", lambda q: q.dma_start(out=t_[:], in_=wd_s[hf, fc]), b_, reads=[b_s_wd[hf][fc]], writes=[b_])
        return t_, b_

    def dump(name, src, bufs, dst_ap):
        if name not in dbg_d:
            return
        shape = [int(s_) for s_ in src.shape]
        n = 1
        for s_ in shape[1:]:
            n *= s_
        tv = dbgtmp[0:shape[0], 0:n]
        if len(shape) == 3:
            tv = tv.rearrange("p (a b) -> p a b", a=shape[1])
        bt = b_dbgtmp
        S.op("dve", lambda v: v.tensor_copy(out=tv, in_=src), reads=bufs, writes=[bt])
        S.dma("sp", lambda q: q.dma_start(out=dst_ap, in_=tv), bt, reads=[bt])
        if bt not in dbg_final:
            dbg_final.append(bt)

    dbg_final = []

    def dslice(name, nrows, tok0):
        return dbg_d[name][0:nrows, tok0:tok0 + TT].rearrange("(c p) t -> p c t", p=128) if name in dbg_d else None

    def rs_from_ps(pst, psb, scale):
        i = rs_ctr[0] % 2
        rs_ctr[0] += 1
        S.op("act", lambda a: a.activation(out=ln_sb[i][:], in_=pst[:, :TT], func=AF.Ln, scale=scale, bias=EPS),
             reads=[psb], writes=[b_ln[i]])
        S.op("act", lambda a: a.activation(out=rstd[i][:], in_=ln_sb[i][:], func=AF.Exp, scale=-0.5),
             reads=[b_ln[i]], writes=[b_rstd[i]])
        return rstd[i], b_rstd[i]

    def rmsnorm(pool, x_sb, b_x, gain_sb, out_t, out_b):
        pst, psb, _ = pool.get()
        S.op("act", lambda a: a.activation(out=out_t[:], in_=x_sb[:], func=AF.Square), reads=list(b_x), writes=[out_b])
        S.group("pe", [lambda p, c=c: p.matmul(pst[:, :TT], ones_bf[:], out_t[:, c, :], start=(c == 0), stop=(c == 7))
                       for c in range(8)], reads=[out_b, b_par], writes=[psb])
        r_t, r_b = rs_from_ps(pst, psb, 1.0 / D)
        for c in range(8):
            S.op("dve", lambda v: v.scalar_tensor_tensor(
                out=out_t[:, c, :], in0=x_sb[:, c, :], scalar=gain_sb[:, c:c + 1], in1=r_t[:],
                op0=ALU.mult, op1=ALU.mult), reads=[b_x[c], r_b, b_par], writes=[out_b])
        yield 1.3
        yield 6.0

    def proj_fm(slab, bslab, col, rhs_t, rhs_b, nchunk=8, hold=False):
        pst, psb, pi_ = MP.get(hold=hold)
        S.group("pe", [lambda p, c=c: p.matmul(pst[:, :TT], slab[:, c, col:col + 128], rhs_t[:, c, :],
                                               start=(c == 0), stop=(c == nchunk - 1)) for c in range(nchunk)],
                reads=[bslab, rhs_b], writes=[psb])
        return pst, psb, pi_

    def mixer(t):
        tok0 = t * TT
        seq_t = t % TPS
        slot = t % NSLOT
        x_sb, b_x = xs[t % 2], b_xs[t % 2]
        S.dma("sp", lambda q: q.dma_start(out=x_sb[:], in_=xT_d[t]), b_x[0], writes=list(b_x))
        yield 0.0
        if stage < 1:
            return
        yield from rmsnorm(MP, x_sb, b_x, g1_sb, hT, b_hT)
        dump("h", hT[:], [b_hT], dslice("h", 1024, tok0))

        pend = []

        def qk_finish(i, sq, bq):
            pm, pmb, _ = MP.get()
            S.group("pe", [lambda p: p.matmul(pm[:, :TT], bd64[:], sq[:], start=True, stop=True)],
                    reads=[bq, b_par], writes=[pmb])
            S.op("dve", lambda v: v.tensor_copy(out=ms_all[:, i, :], in_=pm[:, :TT]), reads=[pmb], writes=[b_ms])

        def qk_norm():
            S.op("act", lambda a: a.activation(out=ms_all[:], in_=ms_all[:], func=AF.Ln, bias=EPS), reads=[b_ms], writes=[b_ms])
            S.op("act", lambda a: a.activation(out=ms_all[:], in_=ms_all[:], func=AF.Exp, scale=-0.5), reads=[b_ms], writes=[b_ms])
            S.op("dve", lambda v: v.scalar_tensor_tensor(out=qT[:], in0=qT[:], scalar=cq[:, 0:1], in1=ms_all[:, 0:4, :],
                                                         op0=ALU.mult, op1=ALU.mult), reads=[b_qT, b_ms, b_par], writes=[b_qT])
            S.op("dve", lambda v: v.tensor_tensor(out=kT[slot][:], in0=kT[slot][:], in1=ms_all[:, 4:8, :], op=ALU.mult),
                 reads=[b_kT[slot], b_ms], writes=[b_kT[slot]])
        for which in range(2):
            for half in range(2):
                slab, bslab = load_slab(t, 0, 2 * which + half)
                for m in range(2 * half, 2 * half + 2):
                    pq, pqb, pqi = proj_fm(slab, bslab, (m % 2) * 128, hT, b_hT)
                    sq, bq = sq_sb[m % 2], b_sq[m % 2]
                    S.op("act", lambda a: a.activation(out=sq[:], in_=pq[:, :TT], func=AF.Square),
                         reads=[pqb], writes=[bq])
                    if which == 0:
                        S.op("act", lambda a: a.activation(out=qT[:, m, :], in_=pq[:, :TT], func=AF.Copy),
                             reads=[pqb], writes=[b_qT])
                    else:
                        S.op("act", lambda a: a.activation(out=kT[slot][:, m, :], in_=pq[:, :TT], func=AF.Copy),
                             reads=[pqb], writes=[b_kT[slot]])
                    yield 1.2
                    if pend:
                        qk_finish(*pend.pop())
                    pend.append((4 * which + m, sq, bq))
        if stage < 2:
            qk_finish(*pend.pop())
            qk_norm()
            return
        for half in range(2):
            slab, bslab = load_slab(t, 0, 4 + half)
            for blk in range(NB):
                pv, pvb, _ = MP.get()
                S.group("pe", [lambda p, c=c: p.matmul(
                    pv[:, 0:256], hT[:, c, blk * 128:(blk + 1) * 128], slab[:, c, :],
                    start=(c == 0), stop=(c == 7)) for c in range(8)],
                    reads=[bslab, b_hT], writes=[pvb])
                S.op("act", lambda a: a.activation(
                    out=Vaug[slot][:, blk, 4 * half:4 * half + 4, 0:64],
                    in_=pv[:, 0:256].rearrange("p (h d) -> p h d", h=4), func=AF.Copy),
                    reads=[pvb], writes=[b_V[slot]])
                yield 1.2
                if pend:
                    qk_finish(*pend.pop())
                    qk_norm()
        dump("q", qT[:], [b_qT], dslice("q", 512, tok0))
        dump("k", kT[slot][:], [b_kT[slot]], dslice("k", 512, tok0))

        hT_kt = hT[:].rearrange("p c (k t) -> p c t k", t=8)
        Utok_g = Utok[:].rearrange("k t c -> k (t c)").rearrange("k (g t c) -> k g t c", g=32, t=8)
        for half in range(2):
            slab, bslab = load_slab(t, 0, 6 + half)
            for tau in range(8):
                pu, pub, _ = MP.get()
                S.group("pe", [lambda p, c=c: p.matmul(
                    pu[0:NK, 0:256], hT_kt[:, c, tau, :], slab[:, c, :],
                    start=(c == 0), stop=(c == 7)) for c in range(8)],
                    reads=[bslab, b_hT], writes=[pub])
                S.op("act", lambda a: a.activation(
                    out=Utok_g[:, 16 * half:16 * half + 16, tau, :],
                    in_=pu[0:NK, 0:256].rearrange("k (g c) -> k g c", c=16), func=AF.Copy),
                    reads=[pub], writes=[b_Utok])
                yield 1.2

        if stage >= 4:
            pt, ptb, _ = MP.get()
            ptv = pt[:].bitcast(BF16).rearrange("p (g k) -> p g k", k=NK)
            S.group("pe", [lambda p, g=g: p.transpose(ptv[:, g, :], Utok_g[:, g].rearrange("k t c -> k (t c)"),
                                                      identb[0:NK, 0:NK]) for g in range(32)],
                    reads=[b_Utok, b_par], writes=[ptb])
            S.op("act", lambda a: a.activation(out=Ush[:], in_=ptv[:, 0:32, :], func=AF.Copy),
                 reads=[ptb], writes=[b_Ush])
            yield 2.0
            for ri, MB in enumerate((MBre, MBim)):
                pss, pssb, _ = MP.get()
                psv = pss[:, 0:16 * NK].rearrange("p (g k) -> p g k", k=NK)
                S.group("pe", [lambda p, g=g: p.matmul(
                    psv[64 * (g % 2):64 * (g % 2) + 64, g // 2, :], MB[:, g, :], Ush[:, g, :], start=True, stop=True)
                    for g in range(32)], reads=[b_Ush, b_tab], writes=[pssb])
                S.op("act", lambda a: a.activation(out=Sf[:, ri], in_=psv, func=AF.Copy),
                     reads=[pssb], writes=[b_Sf])
                yield 2.0
            if seq_t == 0:
                S.op("dve", lambda v: v.memset(Hf[:, :, :, 0:1], 0.0), writes=[b_Hf])
            else:
                S.op("dve", lambda v: v.tensor_copy(out=Hf[:, :, :, 0:1], in_=Hf[:, :, :, NK:NK + 1]),
                     reads=[b_Hf], writes=[b_Hf])
            c_re, c_im = Hf[:, 0, :, 0], Hf[:, 1, :, 0]

            def dv(fn, reads, writes):
                S.op("dve", fn, reads=reads, writes=writes)
            rw = [b_Hf, b_Sf, b_tab]
            dv(lambda v: v.tensor_tensor(out=cz[:, 0], in0=A8[:, 0], in1=c_re, op=ALU.mult), rw, [b_Sf])
            dv(lambda v: v.tensor_tensor(out=cz[:, 1], in0=A8[:, 1], in1=c_im, op=ALU.mult), rw, [b_Sf])
            dv(lambda v: v.tensor_tensor(out=cz[:, 2], in0=A8[:, 0], in1=c_im, op=ALU.mult), rw, [b_Sf])
            dv(lambda v: v.tensor_tensor(out=cz[:, 3], in0=A8[:, 1], in1=c_re, op=ALU.mult), rw, [b_Sf])
            dv(lambda v: v.tensor_tensor(out=cz[:, 0], in0=cz[:, 0], in1=cz[:, 1], op=ALU.subtract), rw, [b_Sf])
            dv(lambda v: v.tensor_tensor(out=cz[:, 2], in0=cz[:, 2], in1=cz[:, 3], op=ALU.add), rw, [b_Sf])
            dv(lambda v: v.tensor_tensor(out=Sf[:, 0, :, 0], in0=Sf[:, 0, :, 0], in1=cz[:, 0], op=ALU.add), rw, [b_Sf])
            dv(lambda v: v.tensor_tensor(out=Sf[:, 1, :, 0], in0=Sf[:, 1, :, 0], in1=cz[:, 2], op=ALU.add), rw, [b_Sf])
            yield 1.0
            dv(lambda v: v.tensor_tensor(out=Tf[:, 0], in0=Sf[:, 0], in1=Ere[:], op=ALU.mult), [b_Sf, b_tab], [b_Tf])
            dv(lambda v: v.tensor_tensor(out=Tf[:, 1], in0=Sf[:, 1], in1=Eim[:], op=ALU.mult), [b_Sf, b_tab], [b_Tf])
            dv(lambda v: v.tensor_tensor(out=Xf[:, 0], in0=Tf[:, 0], in1=Tf[:, 1], op=ALU.add), [b_Tf], [b_Xf])
            yield 1.0
            dv(lambda v: v.tensor_tensor(out=Tf[:, 0], in0=Sf[:, 1], in1=Ere[:], op=ALU.mult), [b_Sf, b_tab, b_Xf], [b_Tf])
            dv(lambda v: v.tensor_tensor(out=Tf[:, 1], in0=Sf[:, 0], in1=Eim[:], op=ALU.mult), [b_Sf, b_tab], [b_Tf])
            dv(lambda v: v.tensor_tensor(out=Xf[:, 1], in0=Tf[:, 0], in1=Tf[:, 1], op=ALU.subtract), [b_Tf], [b_Xf])
            yield 1.0
            Rflat = Rtab[:].rearrange("p g k -> p (g k)")
            for ri in range(2):
                dv(lambda v: v.tensor_tensor_scan(
                    out=Zf[:, ri].rearrange("p g k -> p (g k)"), data0=Rflat,
                    data1=Xf[:, ri].rearrange("p g k -> p (g k)"), initial=0.0, op0=ALU.mult, op1=ALU.add),
                    [b_Xf, b_tab], [b_Zf])
                yield 1.0
            dv(lambda v: v.tensor_tensor(out=Tf[:, 0], in0=Zf[:, 0], in1=Ere[:], op=ALU.mult), [b_Zf, b_tab], [b_Tf])
            dv(lambda v: v.tensor_tensor(out=Tf[:, 1], in0=Zf[:, 1], in1=Eim[:], op=ALU.mult), [b_Zf, b_tab], [b_Tf])
            dv(lambda v: v.tensor_tensor(out=Hf[:, 0, :, 1:NK + 1], in0=Tf[:, 0], in1=Tf[:, 1], op=ALU.subtract), [b_Tf], [b_Hf])
            yield 1.0
            dv(lambda v: v.tensor_tensor(out=Tf[:, 0], in0=Zf[:, 0], in1=Eim[:], op=ALU.mult), [b_Zf, b_tab, b_Hf], [b_Tf])
            dv(lambda v: v.tensor_tensor(out=Tf[:, 1], in0=Zf[:, 1], in1=Ere[:], op=ALU.mult), [b_Zf, b_tab], [b_Tf])
            dv(lambda v: v.tensor_tensor(out=Hf[:, 1, :, 1:NK + 1], in0=Tf[:, 0], in1=Tf[:, 1], op=ALU.add), [b_Tf], [b_Hf])
            yield 1.0
            S.op("dve", lambda v: v.tensor_copy(out=Hb[:], in_=Hf[:, :, :, 0:NK]), reads=[b_Hf], writes=[b_Hb])
            yield 1.0

        for j in range(8):
            slab, bslab = load_slab(t, 0, 8 + j)
            for m in range(2):
                ci = j * 2 + m
                pg, pgb, _ = proj_fm(slab, bslab, m * 128, hT, b_hT)
                S.op("act", lambda a: a.activation(
                    out=gateT[:, ci, :], in_=pg[:, :TT], func=AF.Tanh, bias=hbias[:, ci:ci + 1], scale=0.5),
                    reads=[pgb, b_par], writes=[b_gate])
                yield 1.2
        if stage < 3:
            return

        for blk in range(NB):
            m_abs = seq_t * NB + blk
            kbs = [kb for kb in range(5) if m_abs - 4 + kb >= 0]
            for hg in range(2):
                po, pob, poi = MP.get(hold=True)
                po4 = po[:, 0:260].rearrange("p (h d) -> p h d", h=4)
                pend = []
                first = [True]

                def pv_step(ip, ks, kblk):
                    fns = []
                    for j in range(4):
                        h = 2 * j + hg
                        fns.append(lambda p, j=j, h=h, st=(first[0] and j == 0): p.matmul(
                            po[:, j * 65:(j + 1) * 65], PT[ip][:, j * 128:(j + 1) * 128],
                            Vaug[ks][:, kblk, h, :], start=st, stop=True, skip_group_check=True))
                    S.group("pe", fns, reads=[b_PT[ip], b_V[ks]], writes=[pob])
                    first[0] = False
                for kb in kbs:
                    ab = m_abs - 4 + kb
                    kt_ = (t - seq_t) + ab // NB
                    ks = kt_ % NSLOT
                    kblk = ab % NB
                    pst, psb, _ = MP.get()
                    fns = []
                    for j in range(4):
                        h = 2 * j + hg
                        mchunk, hb = h // 2, 64 * (h % 2)
                        fns.append(lambda p, j=j, mchunk=mchunk, hb=hb: p.matmul(
                            pst[:, j * 128:(j + 1) * 128],
                            kT[ks][hb:hb + 64, mchunk, kblk * 128:(kblk + 1) * 128],
                            qT[hb:hb + 64, mchunk, blk * 128:(blk + 1) * 128], start=True, stop=True))
                    S.group("pe", fns, reads=[b_kT[ks], b_qT], writes=[psb])
                    ia = att_ctr[0] % 2
                    att_ctr[0] += 1
                    S.op("dve", lambda v: v.tensor_tensor(
                        out=att_tmp[ia][:].rearrange("p (h q) -> p h q", h=4),
                        in0=pst[:, :].rearrange("p (h q) -> p h q", h=4),
                        in1=biasT4[:, kb, :, :].rearrange("p (j two) q -> p j two q", two=2)[:, :, hg, :], op=ALU.add),
                        reads=[psb, b_bias], writes=[b_att_tmp[ia]])
                    ip = att_ctr[1] % 3
                    att_ctr[1] += 1
                    S.op("act", lambda a: a.activation(out=PT[ip][:], in_=att_tmp[ia][:], func=AF.Exp),
                         reads=[b_att_tmp[ia]], writes=[b_PT[ip]])
                    yield 0.4
                    if len(pend) >= 2:
                        pv_step(*pend.pop(0))
                    pend.append((ip, ks, kblk))
                while pend:
                    pv_step(*pend.pop(0))
                    yield 0.3
                S.op("dve", lambda v: v.reciprocal(out=rden[:, hg, :], in_=po4[:, :, 64]),
                     reads=[pob], writes=[b_rden[hg]])
                S.op("dve", lambda v: v.tensor_tensor(
                    out=yatt[:, blk, :].rearrange("p (j two d) -> p j two d", two=2, d=64)[:, :, hg, :],
                    in0=po4[:, :, 0:64],
                    in1=rden[:, hg, :].rearrange("p (h o) -> p h o", o=1).to_broadcast([128, 4, 64]),
                    op=ALU.mult), reads=[pob, b_rden[hg]], writes=[b_yatt[blk]])
                MP.release(poi)
            pt, ptb, _ = MP.get()
            ptv = pt[:].bitcast(BF16)
            S.group("pe", [lambda p, c=c: p.transpose(
                ptv[:, c * 128:(c + 1) * 128], yatt[:, blk, c * 128:(c + 1) * 128], identb[:]) for c in range(4)],
                reads=[b_yatt[blk], b_par], writes=[ptb])
            S.op("act", lambda a: a.activation(
                out=yaT[:, :, blk * 128:(blk + 1) * 128], in_=ptv[:, 0:512].rearrange("p (c t) -> p c t", c=4),
                func=AF.Copy), reads=[ptb], writes=[b_yaT])
            yield 0.3
        dump("ya", yaT[:], [b_yaT], dslice("ya", 512, tok0))
        if stage < 4:
            return

        for g0 in range(0, 32, 4):
            py, pyb, _ = MP.get()
            fns = []
            for gi in range(4):
                g = g0 + gi
                gp = g // 2
                o = py[0:NK, gi * 128:(gi + 1) * 128]
                fns.append(lambda p, o=o, g=g, st=(gi == 0): p.matmul(o, Ush[:, g, :], T_all[:, g, :], start=st, stop=False,
                                                                      skip_group_check=True))
                fns.append(lambda p, o=o, gp=gp, g=g: p.matmul(o, Hb[:, 0, gp, :], MCre[:, g, :],
                                                              start=False, stop=False, skip_group_check=True))
                fns.append(lambda p, o=o, gp=gp, g=g: p.matmul(o, Hb[:, 1, gp, :], MCim[:, g, :],
                                                              start=False, stop=True, skip_group_check=True))
            S.group("pe", fns, reads=[b_Ush, b_Hb, b_tab], writes=[pyb])
            S.op("act", lambda a: a.activation(
                out=Utok[:, :, g0 * 16:(g0 + 4) * 16].rearrange("k t (g c) -> k t g c", g=4),
                in_=py[0:NK, :].rearrange("k (g t c) -> k t g c", g=4, t=8), func=AF.Gelu_apprx_tanh),
                reads=[pyb, b_Ush], writes=[b_Utok])
            yield 0.8
        pt, ptb, _ = MP.get()
        ptz = pt[:].bitcast(BF16)[:, 0:4 * TT].rearrange("p (c t k) -> p c t k", c=4, t=8)
        S.group("pe", [lambda p, cc=cc, tau=tau: p.transpose(
            ptz[:, cc, tau, :], Utok[:, tau, cc * 128:(cc + 1) * 128], identb[0:NK, 0:NK])
            for cc in range(4) for tau in range(8)], reads=[b_Utok, b_par], writes=[ptb])
        S.op("act", lambda a: a.activation(out=zT[:].rearrange("p c (k t) -> p c t k", t=8), in_=ptz, func=AF.Copy, scale=0.5),
             reads=[ptb], writes=[b_zT])
        yield 2.0
        dump("z", zT[:], [b_zT], dslice("z", 512, tok0))
        for m in range(4):
            pg, pgb, _ = MP.get()
            S.group("pe", [lambda p, c=c: p.matmul(pg[:, :TT], wglu[:, c, m * 128:(m + 1) * 128], zT[:, c, :],
                                                   start=(c == 0), stop=(c == 3)) for c in range(4)],
                    reads=[b_wres, b_zT], writes=[pgb])
            S.op("act", lambda a: a.activation(out=sig[m % 2][:], in_=pg[:, :TT], func=AF.Tanh,
                                               bias=hbias[:, 16 + m:17 + m], scale=1.0),
                 reads=[pgb, b_par], writes=[b_sig[m % 2]])
            S.op("dve", lambda v: v.scalar_tensor_tensor(out=ysT[:, m, :], in0=sig[m % 2][:], scalar=1.0, in1=zT[:, m, :],
                                                         op0=ALU.add, op1=ALU.mult),
                 reads=[b_zT, b_sig[m % 2]], writes=[b_ysT])
            yield 0.6
        dump("ys", ysT[:], [b_ysT], dslice("ys", 512, tok0))
        if stage < 5:
            return

        for fc in range(8):
            if fc % 2 == 0:
                wbr, b_wbr = load_slab(t, 1, fc // 2)
            fo = (fc % 2) * 128
            pa, pab, _ = MP.get()
            S.group("pe", [lambda p, c=c: p.matmul(pa[:, :TT], wbr[:, c, fo:fo + 128], yaT[:, c, :],
                                                   start=(c == 0), stop=(c == 3)) for c in range(4)],
                    reads=[b_wbr, b_yaT], writes=[pab])
            pb, pbb, _ = MP.get()
            S.group("pe", [lambda p, c=c: p.matmul(pb[:, :TT], wbr[:, 4 + c, fo:fo + 128], ysT[:, c, :],
                                                   start=(c == 0), stop=(c == 3)) for c in range(4)],
                    reads=[b_wbr, b_ysT], writes=[pbb])
            S.op("dve", lambda v: v.scalar_tensor_tensor(out=m12[0][:], in0=gateT[:, fc, :], scalar=1.0, in1=pa[:, :TT],
                                                         op0=ALU.add, op1=ALU.mult),
                 reads=[pab, b_gate], writes=[b_m12[0]])
            S.op("dve", lambda v: v.scalar_tensor_tensor(out=m12[1][:], in0=gateT[:, 8 + fc, :], scalar=1.0, in1=pb[:, :TT],
                                                         op0=ALU.add, op1=ALU.mult),
                 reads=[pbb, b_gate], writes=[b_m12[1]])
            S.op("dve", lambda g_: g_.tensor_tensor(out=mT[:, fc, :], in0=m12[0][:], in1=m12[1][:], op=ALU.add),
                 reads=[b_m12[0], b_m12[1]], writes=[b_mT])
            yield 1.2
        for fc in range(8):
            if fc % 2 == 0:
                wout, b_wout = load_slab(t, 2, fc // 2)
            fo = (fc % 2) * 128
            po, pob, _ = MP.get()
            S.group("pe", [lambda p, c=c: p.matmul(po[:, :TT], wout[:, c, fo:fo + 128], mT[:, c, :],
                                                   start=(c == 0), stop=(c == 7)) for c in range(8)],
                    reads=[b_wout, b_mT], writes=[pob])
            S.op("dve", lambda v: v.scalar_tensor_tensor(out=x_sb[:, fc, :], in0=po[:, :TT], scalar=0.5, in1=x_sb[:, fc, :],
                                                         op0=ALU.mult, op1=ALU.add),
                 reads=[pob, b_x[fc]], writes=[b_x[fc]])
            yield 1.2
        dump("x1", x_sb[:], b_x, dslice("x1", 1024, tok0))

    def store_x(t):
        tok0 = t * TT
        x_sb, b_x = xs[t % 2], b_xs[t % 2]
        for c in range(8):
            S.dma("sp", lambda q: q.dma_start(out=outT_d[t, :, c, :], in_=x_sb[:, c, :]),
                  b_x[c], reads=[b_x[c]])

    def moe(t):
        tok0 = t * TT
        x_sb, b_x = xs[t % 2], b_xs[t % 2]
        if stage < 6:
            store_x(t)
            return
        yield from rmsnorm(EP, x_sb, b_x, g2_sb, h2T, b_h2T)
        pr_, prb, _ = EP.get()
        prv = pr_[:, 0:NB * 20].rearrange("p (b j) -> p b j", j=20)
        for blk in range(NB):
            S.group("pe", [lambda p, c=c: p.matmul(prv[:, blk, :], h2T[:, c, blk * 128:(blk + 1) * 128], wr[:, c, :],
                                                   start=(c == 0), stop=(c == 7)) for c in range(8)],
                    reads=[b_h2T, b_wres], writes=[prb])
        R = [b_rt, b_par]

        def rv(fn, reads=R, writes=(b_rt,)):
            S.op("dve", fn, reads=list(reads), writes=list(writes))
        rv(lambda v: v.tensor_tensor(out=Lr[:], in0=prv, in1=rbias_sb.rearrange("p (o j) -> p o j", o=1).to_broadcast([128, NB, 20]),
                                     op=ALU.add), reads=[prb, b_par, b_rt])
        G = Lr[:, :, 0:4]
        E4 = Lr[:, :, 4:20].rearrange("p b (g j) -> p b g j", g=4)
        gmax, gsum, gprob, m1, m2, dd, w1, w2 = [rt[:, i, :, 0:1] for i in range(8)]
        gmask, ge, ing, x2 = [rt[:, 8 + i] for i in range(4)]
        mask1 = rt16[:, :, 0:4]
        mask2 = rt16[:, :, 4:8]
        wj = rt16[:, :, 8:12]
        tj = rt16[:, :, 12:16]

        def b4(x):
            return x.to_broadcast([128, NB, 4])
        rv(lambda v: v.tensor_reduce(out=gmax, in_=G, axis=AX.X, op=ALU.max))
        rv(lambda v: v.tensor_tensor(out=ge, in0=G, in1=b4(gmax), op=ALU.subtract))
        S.op("act", lambda a: a.activation(out=ge, in_=ge, func=AF.Exp), reads=R, writes=[b_rt])
        rv(lambda v: v.tensor_reduce(out=gsum, in_=ge, axis=AX.X, op=ALU.add))
        rv(lambda v: v.reciprocal(out=gprob, in_=gsum))
        rv(lambda v: v.tensor_tensor(out=gmask, in0=G, in1=b4(gmax), op=ALU.is_equal))
        sel4 = gates[:].rearrange("p b (g j) -> p b g j", g=4)
        rv(lambda v: v.tensor_tensor(out=sel4, in0=E4, in1=gmask.rearrange("p b (g o) -> p b g o", o=1).to_broadcast([128, NB, 4, 4]),
                                     op=ALU.mult))
        rv(lambda v: v.tensor_reduce(out=ing, in_=sel4.rearrange("p b g j -> p b j g"), axis=AX.X, op=ALU.add))
        rv(lambda v: v.tensor_reduce(out=m1, in_=ing, axis=AX.X, op=ALU.max))
        rv(lambda v: v.tensor_tensor(out=mask1, in0=ing, in1=b4(m1), op=ALU.is_equal))
        rv(lambda v: v.scalar_tensor_tensor(out=x2, in0=mask1, scalar=-1e30, in1=ing, op0=ALU.mult, op1=ALU.add))
        rv(lambda v: v.tensor_reduce(out=m2, in_=x2, axis=AX.X, op=ALU.max))
        rv(lambda v: v.tensor_tensor(out=mask2, in0=x2, in1=b4(m2), op=ALU.is_equal))
        rv(lambda v: v.tensor_tensor(out=dd, in0=m2, in1=m1, op=ALU.subtract))
        S.op("act", lambda a: a.activation(out=dd, in_=dd, func=AF.Exp), reads=R, writes=[b_rt])
        rv(lambda v: v.tensor_scalar(out=w1, in0=dd, scalar1=1.0, scalar2=None, op0=ALU.add))
        rv(lambda v: v.reciprocal(out=w1, in_=w1))
        rv(lambda v: v.tensor_tensor(out=w2, in0=dd, in1=w1, op=ALU.mult))
        rv(lambda v: v.tensor_tensor(out=w1, in0=w1, in1=gprob, op=ALU.mult))
        rv(lambda v: v.tensor_tensor(out=w2, in0=w2, in1=gprob, op=ALU.mult))
        rv(lambda v: v.tensor_tensor(out=wj, in0=mask1, in1=b4(w1), op=ALU.mult))
        rv(lambda v: v.tensor_tensor(out=tj, in0=mask2, in1=b4(w2), op=ALU.mult))
        rv(lambda v: v.tensor_tensor(out=wj, in0=wj, in1=tj, op=ALU.add))
        rv(lambda v: v.tensor_tensor(out=sel4, in0=gmask.rearrange("p b (g o) -> p b g o", o=1).to_broadcast([128, NB, 4, 4]),
                                     in1=wj.rearrange("p b (o j) -> p b o j", o=1).to_broadcast([128, NB, 4, 4]), op=ALU.mult))
        yield 12.0
        pgt, pgtb, _ = EP.get()
        S.group("pe", [lambda p, blk=blk: p.transpose(pgt[0:16, blk * 128:(blk + 1) * 128], gates[:, blk, :], identf)
                       for blk in range(NB)], reads=[b_rt, b_par], writes=[pgtb])
        S.op("act", lambda a: a.activation(out=gatesT[:], in_=pgt[0:16, 0:TT], func=AF.Copy), reads=[pgtb], writes=[b_gatesT])
        if "gates" in dbg_d:
            dump("gates", gatesT[:], [b_gatesT], dbg_d["gates"][0:16, tok0:tok0 + TT])
        yield 3.0
        if stage < 7:
            store_x(t)
            return

        for i_ in range(NWD):
            wd_prefetch(t, i_)
        for i_ in range(NGU):
            wgu_prefetch(t, i_)

        def emit_gm(e):
            if e < NE:
                S.op("dve", lambda v: v.tensor_scalar(out=gm[e % 3][:], in0=gatesT[:], scalar1=identf[0:16, e:e + 1],
                                                      scalar2=0.5, op0=ALU.mult, op1=ALU.mult),
                     reads=[b_gatesT, b_par], writes=[b_gm[e % 3]])

        def emit_bcast(e):
            if e < NE:
                pgb_, pgbb, _ = EP.get()
                S.group("pe", [lambda p: p.matmul(pgb_[:, :TT], ones_bf[0:16, :], gm[e % 3][:], start=True, stop=True)],
                        reads=[b_gm[e % 3], b_par], writes=[pgbb])
                S.op("act", lambda a: a.activation(out=gbs[e % 2][:], in_=pgb_[:, :TT], func=AF.Copy),
                     reads=[pgbb], writes=[b_gbs[e % 2]])
        emit_gm(0)
        emit_gm(1)
        emit_bcast(0)
        for hf in range(4):
            for e in range(4 * hf, 4 * hf + 4):
                emit_gm(e + 2)
                for fh in range(2):
                    if fh == 1:
                        emit_bcast(e + 1)
                    kc = 2 * (e - 4 * hf) + fh
                    w_, bw_ = wgu_live.pop((t, 2 * e + fh))
                    pa, pab, _ = EP.get()
                    S.group("pe", [lambda p, c=c: p.matmul(pa[:, :TT], w_[:, 0, c, :], h2T[:, c, :], start=(c == 0), stop=(c == 7))
                                   for c in range(8)], reads=[bw_, b_h2T], writes=[pab])
                    pu, pub, _ = EP.get()
                    S.group("pe", [lambda p, c=c: p.matmul(pu[:, :TT], w_[:, 1, c, :], h2T[:, c, :], start=(c == 0), stop=(c == 7))
                                   for c in range(8)], reads=[bw_, b_h2T], writes=[pub])
                    i2 = kc % 2
                    S.op("act", lambda a: a.activation(out=sil[i2][:], in_=pa[:, :TT], func=AF.Tanh, scale=0.5),
                         reads=[pab], writes=[b_sil[i2]])
                    S.op("dve", lambda v: v.scalar_tensor_tensor(out=s2b[i2][:], in0=sil[i2][:], scalar=1.0, in1=pa[:, :TT],
                                                                 op0=ALU.add, op1=ALU.mult),
                         reads=[b_sil[i2], pab], writes=[b_s2[i2]])
                    S.op("dve", lambda v: v.tensor_tensor(out=gsb[i2][:], in0=s2b[i2][:], in1=gbs[e % 2][:], op=ALU.mult),
                         reads=[b_s2[i2], b_gbs[e % 2]], writes=[b_gs[i2]])
                    S.op("dve", lambda v: v.tensor_tensor(out=hid[:, kc, :], in0=gsb[i2][:], in1=pu[:, :TT], op=ALU.mult),
                         reads=[b_gs[i2], pub], writes=[b_hid[kc]])
                    wgu_prefetch(t, 2 * e + fh + NGU)
                    yield 2.3
            for fc in range(8):
                w_, bw_ = wd_live.pop((t, 8 * hf + fc))
                pd, pdb, _ = EP.get()
                S.group("pe", [lambda p, kc=kc: p.matmul(pd[:, :TT], w_[:, kc, :], hid[:, kc, :],
                                                         start=(kc == 0), stop=(kc == 7)) for kc in range(8)],
                        reads=[bw_] + b_hid, writes=[pdb])
                S.op("dve", lambda v: v.tensor_tensor(out=x_sb[:, fc, :], in0=pd[:, :TT], in1=x_sb[:, fc, :], op=ALU.add),
                     reads=[pdb, b_x[fc]], writes=[b_x[fc]])
                wd_prefetch(t, 8 * hf + fc + NWD)
                if hf == 3:
                    S.dma("sp", lambda q: q.dma_start(out=outT_d[t, :, fc, :], in_=x_sb[:, fc, :]),
                          b_x[fc], reads=[b_x[fc]])
                yield 1.15

    def run_interleaved(ga, gb):
        wa = wb_ = 0.0
        a_live, b_live = ga is not None, gb is not None
        if a_live and b_live:
            next(gb)
            wb_ = TUNE["head"]
        while a_live or b_live:
            if a_live and (not b_live or wa <= wb_):
                try:
                    wa += next(ga)
                except StopIteration:
                    a_live = False
            else:
                try:
                    wb_ += next(gb) * TUNE["mix_scale"]
                except StopIteration:
                    b_live = False

    ntl = NT if ntiles is None else ntiles
    run_interleaved(mixer(0), None)
    for t in range(ntl):
        run_interleaved(moe(t), mixer(t + 1) if t + 1 < ntl else None)

    for i in range(2):
        for c in range(8):
            for ev in list(b_xs[i][c].r.values()):
                S.wait_event("sp", ev)
    for bt in dbg_final:
        for ev in list(bt.r.values()):
            S.wait_event("sp", ev)
    if debug:
        print("ins per engine", S.nins, "counts", S.cnt)
    S.emit()
    es.close()
    return nc


def _consts():
    identf = np.eye(128, dtype=np.float32)
    s_idx = np.arange(128) // 16
    maskLT = (s_idx[None, :] >= s_idx[:, None]).astype(np.float32)
    hb = np.arange(128) // 64
    bd64 = (hb[:, None] == hb[None, :]).astype(np.float32) / 64.0
    return np.ascontiguousarray(np.concatenate([identf, maskLT, bd64], axis=1))


def _bias_index():
    k = np.arange(128)[:, None, None]
    kb = np.arange(5)[None, :, None]
    q = np.arange(128)[None, None, :]
    qpos = 512 + q
    kpos = kb * 128 + k
    idx = np.clip(qpos - kpos, -63, 256) + 63
    qchunk = qpos // 64
    kchunk = kpos // 64
    valid = (kchunk <= qchunk) & (kchunk >= qchunk - 8)
    return idx, valid


def prepare_inputs(inputs):
    f = lambda a: np.ascontiguousarray(np.asarray(a, dtype=np.float32))
    x = f(inputs["x"])
    L = 0
    vec = np.zeros((128, 64), np.float32)
    vec[:, 0:8] = f(inputs["mix_norm_gain"])[L].reshape(8, 128).T
    vec[:, 8:16] = f(inputs["ffn_norm_gain"])[L].reshape(8, 128).T
    vec[:, 16:32] = f(inputs["b_gate"])[L].reshape(16, 128).T
    vec[:, 32:36] = f(inputs["b_glu"])[L].reshape(4, 128).T
    vec[:, 36] = np.tile(f(inputs["q_gain"])[L], 2)
    vec[:, 37] = np.tile(f(inputs["k_gain"])[L], 2)
    vec[:, 40:44] = f(inputs["group_bias"])[L][None, :]
    vec[:, 44:60] = f(inputs["expert_bias"])[L][None, :]
    idx, valid = _bias_index()
    rb = f(inputs["rel_bias"])[L]
    bt = rb[:, idx]
    bt = np.where(valid[None], bt, np.float32(NEG)).astype(np.float32)
    biasT = np.ascontiguousarray(bt.transpose(1, 2, 0, 3)).reshape(128, 5 * 8 * 128)

    def gp_layout(a):
        return a.reshape(16, 2, 64).transpose(1, 2, 0).reshape(128, 16)
    small = np.zeros((128, 48), np.float32)
    small[:, 0:16] = gp_layout(f(inputs["ssm_lambda_re"])[L])
    small[:, 16:32] = gp_layout(f(inputs["ssm_lambda_im"])[L])
    small[:, 32:48] = gp_layout(np.broadcast_to(f(inputs["ssm_log_step"])[L][:, None], (32, 64)))
    bc = np.zeros((128, 4, 256), np.float32)
    for i, nm in enumerate(("ssm_b_re", "ssm_b_im")):
        a = f(inputs[nm])[L].reshape(16, 2, 64, 16).transpose(1, 2, 0, 3).reshape(128, 256)
        bc[:, i] = a
    for i, nm in enumerate(("ssm_c_re", "ssm_c_im")):
        a = f(inputs[nm])[L].reshape(16, 2, 16, 64).transpose(1, 3, 0, 2).reshape(128, 256)
        bc[:, 2 + i] = a
    drep = np.ascontiguousarray(np.tile(f(inputs["ssm_d"])[L].reshape(32, 16).T, (8, 1)))
    w_r = np.ascontiguousarray(np.concatenate([f(inputs["w_group_router"])[L], f(inputs["w_expert_router"])[L]], axis=1))
    common = {
        "w_in": f(inputs["w_in"])[L], "w_glu": f(inputs["w_glu"])[L], "w_branch": f(inputs["w_branch"])[L],
        "w_out": f(inputs["w_out"])[L], "w_e_gate": f(inputs["w_e_gate"])[L], "w_e_up": f(inputs["w_e_up"])[L],
        "w_e_down": f(inputs["w_e_down"])[L], "w_r": w_r, "vecs": vec, "biasT": biasT, "ssm_small": small,
        "ssm_bc": bc, "drep": drep, "consts": _consts(),
    }
    xs = x.reshape(NCORE, TOK, D)
    in_maps = []
    for i in range(NCORE):
        m = dict(common)
        m["xT"] = np.ascontiguousarray(xs[i].reshape(TOK // 256, 256, 8, 128).transpose(0, 3, 2, 1))
        in_maps.append(m)
    return in_maps


_CACHE = {}


def kernel(**inputs):
    in_maps = prepare_inputs(inputs)
    if "nc" not in _CACHE:
        _CACHE["nc"] = build_program(TT=256)
    res = run_bass_kernel_spmd(_CACHE["nc"], in_maps, core_ids=list(range(NCORE)))
    out = np.stack([np.asarray(r["outT"]).transpose(0, 3, 2, 1).reshape(TOK, D) for r in res.results], axis=0)
    return np.ascontiguousarray(out.reshape(16, SEQ, D).astype(np.float32))
```
